# Optimizing a Trainium2 kernel written in Bass

```python
import math, functools
import jax, jax.numpy as jnp
from jax import lax
import numpy as np

D_MODEL = 1024
BATCH = 8
SEQ = 4096
DEPTH = 2
DEC_BATCH = 128
DEC_SEQ = 8
PAST_LEN = 16384
PAGE_SIZE = 128

N_A_LAYERS = DEPTH // 2
N_B_LAYERS = DEPTH - N_A_LAYERS
N_DENSE = (DEPTH + 1) // 2
N_MOE = DEPTH // 2
HGRN_EXPAND = 128
HGRN_HEADS = D_MODEL // HGRN_EXPAND
HGRN_DK = HGRN_EXPAND
HGRN_DV = D_MODEL // HGRN_HEADS
HGRN_CHUNK = 64
MLA_HEADS = 8
QK_NOPE = 128
QK_ROPE = 64
V_DIM = 128
KV_LORA = D_MODEL // 4
Q_LORA = (3 * D_MODEL) // 8
ROPE_THETA = 10000.0
Q_BLOCK = 128
SM_SCALE = (QK_NOPE + QK_ROPE) ** -0.5
NEG_INF = -1e30
D_FF = 2816
N_EXPERTS = 8
TOP_K = 2
D_FF_EXPERT = 2816
RMS_EPS = 1e-6

kernel_name = "hgrn2_mla_yoco_moe_step"


def _rmsnorm(x, g):
    xf = x.astype(jnp.float32)
    y = xf * lax.rsqrt(jnp.mean(xf * xf, axis=-1, keepdims=True) + RMS_EPS)
    return (y * g.astype(jnp.float32)).astype(x.dtype)


def _rope(x, pos):
    half = x.shape[-1] // 2
    inv = ROPE_THETA ** (-jnp.arange(half, dtype=jnp.float32) / half)
    ang = pos.astype(jnp.float32)[:, None] * inv[None, :]
    shape = (pos.shape[0],) + (1,) * (x.ndim - 3) + (half,)
    cos = jnp.cos(ang).reshape(shape)
    sin = jnp.sin(ang).reshape(shape)
    xf = x.astype(jnp.float32)
    x1, x2 = xf[..., :half], xf[..., half:]
    return jnp.concatenate([x1 * cos - x2 * sin, x2 * cos + x1 * sin], axis=-1).astype(x.dtype)


def _gla_recurrence(q, k, v, log_f, S0):
    B, T, H, DK = q.shape
    DV = v.shape[-1]
    L = HGRN_CHUNK if T % HGRN_CHUNK == 0 else T
    n = T // L

    def blocks(a):
        return a.reshape(B, n, L, H, a.shape[-1]).transpose(1, 0, 3, 2, 4)

    t = jnp.arange(L)
    causal = (t[:, None] >= t[None, :])[:, :, None]

    def step(S, inp):
        qc, kc, vc, gc = inp
        b = jnp.cumsum(gc, axis=2)
        o_inter = jnp.einsum('bhtk,bhkv->bhtv', qc * jnp.exp(b), S)
        diff = b[:, :, :, None, :] - b[:, :, None, :, :]
        decay = jnp.where(causal, jnp.exp(jnp.minimum(diff, 0.0)), 0.0)
        scores = jnp.einsum('bhtk,bhsk,bhtsk->bhts', qc, kc, decay)
        o_intra = jnp.einsum('bhts,bhsv->bhtv', scores, vc)
        b_last = b[:, :, -1:, :]
        S_new = (jnp.exp(b_last[:, :, 0, :])[..., None] * S
                 + jnp.einsum('bhsk,bhsv->bhkv', kc * jnp.exp(b_last - b), vc))
        return S_new, o_inter + o_intra

    S_T, o = lax.scan(step, S0, (blocks(q), blocks(k), blocks(v), blocks(log_f)))
    o = o.transpose(1, 0, 3, 2, 4).reshape(B, T, H, DV)
    return o, S_T


def _hgrn2_mixer(xn, S0, w_in, lb, g_o, w_out):
    B, T, _ = xn.shape
    hk = HGRN_HEADS * HGRN_DK
    hv = HGRN_HEADS * HGRN_DV
    proj = xn @ w_in
    q, z, i, g = jnp.split(proj, [hk, 2 * hk, 2 * hk + hv], axis=-1)
    zf = z.astype(jnp.float32)
    lb = lb.astype(jnp.float32)
    log_f = jnp.logaddexp(jnp.log(lb), jnp.log1p(-lb) + jax.nn.log_sigmoid(zf))
    k = (1.0 - lb) * jax.nn.sigmoid(-zf)
    qf = jax.nn.silu(q.astype(jnp.float32))
    shp_k = (B, T, HGRN_HEADS, HGRN_DK)
    shp_v = (B, T, HGRN_HEADS, HGRN_DV)
    o, S_T = _gla_recurrence(qf.reshape(shp_k), k.reshape(shp_k),
                             i.astype(jnp.float32).reshape(shp_v), log_f.reshape(shp_k),
                             S0.astype(jnp.float32))
    o = _rmsnorm(o, g_o) * jax.nn.silu(g.astype(jnp.float32)).reshape(shp_v)
    return o.reshape(B, T, hv).astype(xn.dtype) @ w_out, S_T


def _shared_kv(h, pos, g_in, w_dkv, g_kv):
    n = _rmsnorm(h, g_in)
    kv = n @ w_dkv
    c = _rmsnorm(kv[..., :KV_LORA], g_kv)
    kr = _rope(kv[..., KV_LORA:], pos)
    return c, kr


def _attend_prompt(q_abs, q_rope, c, kr):
    B, T, H, C = q_abs.shape
    nb = T // Q_BLOCK
    qa = q_abs.reshape(B, nb, Q_BLOCK, H, C).transpose(1, 0, 2, 3, 4)
    qr = q_rope.reshape(B, nb, Q_BLOCK, H, QK_ROPE).transpose(1, 0, 2, 3, 4)
    cf = c.astype(jnp.float32)
    krf = kr.astype(jnp.float32)
    key_pos = jnp.arange(T)

    def block(args):
        qa_b, qr_b, start = args
        s = (jnp.einsum('bqhc,bkc->bhqk', qa_b.astype(jnp.float32), cf)
             + jnp.einsum('bqhr,bkr->bhqk', qr_b.astype(jnp.float32), krf)) * SM_SCALE
        q_pos = start + jnp.arange(Q_BLOCK)
        mask = key_pos[None, :] <= q_pos[:, None]
        p = jax.nn.softmax(jnp.where(mask, s, NEG_INF), axis=-1)
        return jnp.einsum('bhqk,bkc->bqhc', p, cf)

    o = lax.map(block, (qa, qr, jnp.arange(nb) * Q_BLOCK))
    return o.transpose(1, 0, 2, 3, 4).reshape(B, T, H, C).astype(q_abs.dtype)


def _attend_sample(q_abs, q_rope, c, kr, cache_ckv, cache_krope, page_table):
    B, T, H, C = q_abs.shape
    P = page_table.shape[1] * PAGE_SIZE
    key_pos = jnp.arange(P + T)
    q_pos = P + jnp.arange(T)
    mask = key_pos[None, :] <= q_pos[:, None]

    def one(args):
        pt, qa, qr, cn, krn = args
        ck = jnp.concatenate([cache_ckv[pt].reshape(P, C).astype(jnp.float32),
                              cn.astype(jnp.float32)], axis=0)
        kk = jnp.concatenate([cache_krope[pt].reshape(P, QK_ROPE).astype(jnp.float32),
                              krn.astype(jnp.float32)], axis=0)
        s = (jnp.einsum('qhc,kc->hqk', qa.astype(jnp.float32), ck)
             + jnp.einsum('qhr,kr->hqk', qr.astype(jnp.float32), kk)) * SM_SCALE
        p = jax.nn.softmax(jnp.where(mask, s, NEG_INF), axis=-1)
        return jnp.einsum('hqk,kc->qhc', p, ck)

    o = lax.map(one, (page_table, q_abs, q_rope, c, kr))
    return o.astype(q_abs.dtype)


def _mla_mixer(xn, pos, c, kr, w_dq, g_q, w_uq, w_ukv, w_out, attend):
    B, T, _ = xn.shape
    cq = _rmsnorm(xn @ w_dq, g_q)
    q = (cq @ w_uq).reshape(B, T, MLA_HEADS, QK_NOPE + QK_ROPE)
    q_nope = q[..., :QK_NOPE]
    q_rope = _rope(q[..., QK_NOPE:], pos)
    w = w_ukv.reshape(KV_LORA, MLA_HEADS, QK_NOPE + V_DIM)
    q_abs = jnp.einsum('bthn,chn->bthc', q_nope, w[..., :QK_NOPE])
    o_lat = attend(q_abs, q_rope, c, kr)
    o = jnp.einsum('bthc,chv->bthv', o_lat, w[..., QK_NOPE:])
    return o.reshape(B, T, MLA_HEADS * V_DIM) @ w_out


def _swiglu(x, wg, wu, wd):
    return (jax.nn.silu(x @ wg) * (x @ wu)) @ wd


def _moe(x, w_r, wg, wu, wd):
    B, T, D = x.shape
    xf = x.reshape(B * T, D)
    logits = (xf @ w_r).astype(jnp.float32)
    top_v, top_i = lax.top_k(logits, TOP_K)
    gates = jax.nn.softmax(top_v, axis=-1)
    combine = jnp.einsum('nk,nke->ne', gates,
                         jax.nn.one_hot(top_i, N_EXPERTS, dtype=jnp.float32)).astype(x.dtype)
    out = jnp.zeros_like(xf)
    for e in range(N_EXPERTS):
        out = out + combine[:, e:e + 1] * _swiglu(xf, wg[e], wu[e], wd[e])
    return out.reshape(B, T, D)


def _trunk(x, pos, S0, attend, p):
    lb_all = jnp.cumsum(jax.nn.softmax(p["gamma_lb"].astype(jnp.float32), axis=0), axis=0)
    h = x
    c = kr = None
    new_S = []
    for layer in range(DEPTH):
        if layer < N_A_LAYERS:
            a = layer
            o, S = _hgrn2_mixer(_rmsnorm(h, p["g_mix_a"][a]), S0[a], p["w_in_a"][a], lb_all[a],
                                p["g_onorm_a"][a], p["w_out_a"][a])
            new_S.append(S)
        else:
            b = layer - N_A_LAYERS
            if b == 0:
                c, kr = _shared_kv(h, pos, p["g_kv_in"], p["w_dkv"], p["g_kv"])
            o = _mla_mixer(_rmsnorm(h, p["g_mix_b"][b]), pos, c, kr, p["w_dq"][b], p["g_q"][b],
                           p["w_uq"][b], p["w_ukv"], p["w_out_b"][b], attend)
        h = h + o
        hn = _rmsnorm(h, p["g_ffn"][layer])
        if layer % 2 == 0:
            d = layer // 2
            h = h + _swiglu(hn, p["w_ff_gate"][d], p["w_ff_up"][d], p["w_ff_down"][d])
        else:
            m = layer // 2
            h = h + _moe(hn, p["w_router"][m], p["w_e_gate"][m], p["w_e_up"][m], p["w_e_down"][m])
    y = _rmsnorm(h, p["g_final"])
    return y, c, kr, jnp.stack(new_S)


def setup_inputs(seed: int = 0) -> dict:
    key = jax.random.key(seed)
    ks = iter(jax.random.split(key, 48))

    def nrm(shape, scale):
        return jax.random.normal(next(ks), shape, jnp.float32) * scale

    def gain(shape):
        return 1.0 + 0.02 * jax.random.normal(next(ks), shape, jnp.float32)

    n_pages = PAST_LEN // PAGE_SIZE
    n_pool = (5 * DEC_BATCH * n_pages) // 4
    hk = HGRN_HEADS * HGRN_DK
    hv = HGRN_HEADS * HGRN_DV
    x_prompt = nrm((BATCH, SEQ, D_MODEL), 1.0)
    x_sample = nrm((DEC_BATCH, DEC_SEQ, D_MODEL), 1.0)
    cache_ckv = nrm((n_pool, PAGE_SIZE, KV_LORA), 1.0)
    cache_krope = nrm((n_pool, PAGE_SIZE, QK_ROPE), 1.0)
    state_hgrn = nrm((N_A_LAYERS, DEC_BATCH, HGRN_HEADS, HGRN_DK, HGRN_DV), 0.3)
    page_table = jax.random.permutation(next(ks), n_pool)[:DEC_BATCH * n_pages].reshape(
        DEC_BATCH, n_pages).astype(jnp.int32)
    return {
        "x_prompt": x_prompt,
        "x_sample": x_sample,
        "cache_ckv": cache_ckv,
        "cache_krope": cache_krope,
        "state_hgrn": state_hgrn,
        "page_table": page_table,
        "g_mix_a": gain((N_A_LAYERS, D_MODEL)),
        "w_in_a": nrm((N_A_LAYERS, D_MODEL, 2 * hk + 2 * hv), D_MODEL ** -0.5),
        "gamma_lb": nrm((DEPTH, hk), 0.1),
        "g_onorm_a": gain((N_A_LAYERS, HGRN_DV)),
        "w_out_a": nrm((N_A_LAYERS, hv, D_MODEL), hv ** -0.5),
        "g_kv_in": gain((D_MODEL,)),
        "w_dkv": nrm((D_MODEL, KV_LORA + QK_ROPE), D_MODEL ** -0.5),
        "g_kv": gain((KV_LORA,)),
        "w_ukv": nrm((KV_LORA, MLA_HEADS * (QK_NOPE + V_DIM)), KV_LORA ** -0.5),
        "g_mix_b": gain((N_B_LAYERS, D_MODEL)),
        "w_dq": nrm((N_B_LAYERS, D_MODEL, Q_LORA), D_MODEL ** -0.5),
        "g_q": gain((N_B_LAYERS, Q_LORA)),
        "w_uq": nrm((N_B_LAYERS, Q_LORA, MLA_HEADS * (QK_NOPE + QK_ROPE)), Q_LORA ** -0.5),
        "w_out_b": nrm((N_B_LAYERS, MLA_HEADS * V_DIM, D_MODEL), (MLA_HEADS * V_DIM) ** -0.5),
        "g_ffn": gain((DEPTH, D_MODEL)),
        "w_ff_gate": nrm((N_DENSE, D_MODEL, D_FF), D_MODEL ** -0.5),
        "w_ff_up": nrm((N_DENSE, D_MODEL, D_FF), D_MODEL ** -0.5),
        "w_ff_down": nrm((N_DENSE, D_FF, D_MODEL), D_FF ** -0.5),
        "w_router": nrm((N_MOE, D_MODEL, N_EXPERTS), D_MODEL ** -0.5),
        "w_e_gate": nrm((N_MOE, N_EXPERTS, D_MODEL, D_FF_EXPERT), D_MODEL ** -0.5),
        "w_e_up": nrm((N_MOE, N_EXPERTS, D_MODEL, D_FF_EXPERT), D_MODEL ** -0.5),
        "w_e_down": nrm((N_MOE, N_EXPERTS, D_FF_EXPERT, D_MODEL), D_FF_EXPERT ** -0.5),
        "g_final": gain((D_MODEL,)),
    }


def reference(x_prompt, x_sample, cache_ckv, cache_krope, state_hgrn, page_table,
              g_mix_a, w_in_a, gamma_lb, g_onorm_a, w_out_a,
              g_kv_in, w_dkv, g_kv, w_ukv,
              g_mix_b, w_dq, g_q, w_uq, w_out_b,
              g_ffn, w_ff_gate, w_ff_up, w_ff_down,
              w_router, w_e_gate, w_e_up, w_e_down, g_final):
    params = {
        "g_mix_a": g_mix_a, "w_in_a": w_in_a, "gamma_lb": gamma_lb,
        "g_onorm_a": g_onorm_a, "w_out_a": w_out_a,
        "g_kv_in": g_kv_in, "w_dkv": w_dkv, "g_kv": g_kv, "w_ukv": w_ukv,
        "g_mix_b": g_mix_b, "w_dq": w_dq, "g_q": g_q, "w_uq": w_uq, "w_out_b": w_out_b,
        "g_ffn": g_ffn, "w_ff_gate": w_ff_gate, "w_ff_up": w_ff_up, "w_ff_down": w_ff_down,
        "w_router": w_router, "w_e_gate": w_e_gate, "w_e_up": w_e_up, "w_e_down": w_e_down,
        "g_final": g_final,
    }
    b_p, t_p, _ = x_prompt.shape
    pos_p = jnp.arange(t_p, dtype=jnp.float32)
    S0_p = jnp.zeros((N_A_LAYERS, b_p, HGRN_HEADS, HGRN_DK, HGRN_DV), jnp.float32)
    y_prompt, ckv_prompt, krope_prompt, hgrn_prompt = _trunk(x_prompt, pos_p, S0_p, _attend_prompt, params)
    past = page_table.shape[1] * PAGE_SIZE
    pos_s = past + jnp.arange(x_sample.shape[1], dtype=jnp.float32)
    attend_s = functools.partial(_attend_sample, cache_ckv=cache_ckv, cache_krope=cache_krope,
                                 page_table=page_table)
    y_sample, ckv_sample, krope_sample, hgrn_sample = _trunk(x_sample, pos_s, state_hgrn, attend_s, params)
    hgrn_prompt = hgrn_prompt.astype(state_hgrn.dtype)
    hgrn_sample = hgrn_sample.astype(state_hgrn.dtype)
    return (y_prompt, y_sample, ckv_prompt, krope_prompt, ckv_sample, krope_sample, hgrn_prompt, hgrn_sample)
```

```python
import numpy as np
import ml_dtypes
from contextlib import ExitStack
import concourse.bass as bass
import concourse.mybir as mybir
from concourse.bass_utils import run_bass_kernel_spmd

F32 = mybir.dt.float32
BF16 = mybir.dt.bfloat16
I32 = mybir.dt.int32
AF = mybir.ActivationFunctionType
ALU = mybir.AluOpType
AX = mybir.AxisListType

D = 1024
NH = 8
DFF = 2816
NFC = DFF // 128
NE = 8
KVL = 256
QKR = 64
QL = 384
EPS = 1e-6
SM_SCALE = (128 + 64) ** -0.5
NEG = -1e30
NCORES = 8
SB = 16
ST = 8
PAGE = 128


class Tr:
    def __init__(self, nc):
        self.nc = nc
        self.eng = {'pe': nc.tensor, 'act': nc.scalar, 'dve': nc.vector, 'pool': nc.gpsimd, 'sp': nc.sync}
        self.sem = {e: nc.alloc_semaphore("sem_" + e) for e in self.eng}
        self.cnt = {e: 0 for e in self.eng}
        self.pending = {e: False for e in self.eng}
        self.W = {}
        self.R = {}
        self.waited = {e: {} for e in self.eng}
        self.dsem = {}
        self.nwait = 0
        self.nins = 0

    def _deps(self, reads, writes):
        deps = {}

        def add(sem, val):
            if deps.get(sem, 0) < val:
                deps[sem] = val
        for r in reads:
            w = self.W.get(r)
            if w:
                add(*w)
        for w_ in writes:
            w = self.W.get(w_)
            if w:
                add(*w)
            for s, v in self.R.get(w_, {}).items():
                add(s, v)
        return deps

    def _wait(self, e, deps):
        wd = self.waited[e]
        for sem, val in deps.items():
            if e == 'pe' and sem is self.sem['pe']:
                continue
            if wd.get(sem, 0) >= val:
                continue
            self.eng[e].wait_ge(sem, val)
            wd[sem] = val
            self.nwait += 1

    def _record(self, ev, reads, writes):
        for r in reads:
            d = self.R.setdefault(r, {})
            if d.get(ev[0], 0) < ev[1]:
                d[ev[0]] = ev[1]
        for w in writes:
            self.W[w] = ev
            self.R[w] = {}

    def op(self, e, fn, reads=(), writes=(), inc=True):
        self._wait(e, self._deps(reads, writes))
        ins = fn(self.eng[e])
        self.nins += 1
        if inc:
            self.cnt[e] += 1
            ins.then_inc(self.sem[e], 1)
            ev = (self.sem[e], self.cnt[e])
        else:
            ev = (self.sem[e], self.cnt[e] + 1)
        self._record(ev, reads, writes)
        return ins

    def dma(self, e, out, in_, reads, writes, key, indirect=None):
        self._wait(e, self._deps(reads, writes))
        if key not in self.dsem:
            self.dsem[key] = [self.nc.alloc_semaphore("dsem_%d" % len(self.dsem)), 0]
        ds = self.dsem[key]
        if indirect is None:
            ins = self.eng[e].dma_start(out=out, in_=in_)
        else:
            idx_ap, eoff = indirect
            ins = self.eng[e].indirect_dma_start(
                out=out, out_offset=None, in_=in_,
                in_offset=bass.IndirectOffsetOnAxis(ap=idx_ap, axis=0), element_offset=eoff)
        ds[1] += 16
        ins.then_inc(ds[0], 16)
        self.nins += 1
        self._record((ds[0], ds[1]), reads, writes)

    def barrier(self):
        deps = {self.sem[e]: self.cnt[e] for e in self.eng if self.cnt[e] > 0}
        for k, ds in self.dsem.items():
            if ds[1] > 0:
                deps[ds[0]] = ds[1]
        for e in self.eng:
            self._wait(e, deps)

    def final_wait(self, e, keys):
        deps = {}
        for k in keys:
            ds = self.dsem[k]
            deps[ds[0]] = ds[1]
        self._wait(e, deps)


def build(T, NPG, NPOOL, phases=4):
    NT = T // 128
    NTT = NT + 1
    nc = bass.Bass("TRN2", target_bir_lowering=False)
    tr = Tr(nc)

    def din(name, shape, dt=F32):
        return nc.dram_tensor(name, list(shape), dt, kind="ExternalInput").ap()

    def dout(name, shape, dt=F32):
        return nc.dram_tensor(name, list(shape), dt, kind="ExternalOutput").ap()

    x_p = din("x_p", [T, D]); x_s = din("x_s", [128, D])
    ckv = din("ckv", [NPOOL, PAGE, KVL]); ckr = din("ckr", [NPOOL, PAGE, QKR])
    st_in = din("st_in", [SB, NH, 128, 128]); ptT = din("ptT", [NPG, SB], I32)
    w_in = din("w_in", [D, 4 * D]); w_outa = din("w_outa", [D, D])
    glb = din("glb", [2, D])
    glbT = din("glbT", [128, 2, NH])
    gv = din("gv", [128, 5, 8])
    gq = din("gq", [128, 3])
    g_o = din("g_o", [128]); g_kv = din("g_kv", [KVL]); g_fin = din("g_fin", [D])
    w_dkv = din("w_dkv", [D, KVL + QKR]); w_ukv = din("w_ukv", [KVL, NH * 256])
    w_dq = din("w_dq", [D, QL]); w_uq = din("w_uq", [QL, NH * 192]); w_outb = din("w_outb", [D, D])
    w_fg = din("w_fg", [D, DFF]); w_fu = din("w_fu", [D, DFF]); w_fd = din("w_fd", [DFF, D])
    w_r = din("w_r", [D, NE])
    w_eg = din("w_eg", [NE, D, DFF]); w_eu = din("w_eu", [NE, D, DFF]); w_ed = din("w_ed", [NE, DFF, D])
    c_idf = din("c_idf", [128, 128]); c_idb = din("c_idb", [128, 128], BF16)
    c_tri = din("c_tri", [2, 128, 128]); c_up = din("c_up", [2, 128, 128])
    c_cb = din("c_cb", [128, 128]); c_cbs = din("c_cbs", [64, 8]); c_bm = din("c_bm", [128, SB])
    c_msk = din("c_msk", [SB, 64, 128])
    c_cos = din("c_cos", [NTT, 128, 32]); c_sin = din("c_sin", [NTT, 128, 32])

    y_p = dout("y_p", [T, D]); y_s = dout("y_s", [128, D])
    ckv_p = dout("ckv_p", [T, KVL]); kr_p = dout("kr_p", [T, QKR])
    ckv_s = dout("ckv_s", [128, KVL]); kr_s = dout("kr_s", [128, QKR])
    hg_p = dout("hg_p", [NH, 128, 128]); hg_s = dout("hg_s", [SB, NH, 128, 128])
    hA = nc.dram_tensor("hA", [NTT, 128, D], F32, kind="Internal").ap()
    hB = nc.dram_tensor("hB", [NTT, 128, D], F32, kind="Internal").ap()

    def xrows(i):
        return x_p[i * 128:(i + 1) * 128, :] if i < NT else x_s[:, :]

    ps = [nc.alloc_psum_tensor("ps%d" % i, [128, 512], F32) for i in range(8)]

    def psk(i):
        return ("ps", i)

    with ExitStack() as g:
        used_names = {}

        def sb(stack, name, shape, dt=F32):
            n = used_names.get(name, 0)
            used_names[name] = n + 1
            if n:
                name = "%s_v%d" % (name, n)
            return stack.enter_context(nc.sbuf_tensor(name, list(shape), dt))

        idf = sb(g, "idf", [128, 128]); idb = sb(g, "idb", [128, 128], BF16)
        tri = sb(g, "tri", [128, 2, 128]); up = sb(g, "up", [128, 2, 128])
        trib = sb(g, "trib", [128, 2, 128], BF16)
        cb = sb(g, "cb", [128, 128]); cbs = sb(g, "cbs", [64, 8]); bm = sb(g, "bm", [128, SB])
        gvs = sb(g, "gvs", [128, 5, 8]); gqs = sb(g, "gqs", [128, 3])
        glbTs = sb(g, "glbTs", [128, 2, NH]); omlT = sb(g, "omlT", [128, NH])
        lb_b = sb(g, "lb_b", [128, D]); oml_b = sb(g, "oml_b", [128, D])
        go_b = sb(g, "go_b", [128, 128]); gkv_b = sb(g, "gkv_b", [128, KVL]); gfin_b = sb(g, "gfin_b", [128, D])
        ctmp = sb(g, "ctmp", [128, D])

        def ld(dst, src, key, eng='sp'):
            tr.dma(eng, dst, src, [], [key], key)
        ld(idf[:], c_idf[:, :], "idf"); ld(idb[:], c_idb[:, :], "idb")
        ld(tri[:], c_tri.rearrange("a s t -> s a t"), "tri"); ld(up[:], c_up.rearrange("a s t -> s a t"), "up")
        ld(cb[:], c_cb[:, :], "cb"); ld(cbs[:], c_cbs[:, :], "cbs"); ld(bm[:], c_bm[:, :], "bm")
        ld(gvs[:], gv[:, :, :], "gvs"); ld(gqs[:], gq[:, :], "gqs"); ld(glbTs[:], glbT[:, :, :], "glbTs")
        ld(lb_b[:], glb[0, :].partition_broadcast(128), "lb_b")
        ld(ctmp[:], glb[1, :].partition_broadcast(128), "ctmp")
        ld(go_b[:], g_o.partition_broadcast(128), "go_b")
        ld(gkv_b[:], g_kv.partition_broadcast(128), "gkv_b")
        ld(gfin_b[:], g_fin.partition_broadcast(128), "gfin_b")
        tr.op('dve', lambda e: e.tensor_copy(out=trib[:], in_=tri[:]), ["tri"], ["trib"])
        tr.op('dve', lambda e: e.tensor_tensor(out=ctmp[:], in0=ctmp[:], in1=lb_b[:], op=ALU.subtract), ["ctmp", "lb_b"], ["ctmp"])
        tr.op('act', lambda e: e.activation(out=ctmp[:], in_=ctmp[:], func=AF.Exp), ["ctmp"], ["ctmp"])
        tr.op('dve', lambda e: e.tensor_scalar(out=ctmp[:], in0=ctmp[:], scalar1=1.0, scalar2=None, op0=ALU.add), ["ctmp"], ["ctmp"])
        tr.op('dve', lambda e: e.reciprocal(out=lb_b[:], in_=ctmp[:]), ["ctmp"], ["lb_b"])
        tr.op('dve', lambda e: e.tensor_scalar(out=oml_b[:], in0=lb_b[:], scalar1=-1.0, scalar2=1.0, op0=ALU.mult, op1=ALU.add), ["lb_b"], ["oml_b"])
        tr.op('dve', lambda e: e.tensor_tensor(out=omlT[:], in0=glbTs[:, 1, :], in1=glbTs[:, 0, :], op=ALU.subtract), ["glbTs"], ["omlT"])
        tr.op('act', lambda e: e.activation(out=omlT[:], in_=omlT[:], func=AF.Exp), ["omlT"], ["omlT"])
        tr.op('dve', lambda e: e.tensor_scalar(out=omlT[:], in0=omlT[:], scalar1=1.0, scalar2=None, op0=ALU.add), ["omlT"], ["omlT"])
        tr.op('dve', lambda e: e.reciprocal(out=omlT[:], in_=omlT[:]), ["omlT"], ["omlT"])
        tr.op('dve', lambda e: e.tensor_scalar(out=omlT[:], in0=omlT[:], scalar1=-1.0, scalar2=1.0, op0=ALU.mult, op1=ALU.add), ["omlT"], ["omlT"])

        def rms_scale(stack_bufs, xt, xkey, width, jkey="junk"):
            junk, ss, rstd = stack_bufs
            tr.op('act', lambda e: e.activation(out=junk[:, 0:width], in_=xt, func=AF.Square, accum_out=ss[:, 0:1]),
                  [xkey], [jkey, "ss"])
            tr.op('dve', lambda e: e.tensor_scalar(out=rstd[:, 0:1], in0=ss[:, 0:1], scalar1=1.0 / width, scalar2=EPS,
                                                   op0=ALU.mult, op1=ALU.add), ["ss"], ["rstd"])
            tr.op('act', lambda e: e.activation(out=rstd[:, 0:1], in_=rstd[:, 0:1], func=AF.Ln), ["rstd"], ["rstd"])
            tr.op('act', lambda e: e.activation(out=rstd[:, 0:1], in_=rstd[:, 0:1], func=AF.Exp, scale=-0.5), ["rstd"], ["rstd"])

        def transpose_bf(dstT, dkey, src_bf, skey, nchunk, bank, gain=None, gkey=None, eng='dve'):
            pt = ps[bank][:, :].bitcast(BF16)
            for c in range(nchunk):
                tr.op('pe', lambda e, c=c: e.transpose(out=pt[:, c * 128:(c + 1) * 128], in_=src_bf[:, c * 128:(c + 1) * 128], identity=idb[:]),
                      [skey, "idb"], [psk(bank)], inc=(c == nchunk - 1))
            pv = pt[:, 0:nchunk * 128].rearrange("p (c t) -> p c t", c=nchunk)
            if gain is None:
                if eng == 'act':
                    tr.op('act', lambda e: e.activation(out=dstT, in_=pv, func=AF.Copy), [psk(bank)], [dkey])
                else:
                    tr.op('dve', lambda e: e.tensor_copy(out=dstT, in_=pv), [psk(bank)], [dkey])
            else:
                tr.op('dve', lambda e: e.tensor_tensor(out=dstT, in0=pv, in1=gain.unsqueeze(2).broadcast_to([128, nchunk, 128]), op=ALU.mult),
                      [psk(bank), gkey], [dkey])

        def sigmoid_from_exp(buf, key, eng='dve'):
            tr.op(eng, lambda e: e.tensor_scalar(out=buf, in0=buf, scalar1=1.0, scalar2=None, op0=ALU.add), [key], [key])
            tr.op('dve', lambda e: e.reciprocal(out=buf, in_=buf), [key], [key])

        with ExitStack() as p1:
            w_in_sb = sb(p1, "w_in_sb", [128, 8, 4 * D], BF16)
            w_out_sb = sb(p1, "w_out_sb", [128, 8, D], BF16)
            for dc in range(8):
                for hh in range(2):
                    tr.dma('pool', w_in_sb[:, dc, hh * 2048:(hh + 1) * 2048], w_in[dc * 128:(dc + 1) * 128, hh * 2048:(hh + 1) * 2048],
                           [], ["w_in_sb"], "w_in_sb")
                tr.dma('pool', w_out_sb[:, dc, :], w_outa[dc * 128:(dc + 1) * 128, :], [], ["w_out_sb"], "w_out_sb")
            xt = [sb(p1, "xt%d" % i, [128, D]) for i in range(2)]
            ss = sb(p1, "ss", [128, 1]); rstd = sb(p1, "rstd", [128, 1])
            xs = sb(p1, "xs", [128, D], BF16); xnT = sb(p1, "xnT", [128, 8, 128], BF16)
            tA = sb(p1, "tA", [128, D]); tB = sb(p1, "tB", [128, D]); tC = sb(p1, "tC", [128, D])
            logf = sb(p1, "logf", [128, D]); ktok = sb(p1, "ktok", [128, D])
            kd = sb(p1, "kd", [128, D], BF16); vtok = sb(p1, "vtok", [128, D], BF16)
            sgate = sb(p1, "sgate", [128, D])
            sq = sb(p1, "sq", [128, NH, 128]); kT = sb(p1, "kT", [128, NH, 128])
            qeT = sb(p1, "qeT", [128, NH, 128], BF16); keT = sb(p1, "keT", [128, NH, 128], BF16)
            qem = sb(p1, "qem", [128, NH, 2, 128], BF16)
            qems = sb(p1, "qems", [128, SB, 128], BF16)
            ebl = sb(p1, "ebl", [128, NH, SB])
            scm = sb(p1, "scm", [128, NH, 128], BF16)
            S32 = sb(p1, "S32", [128, NH, 128]); S1_32 = sb(p1, "S1_32", [128, NH, 128])
            Sb = sb(p1, "Sb", [128, NH, 128], BF16); S1b = sb(p1, "S1b", [128, NH, 128], BF16)
            rs8 = sb(p1, "rs8", [128, NH]); onb = sb(p1, "onb", [128, D], BF16); onT = sb(p1, "onT", [128, 8, 128], BF16)
            s0f = sb(p1, "s0f", [128, SB, 128]); s0b = sb(p1, "s0b", [128, SB, 128], BF16)
            kdm = sb(p1, "kdm", [128, SB, 128], BF16); snw = s0f

            tr.op('pool', lambda e: e.memset(qem[:], 0.0), [], ["qem"])
            tr.op('pool', lambda e: e.memset(qems[:], 0.0), [], ["qems"])
            tr.op('pool', lambda e: e.memset(S32[:], 0.0), [], ["S32"])
            tr.op('pool', lambda e: e.memset(Sb[:], 0.0), [], ["Sb"])

            def proj_tok(col0, banks):
                for hb in range(2):
                    for dc in range(8):
                        tr.op('pe', lambda e, hb=hb, dc=dc: e.matmul(ps[banks[hb]][:, :], lhsT=xnT[:, dc, :],
                                                                      rhs=w_in_sb[:, dc, col0 + hb * 512: col0 + (hb + 1) * 512],
                                                                      start=(dc == 0), stop=(dc == 7)),
                              ["xnT", "w_in_sb"], [psk(banks[hb])], inc=(dc == 7))

            def proj_feat(col0, banks):
                for fc in range(8):
                    b = banks[fc // 4]
                    for dc in range(8):
                        tr.op('pe', lambda e, fc=fc, dc=dc, b=b: e.matmul(ps[b][:, (fc % 4) * 128:(fc % 4 + 1) * 128],
                                                                          lhsT=w_in_sb[:, dc, col0 + fc * 128: col0 + (fc + 1) * 128],
                                                                          rhs=xnT[:, dc, :], start=(dc == 0), stop=(dc == 7)),
                              ["xnT", "w_in_sb"], [psk(b)], inc=(dc == 7))

            def ps2(banks):
                return [(ps[banks[0]][:, :], slice(0, 512), psk(banks[0])), (ps[banks[1]][:, :], slice(512, 1024), psk(banks[1]))]

            for ti in range(NTT):
                samp = (ti == NT)
                cm = 1 if samp else 0
                x_t = xt[ti % 2]; xk = "xt%d" % (ti % 2)
                tr.dma('sp', x_t[:], xrows(ti), [], [xk], xk)
                rms_scale((tB, ss, rstd), x_t[:], xk, D, jkey="tB")
                tr.op('act', lambda e: e.activation(out=xs[:], in_=x_t[:], func=AF.Copy, scale=rstd[:, 0:1]), [xk, "rstd"], ["xs"])
                transpose_bf(xnT[:], "xnT", xs, "xs", 8, 4, gain=gvs[:, 0, :], gkey="gvs")
                proj_feat(0, (0, 1))
                sqf = sq[:].rearrange("p h t -> p (h t)")
                for pa, sl, pk in ps2((0, 1)):
                    tr.op('act', lambda e, pa=pa, sl=sl: e.activation(out=sqf[:, sl], in_=pa, func=AF.Exp, scale=-1.0), [pk], ["sq"])
                sigmoid_from_exp(sqf, "sq")
                for pa, sl, pk in ps2((0, 1)):
                    tr.op('dve', lambda e, pa=pa, sl=sl: e.tensor_tensor(out=sqf[:, sl], in0=sqf[:, sl], in1=pa, op=ALU.mult), [pk, "sq"], ["sq"])
                proj_feat(D, (2, 3))
                kTf = kT[:].rearrange("p h t -> p (h t)")
                for pa, sl, pk in ps2((2, 3)):
                    tr.op('act', lambda e, pa=pa, sl=sl: e.activation(out=kTf[:, sl], in_=pa, func=AF.Exp), [pk], ["kT"])
                sigmoid_from_exp(kTf, "kT")
                tr.op('dve', lambda e: e.tensor_tensor(out=kT[:], in0=kT[:], in1=omlT[:].unsqueeze(2).broadcast_to([128, NH, 128]), op=ALU.mult),
                      ["kT", "omlT"], ["kT"])
                proj_tok(D, (0, 1))
                for pa, sl, pk in ps2((0, 1)):
                    tr.op('act', lambda e, pa=pa, sl=sl: e.activation(out=tA[:, sl], in_=pa, func=AF.Exp, scale=-1.0), [pk], ["tA"])
                sigmoid_from_exp(tA[:], "tA")
                tr.op('dve', lambda e: e.tensor_tensor(out=tA[:], in0=tA[:], in1=oml_b[:], op=ALU.mult), ["tA", "oml_b"], ["tA"])
                tr.op('dve', lambda e: e.tensor_tensor(out=tA[:], in0=tA[:], in1=lb_b[:], op=ALU.add), ["tA", "lb_b"], ["tA"])
                tr.op('act', lambda e: e.activation(out=logf[:], in_=tA[:], func=AF.Ln), ["tA"], ["logf"])
                tr.op('pool', lambda e: e.tensor_scalar(out=ktok[:], in0=tA[:], scalar1=-1.0, scalar2=1.0, op0=ALU.mult, op1=ALU.add), ["tA"], ["ktok"])
                for h in range(NH):
                    b = 2 + h // 4
                    tr.op('pe', lambda e, h=h, b=b: e.matmul(ps[b][:, (h % 4) * 128:(h % 4 + 1) * 128], lhsT=logf[:, h * 128:(h + 1) * 128],
                                                             rhs=tri[:, cm, :], start=True, stop=True),
                          ["logf", "tri"], [psk(b)], inc=(h % 4 == 3))
                for hb in range(2):
                    tr.op('pe', lambda e, hb=hb: e.matmul(ps[hb][:, :], lhsT=up[:, cm, :], rhs=logf[:, hb * 512:(hb + 1) * 512], start=True, stop=True),
                          ["logf", "up"], [psk(hb)])
                tBf = tB[:]; tCf = tC[:]
                for pa, sl, pk in ps2((2, 3)):
                    tr.op('act', lambda e, pa=pa, sl=sl: e.activation(out=tBf[:, sl], in_=pa, func=AF.Exp), [pk], ["tB"])
                    tr.op('act', lambda e, pa=pa, sl=sl: e.activation(out=tCf[:, sl], in_=pa, func=AF.Exp, scale=-1.0), [pk], ["tC"])
                tr.op('dve', lambda e: e.tensor_tensor(out=qeT[:].rearrange("p h t -> p (h t)"), in0=sqf, in1=tBf, op=ALU.mult), ["sq", "tB"], ["qeT"])
                tr.op('dve', lambda e: e.tensor_tensor(out=keT[:].rearrange("p h t -> p (h t)"), in0=kTf, in1=tCf, op=ALU.mult), ["kT", "tC"], ["keT"])
                nch = SB if samp else 2
                cl = 128 // nch
                tB3 = tB[:].rearrange("p (h c l) -> p h c l", h=NH, c=nch)
                tr.op('pool', lambda e: e.tensor_copy(out=ebl[:, :, 0:nch], in_=tB3[:, :, :, cl - 1]), ["tB"], ["ebl"])
                for pa, sl, pk in ps2((0, 1)):
                    tr.op('act', lambda e, pa=pa, sl=sl: e.activation(out=tA[:, sl], in_=pa, func=AF.Exp), [pk], ["tA"])
                tr.op('dve', lambda e: e.tensor_tensor(out=kd[:], in0=ktok[:], in1=tA[:], op=ALU.mult), ["ktok", "tA"], ["kd"])
                proj_tok(2 * D, (2, 3))
                for pa, sl, pk in ps2((2, 3)):
                    tr.op('act', lambda e, pa=pa, sl=sl: e.activation(out=vtok[:, sl], in_=pa, func=AF.Copy), [pk], ["vtok"])
                proj_tok(3 * D, (0, 1))
                for pa, sl, pk in ps2((0, 1)):
                    tr.op('act', lambda e, pa=pa, sl=sl: e.activation(out=sgate[:, sl], in_=pa, func=AF.Exp, scale=-1.0), [pk], ["sgate"])
                sigmoid_from_exp(sgate[:], "sgate")
                for pa, sl, pk in ps2((0, 1)):
                    tr.op('dve', lambda e, pa=pa, sl=sl: e.tensor_tensor(out=sgate[:, sl], in0=sgate[:, sl], in1=pa, op=ALU.mult), [pk, "sgate"], ["sgate"])
                for h in range(NH):
                    b = 4 + h // 4
                    tr.op('pe', lambda e, h=h, b=b: e.matmul(ps[b][:, (h % 4) * 128:(h % 4 + 1) * 128], lhsT=keT[:, h, :], rhs=qeT[:, h, :],
                                                             start=True, stop=True), ["keT", "qeT"], [psk(b)], inc=(h % 4 == 3))
                for hb in range(2):
                    tr.op('dve', lambda e, hb=hb: e.tensor_tensor(out=scm[:, hb * 4:(hb + 1) * 4, :],
                                                                  in0=ps[4 + hb][:, :].rearrange("p (h t) -> p h t", h=4),
                                                                  in1=tri[:, cm, :].unsqueeze(1).broadcast_to([128, 4, 128]), op=ALU.mult),
                          [psk(4 + hb), "tri"], ["scm"])
                if not samp:
                    tr.op('pool', lambda e: e.tensor_copy(out=qem[:, :, 0, 0:64], in_=qeT[:, :, 0:64]), ["qeT"], ["qem"])
                    tr.op('pool', lambda e: e.tensor_copy(out=qem[:, :, 1, 64:128], in_=qeT[:, :, 64:128]), ["qeT"], ["qem"])
                    for h in range(NH):
                        b = 6 + h // 4
                        tr.op('pe', lambda e, h=h, b=b: e.matmul(ps[b][:, (h % 4) * 128:(h % 4 + 1) * 128], lhsT=kd[0:64, h * 128:(h + 1) * 128],
                                                                 rhs=vtok[0:64, h * 128:(h + 1) * 128], start=True, stop=True),
                              ["kd", "vtok"], [psk(b)], inc=(h % 4 == 3))
                    tr.op('dve', lambda e: e.tensor_tensor(out=S1_32[:], in0=S32[:], in1=ebl[:, :, 0:1].broadcast_to([128, NH, 128]), op=ALU.mult),
                          ["S32", "ebl"], ["S1_32"])
                    for hb in range(2):
                        tr.op('dve', lambda e, hb=hb: e.tensor_tensor(out=S1_32[:, hb * 4:(hb + 1) * 4, :], in0=S1_32[:, hb * 4:(hb + 1) * 4, :],
                                                                      in1=ps[6 + hb][:, :].rearrange("p (h v) -> p h v", h=4), op=ALU.add),
                              [psk(6 + hb), "S1_32"], ["S1_32"])
                    tr.op('pool', lambda e: e.tensor_copy(out=S1b[:], in_=S1_32[:]), ["S1_32"], ["S1b"])
                    for h in range(NH):
                        b = 2 + h // 4
                        osl = ps[b][:, (h % 4) * 128:(h % 4 + 1) * 128]
                        tr.op('pe', lambda e, h=h, osl=osl: e.matmul(osl, lhsT=qem[:, h, 0, :], rhs=Sb[:, h, :], start=True, stop=False),
                              ["qem", "Sb"], [psk(b)], inc=False)
                        tr.op('pe', lambda e, h=h, osl=osl: e.matmul(osl, lhsT=qem[:, h, 1, :], rhs=S1b[:, h, :], start=False, stop=False),
                              ["qem", "S1b"], [psk(b)], inc=False)
                        tr.op('pe', lambda e, h=h, osl=osl: e.matmul(osl, lhsT=scm[:, h, :], rhs=vtok[:, h * 128:(h + 1) * 128], start=False, stop=True),
                              ["scm", "vtok"], [psk(b)], inc=(h % 4 == 3))
                    for h in range(NH):
                        b = 6 + h // 4
                        tr.op('pe', lambda e, h=h, b=b: e.matmul(ps[b][:, (h % 4) * 128:(h % 4 + 1) * 128], lhsT=kd[64:128, h * 128:(h + 1) * 128],
                                                                 rhs=vtok[64:128, h * 128:(h + 1) * 128], start=True, stop=True),
                              ["kd", "vtok"], [psk(b)], inc=(h % 4 == 3))
                    tr.op('dve', lambda e: e.tensor_tensor(out=S32[:], in0=S1_32[:], in1=ebl[:, :, 1:2].broadcast_to([128, NH, 128]), op=ALU.mult),
                          ["S1_32", "ebl"], ["S32"])
                    for hb in range(2):
                        tr.op('dve', lambda e, hb=hb: e.tensor_tensor(out=S32[:, hb * 4:(hb + 1) * 4, :], in0=S32[:, hb * 4:(hb + 1) * 4, :],
                                                                      in1=ps[6 + hb][:, :].rearrange("p (h v) -> p h v", h=4), op=ALU.add),
                              [psk(6 + hb), "S32"], ["S32"])
                    tr.op('pool', lambda e: e.tensor_copy(out=Sb[:], in_=S32[:]), ["S32"], ["Sb"])
                    if ti == NT - 1:
                        tr.dma('sp', hg_p.rearrange("h k v -> k h v"), S32[:], ["S32"], ["hg_p"], "hg_p")
                else:
                    for h in range(NH):
                        tr.dma('sp', s0f[:], st_in[:, h, :, :].rearrange("b k v -> k b v"), [], ["s0f"], "s0f")
                        tr.op('act', lambda e: e.activation(out=s0b[:], in_=s0f[:], func=AF.Copy), ["s0f"], ["s0b"])
                        qv = qems[:].rearrange("p b (c l) -> p b c l", c=SB)
                        for b_ in range(SB):
                            tr.op('pool', lambda e, b_=b_, h=h: e.tensor_copy(out=qems[:, b_, b_ * ST:(b_ + 1) * ST], in_=qeT[:, h, b_ * ST:(b_ + 1) * ST]),
                                  ["qeT"], ["qems"])
                        ob = 4 + h // 4
                        osl = ps[ob][:, (h % 4) * 128:(h % 4 + 1) * 128]
                        for b_ in range(SB):
                            tr.op('pe', lambda e, b_=b_, osl=osl: e.matmul(osl, lhsT=qems[:, b_, :], rhs=s0b[:, b_, :], start=(b_ == 0), stop=False),
                                  ["qems", "s0b"], [psk(ob)], inc=False)
                        tr.op('pe', lambda e, h=h, osl=osl: e.matmul(osl, lhsT=scm[:, h, :], rhs=vtok[:, h * 128:(h + 1) * 128], start=False, stop=True),
                              ["scm", "vtok"], [psk(ob)])
                        tr.op('dve', lambda e, h=h: e.tensor_tensor(out=kdm[:], in0=kd[:, h * 128:(h + 1) * 128].unsqueeze(1).broadcast_to([128, SB, 128]),
                                                                    in1=bm[:].unsqueeze(2).broadcast_to([128, SB, 128]), op=ALU.mult),
                              ["kd", "bm"], ["kdm"])
                        for b_ in range(SB):
                            bk = b_ // 4
                            tr.op('pe', lambda e, b_=b_, bk=bk, h=h: e.matmul(ps[bk][:, (b_ % 4) * 128:(b_ % 4 + 1) * 128], lhsT=kdm[:, b_, :],
                                                                             rhs=vtok[:, h * 128:(h + 1) * 128], start=True, stop=True),
                                  ["kdm", "vtok"], [psk(bk)], inc=(b_ % 4 == 3))
                        tr.op('dve', lambda e, h=h: e.tensor_tensor(out=snw[:], in0=s0f[:], in1=ebl[:, h, :].unsqueeze(2).broadcast_to([128, SB, 128]), op=ALU.mult),
                              ["s0f", "ebl"], ["s0f"])
                        for bk in range(4):
                            tr.op('dve', lambda e, bk=bk: e.tensor_tensor(out=snw[:, bk * 4:(bk + 1) * 4, :], in0=snw[:, bk * 4:(bk + 1) * 4, :],
                                                                          in1=ps[bk][:, :].rearrange("p (b v) -> p b v", b=4), op=ALU.add),
                                  [psk(bk), "s0f"], ["s0f"])
                        tr.dma('sp', hg_s[:, h, :, :].rearrange("b k v -> k b v"), snw[:], ["s0f"], ["hg_s"], "s0f")
                obanks = (4, 5) if samp else (2, 3)
                for pa, sl, pk in ps2(obanks):
                    tr.op('act', lambda e, pa=pa, sl=sl: e.activation(out=tB[:, sl], in_=pa, func=AF.Square), [pk], ["tB"])
                tr.op('dve', lambda e: e.tensor_reduce(out=rs8[:], in_=tB[:].rearrange("p (h v) -> p h v", h=NH), axis=AX.X, op=ALU.add), ["tB"], ["rs8"])
                tr.op('dve', lambda e: e.tensor_scalar(out=rs8[:], in0=rs8[:], scalar1=1.0 / 128, scalar2=EPS, op0=ALU.mult, op1=ALU.add), ["rs8"], ["rs8"])
                tr.op('act', lambda e: e.activation(out=rs8[:], in_=rs8[:], func=AF.Ln), ["rs8"], ["rs8"])
                tr.op('act', lambda e: e.activation(out=rs8[:], in_=rs8[:], func=AF.Exp, scale=-0.5), ["rs8"], ["rs8"])
                for hb, (pa, sl, pk) in enumerate(ps2(obanks)):
                    tr.op('dve', lambda e, pa=pa, sl=sl, hb=hb: e.tensor_tensor(out=tC[:, sl].rearrange("p (h v) -> p h v", h=4),
                                                                               in0=pa.rearrange("p (h v) -> p h v", h=4),
                                                                               in1=rs8[:, hb * 4:(hb + 1) * 4].unsqueeze(2).broadcast_to([128, 4, 128]), op=ALU.mult),
                          [pk, "rs8"], ["tC"])
                tr.op('pool', lambda e: e.tensor_tensor(out=tC[:].rearrange("p (h v) -> p h v", h=NH), in0=tC[:].rearrange("p (h v) -> p h v", h=NH),
                                                        in1=go_b[:].unsqueeze(1).broadcast_to([128, NH, 128]), op=ALU.mult), ["tC", "go_b"], ["tC"])
                tr.op('dve', lambda e: e.tensor_tensor(out=onb[:], in0=tC[:], in1=sgate[:], op=ALU.mult), ["tC", "sgate"], ["onb"])
                transpose_bf(onT[:], "onT", onb, "onb", 8, 6)
                for hb in range(2):
                    for c in range(8):
                        tr.op('pe', lambda e, hb=hb, c=c: e.matmul(ps[hb][:, :], lhsT=onT[:, c, :], rhs=w_out_sb[:, c, hb * 512:(hb + 1) * 512],
                                                                   start=(c == 0), stop=(c == 7)), ["onT", "w_out_sb"], [psk(hb)], inc=(c == 7))
                for pa, sl, pk in ps2((0, 1)):
                    tr.op('dve', lambda e, pa=pa, sl=sl: e.tensor_tensor(out=x_t[:, sl], in0=x_t[:, sl], in1=pa, op=ALU.add), [pk, xk], [xk])
                tr.dma('sp', hA[ti], x_t[:], [xk], [("hA", ti)], xk)

        def emit_debug(hsrc, keys):
            with ExitStack() as pd:
                t_ = sb(pd, "dbg", [128, D])
                for ti in range(NTT):
                    tr.dma('sp', t_[:], hsrc[ti], [(hsrc.tensor.name, ti)], ["dbg"], "dbg")
                    dst = y_p[ti * 128:(ti + 1) * 128, :] if ti < NT else y_s[:, :]
                    tr.dma('sp', dst, t_[:], ["dbg"], ["yout"], "dbgo")
            tr.final_wait('sp', ["dbgo"] + keys)

        if phases == 1:
            emit_debug(hA, ["hg_p", "s0f"])
            return nc, tr

        groups = [list(range(g0, min(g0 + 4, NT))) for g0 in range(0, NT, 4)] + [[NT]]

        def ffn_phase(hin, hout, gidx, experts, moe):
            tr.barrier()
            with ExitStack() as pf:
                hres = sb(pf, "hres", [128, 4, D]); xnTg = sb(pf, "xnTg", [128, 8, 512], BF16)
                xsb = sb(pf, "xsb", [128, D], BF16); ssf = sb(pf, "ssf", [128, 1]); rstf = sb(pf, "rstf", [128, 1])
                jk = sb(pf, "jk", [128, D])
                wgs = [sb(pf, "wgs%d" % i, [128, 8, 512], BF16) for i in range(2)]
                wus = [sb(pf, "wus%d" % i, [128, 8, 512], BF16) for i in range(2)]
                wds = [sb(pf, "wds%d" % i, [128, NFC, 512], BF16) for i in range(2)]
                hT = sb(pf, "hT", [128, NFC, 512], BF16)
                tm = [sb(pf, "tm%d" % i, [128, 512]) for i in range(2)]
                if moe:
                    xnT32 = sb(pf, "xnT32", [128, 8, 128]); wrs = sb(pf, "wrs", [128, 8, NE])
                    lg = sb(pf, "lg", [128, NE]); l2 = sb(pf, "l2", [128, NE]); eq1 = sb(pf, "eq1", [128, NE]); eq2 = sb(pf, "eq2", [128, NE])
                    m1 = sb(pf, "m1", [128, 1]); m2 = sb(pf, "m2", [128, 1]); g1 = sb(pf, "g1", [128, 1]); g2 = sb(pf, "g2", [128, 1])
                    comb = sb(pf, "comb", [128, 4, NE]); yst = sb(pf, "yst", [128, D])
                    tr.dma('sp', wrs[:], w_r.rearrange("(c p) e -> p c e", p=128), [], ["wrs"], "wrs")
                slab_n = 0
                wd_n = 0
                for grp in groups:
                    nt_ = len(grp); GT = nt_ * 128
                    for li, ti in enumerate(grp):
                        hk_ = ("hres", li)
                        tr.dma('sp', hres[:, li, :], hin[ti], [(hin.tensor.name, ti)], [hk_], "hres%d" % li)
                        rms_scale((jk, ssf, rstf), hres[:, li, :], hk_, D, jkey="jk")
                        tr.op('act', lambda e, li=li: e.activation(out=xsb[:], in_=hres[:, li, :], func=AF.Copy, scale=rstf[:, 0:1]), [hk_, "rstd"], ["xsb"])
                        transpose_bf(xnTg[:, :, li * 128:(li + 1) * 128], "xnTg", xsb, "xsb", 8, 6, gain=gvs[:, gidx, :], gkey="gvs")
                        if moe:
                            tr.op('act', lambda e, li=li: e.activation(out=jk[:], in_=hres[:, li, :], func=AF.Copy, scale=rstf[:, 0:1]), [hk_, "rstd"], ["jk"])
                            for c in range(8):
                                b = 6 + c // 4
                                tr.op('pe', lambda e, c=c, b=b: e.transpose(out=ps[b][:, (c % 4) * 128:(c % 4 + 1) * 128], in_=jk[:, c * 128:(c + 1) * 128], identity=idf[:]),
                                      ["jk", "idf"], [psk(b)], inc=(c % 4 == 3))
                            for hb in range(2):
                                tr.op('dve', lambda e, hb=hb: e.tensor_tensor(out=xnT32[:, hb * 4:(hb + 1) * 4, :], in0=ps[6 + hb][:, :].rearrange("p (c t) -> p c t", c=4),
                                                                              in1=gvs[:, gidx, hb * 4:(hb + 1) * 4].unsqueeze(2).broadcast_to([128, 4, 128]), op=ALU.mult),
                                      [psk(6 + hb), "gvs"], ["xnT32"])
                            for c in range(8):
                                tr.op('pe', lambda e, c=c: e.matmul(ps[6][:, 0:NE], lhsT=xnT32[:, c, :], rhs=wrs[:, c, :], start=(c == 0), stop=(c == 7)),
                                      ["xnT32", "wrs"], [psk(6)], inc=(c == 7))
                            tr.op('dve', lambda e: e.tensor_copy(out=lg[:], in_=ps[6][:, 0:NE]), [psk(6)], ["lg"])
                            tr.op('dve', lambda e: e.tensor_reduce(out=m1[:], in_=lg[:], axis=AX.X, op=ALU.max), ["lg"], ["m1"])
                            tr.op('dve', lambda e: e.tensor_scalar(out=eq1[:], in0=lg[:], scalar1=m1[:, 0:1], scalar2=None, op0=ALU.is_equal), ["lg", "m1"], ["eq1"])
                            tr.op('dve', lambda e: e.scalar_tensor_tensor(out=l2[:], in0=eq1[:], scalar=NEG, in1=lg[:], op0=ALU.mult, op1=ALU.add), ["eq1", "lg"], ["l2"])
                            tr.op('dve', lambda e: e.tensor_reduce(out=m2[:], in_=l2[:], axis=AX.X, op=ALU.max), ["l2"], ["m2"])
                            tr.op('dve', lambda e: e.tensor_scalar(out=eq2[:], in0=l2[:], scalar1=m2[:, 0:1], scalar2=None, op0=ALU.is_equal), ["l2", "m2"], ["eq2"])
                            tr.op('dve', lambda e: e.tensor_tensor(out=g2[:], in0=m2[:], in1=m1[:], op=ALU.subtract), ["m1", "m2"], ["g2"])
                            tr.op('act', lambda e: e.activation(out=g2[:], in_=g2[:], func=AF.Exp), ["g2"], ["g2"])
                            tr.op('dve', lambda e: e.tensor_scalar(out=g1[:], in0=g2[:], scalar1=1.0, scalar2=None, op0=ALU.add), ["g2"], ["g1"])
                            tr.op('dve', lambda e: e.reciprocal(out=g1[:], in_=g1[:]), ["g1"], ["g1"])
                            tr.op('dve', lambda e: e.tensor_tensor(out=g2[:], in0=g2[:], in1=g1[:], op=ALU.mult), ["g1", "g2"], ["g2"])
                            tr.op('dve', lambda e, li=li: e.tensor_scalar(out=comb[:, li, :], in0=eq1[:], scalar1=g1[:, 0:1], scalar2=None, op0=ALU.mult), ["eq1", "g1"], ["comb"])
                            tr.op('dve', lambda e, li=li: e.scalar_tensor_tensor(out=comb[:, li, :], in0=eq2[:], scalar=g2[:, 0:1], in1=comb[:, li, :], op0=ALU.mult, op1=ALU.add),
                                  ["eq2", "g2", "comb"], ["comb"])
                    for ex in range(experts):
                        wg_, wu_, wd_ = (w_eg[ex], w_eu[ex], w_ed[ex]) if moe else (w_fg, w_fu, w_fd)
                        for jb in range(6):
                            ncol = 512 if jb < 5 else 256
                            sl_ = slab_n % 2; slab_n += 1
                            for dc in range(8):
                                tr.dma('pool', wgs[sl_][:, dc, 0:ncol], wg_[dc * 128:(dc + 1) * 128, jb * 512: jb * 512 + ncol], [], ["wgs%d" % sl_], "wgs%d" % sl_)
                                tr.dma('pool', wus[sl_][:, dc, 0:ncol], wu_[dc * 128:(dc + 1) * 128, jb * 512: jb * 512 + ncol], [], ["wus%d" % sl_], "wus%d" % sl_)
                            for jj in range(ncol // 128):
                                j = jb * 4 + jj
                                ba, bb = (0, 1) if j % 2 == 0 else (2, 3)
                                for dc in range(8):
                                    tr.op('pe', lambda e, dc=dc, jj=jj, ba=ba: e.matmul(ps[ba][:, 0:GT], lhsT=wgs[sl_][:, dc, jj * 128:(jj + 1) * 128], rhs=xnTg[:, dc, 0:GT],
                                                                                      start=(dc == 0), stop=(dc == 7)), ["wgs%d" % sl_, "xnTg"], [psk(ba)], inc=(dc == 7))
                                for dc in range(8):
                                    tr.op('pe', lambda e, dc=dc, jj=jj, bb=bb: e.matmul(ps[bb][:, 0:GT], lhsT=wus[sl_][:, dc, jj * 128:(jj + 1) * 128], rhs=xnTg[:, dc, 0:GT],
                                                                                      start=(dc == 0), stop=(dc == 7)), ["wus%d" % sl_, "xnTg"], [psk(bb)], inc=(dc == 7))
                                t_ = tm[j % 2]; tk = "tm%d" % (j % 2)
                                tr.op('act', lambda e, t_=t_, ba=ba: e.activation(out=t_[:, 0:GT], in_=ps[ba][:, 0:GT], func=AF.Exp, scale=-1.0), [psk(ba)], [tk])
                                tr.op('pool', lambda e, t_=t_: e.tensor_scalar(out=t_[:, 0:GT], in0=t_[:, 0:GT], scalar1=1.0, scalar2=None, op0=ALU.add), [tk], [tk])
                                tr.op('dve', lambda e, t_=t_: e.reciprocal(out=t_[:, 0:GT], in_=t_[:, 0:GT]), [tk], [tk])
                                tr.op('dve', lambda e, t_=t_, ba=ba: e.tensor_tensor(out=t_[:, 0:GT], in0=t_[:, 0:GT], in1=ps[ba][:, 0:GT], op=ALU.mult), [tk, psk(ba)], [tk])
                                tr.op('dve', lambda e, t_=t_, bb=bb, j=j: e.tensor_tensor(out=hT[:, j, 0:GT], in0=t_[:, 0:GT], in1=ps[bb][:, 0:GT], op=ALU.mult), [tk, psk(bb)], [("hT", j)])
                        for half in range(2):
                            ws_ = wd_n % 2; wd_n += 1
                            for j0 in range(0, NFC, 2):
                                tr.dma('pool', wds[ws_][:, j0:j0 + 2, :], wd_[j0 * 128:(j0 + 2) * 128, half * 512:(half + 1) * 512].rearrange("(j p) d -> p j d", p=128),
                                       [], ["wds%d" % ws_], "wds%d" % ws_)
                            for li in range(nt_):
                                bk = 4 + (li % 2)
                                for j in range(NFC):
                                    tr.op('pe', lambda e, j=j, li=li, bk=bk: e.matmul(ps[bk][:, :], lhsT=hT[:, j, li * 128:(li + 1) * 128], rhs=wds[ws_][:, j, :],
                                                                                    start=(j == 0), stop=(j == NFC - 1)), [("hT", j), "wds%d" % ws_], [psk(bk)], inc=(j == NFC - 1))
                                hsl = hres[:, li, half * 512:(half + 1) * 512]
                                if moe:
                                    tr.op('dve', lambda e, hsl=hsl, bk=bk, li=li, ex=ex: e.scalar_tensor_tensor(out=hsl, in0=ps[bk][:, :], scalar=comb[:, li, ex:ex + 1], in1=hsl,
                                                                                                           op0=ALU.mult, op1=ALU.add), [psk(bk), "comb", ("hres", li)], [("hres", li)])
                                else:
                                    tr.op('dve', lambda e, hsl=hsl, bk=bk: e.tensor_tensor(out=hsl, in0=hsl, in1=ps[bk][:, :], op=ALU.add), [psk(bk), ("hres", li)], [("hres", li)])
                    for li, ti in enumerate(grp):
                        hk_ = ("hres", li)
                        if not moe:
                            tr.dma('sp', hout[ti], hres[:, li, :], [hk_], [(hout.tensor.name, ti)], "hres%d" % li)
                        else:
                            rms_scale((jk, ssf, rstf), hres[:, li, :], hk_, D, jkey="jk")
                            tr.op('dve', lambda e, li=li: e.scalar_tensor_tensor(out=yst[:], in0=hres[:, li, :], scalar=rstf[:, 0:1], in1=gfin_b[:], op0=ALU.mult, op1=ALU.mult),
                                  [hk_, "rstd", "gfin_b"], ["yst"])
                            dst = y_p[ti * 128:(ti + 1) * 128, :] if ti < NT else y_s[:, :]
                            tr.dma('sp', dst, yst[:], ["yst"], ["yout"], "yst")

        ffn_phase(hA, hB, 1, 1, False)
        if phases == 2:
            emit_debug(hB, ["hg_p", "s0f"])
            return nc, tr
        if phases == 24:
            ffn_phase(hB, None, 4, NE, True)
            tr.final_wait('sp', ["hg_p", "s0f", "yst"])
            return nc, tr

        tr.barrier()
        with ExitStack() as p3:
            wdkv_sb = sb(p3, "wdkv_sb", [128, 8, 320], BF16); wdq_sb = sb(p3, "wdq_sb", [128, 8, QL], BF16)
            wuq_sb = sb(p3, "wuq_sb", [128, 3, NH * 192], BF16); wukv_sb = sb(p3, "wukv_sb", [128, 2, NH * 256], BF16)
            woutb_sb = sb(p3, "woutb_sb", [128, 8, D], BF16); wukT = sb(p3, "wukT", [128, NH, 256], BF16)
            tr.dma('pool', wdkv_sb[:], w_dkv.rearrange("(c p) f -> p c f", p=128), [], ["wdkv_sb"], "wdkv_sb")
            tr.dma('pool', wdq_sb[:], w_dq.rearrange("(c p) f -> p c f", p=128), [], ["wdq_sb"], "wdq_sb")
            tr.dma('pool', wuq_sb[:], w_uq.rearrange("(c p) f -> p c f", p=128), [], ["wuq_sb"], "wuq_sb")
            for cc in range(2):
                tr.dma('pool', wukv_sb[:, cc, :], w_ukv[cc * 128:(cc + 1) * 128, :], [], ["wukv_sb"], "wukv_sb")
            for c in range(8):
                tr.dma('pool', woutb_sb[:, c, :], w_outb[c * 128:(c + 1) * 128, :], [], ["woutb_sb"], "woutb_sb")
            ptb = ps[6][:, :].bitcast(BF16)
            for h in range(NH):
                for cc in range(2):
                    tr.op('pe', lambda e, h=h, cc=cc: e.transpose(out=ptb[:, (h % 4) * 256 + cc * 128:(h % 4) * 256 + (cc + 1) * 128],
                                                                  in_=wukv_sb[:, cc, h * 256:h * 256 + 128], identity=idb[:]),
                          ["wukv_sb", "idb"], [psk(6)], inc=(cc == 1 and h % 4 == 3))
                if h % 4 == 3:
                    tr.op('dve', lambda e, h=h: e.tensor_copy(out=wukT[:, h - 3:h + 1, :], in_=ptb[:, :].rearrange("p (h c) -> p h c", h=4)), [psk(6)], ["wukT"])
            cT = sb(p3, "cT", [128, 2, T], BF16); ctok = sb(p3, "ctok", [128, NT, KVL], BF16); krT = sb(p3, "krT", [64, T], BF16)
            cTs = sb(p3, "cTs", [128, 2, 128], BF16); ctoks = sb(p3, "ctoks", [128, KVL], BF16); krTs = sb(p3, "krTs", [64, 128], BF16)
            ht = sb(p3, "ht", [128, D]); jk3 = sb(p3, "jk3", [128, D]); ss3 = sb(p3, "ss3", [128, 1]); rs3 = sb(p3, "rs3", [128, 1])
            xs3 = sb(p3, "xs3", [128, D], BF16); nkvT = sb(p3, "nkvT", [128, 8, 128], BF16); xnbT = sb(p3, "xnbT", [128, 8, 128], BF16)
            cf = sb(p3, "cf", [128, KVL]); cbf = sb(p3, "cbf", [128, KVL], BF16); krf = sb(p3, "krf", [128, QKR]); krb = sb(p3, "krb", [128, QKR], BF16)
            cosb = sb(p3, "cosb", [128, 32]); sinb = sb(p3, "sinb", [128, 32]); r1 = sb(p3, "r1", [128, NH, 32]); r2 = sb(p3, "r2", [128, NH, 32])
            cqb = sb(p3, "cqb", [128, QL], BF16); cqT = sb(p3, "cqT", [128, 3, 128], BF16)
            qnT = sb(p3, "qnT", [128, NH, 128], BF16); qaT = sb(p3, "qaT", [128, 2, NH, 128], BF16)
            qrf = sb(p3, "qrf", [128, NH, QKR]); qrb = sb(p3, "qrb", [128, NH, QKR], BF16); qrT = sb(p3, "qrT", [64, NH, 128], BF16)
            mst = sb(p3, "mst", [128, 1]); mnew = sb(p3, "mnew", [128, 1]); lst = sb(p3, "lst", [128, 1]); alp = sb(p3, "alp", [128, 1])
            nbias = sb(p3, "nbias", [128, 1]); rsum = sb(p3, "rsum", [128, 1]); mx = sb(p3, "mx", [128, 1])
            oacc = sb(p3, "oacc", [128, KVL]); pbf = sb(p3, "pbf", [128, 512], BF16); pT = sb(p3, "pT", [128, 4, 128], BF16)
            olat = sb(p3, "olat", [128, KVL], BF16); olatT = sb(p3, "olatT", [128, 2, NH, 128], BF16); oT = sb(p3, "oT", [128, NH, 128], BF16)
            qas = sb(p3, "qas", [128, 2, NH, ST], BF16); qrs = sb(p3, "qrs", [64, NH, ST], BF16)
            KC = 16
            gc = [sb(p3, "gc%d" % i, [128, KC, KVL], BF16) for i in range(2)]
            gr = [sb(p3, "gr%d" % i, [128, KC, QKR], BF16) for i in range(2)]
            gcT = sb(p3, "gcT", [128, 2, 512], BF16); grT = sb(p3, "grT", [64, 512], BF16)
            pti = sb(p3, "pti", [128, SB], I32); msk = sb(p3, "msk", [64, SB, 128])
            tr.dma('sp', pti[0:NPG, :], ptT[:, :], [], ["pti"], "pti")
            tr.dma('sp', msk[:], c_msk.rearrange("b r k -> r b k"), [], ["msk"], "msk")

            def attend_block(M, qk, N, mask, vts, first):
                sbk = attend_block.n % 2; attend_block.n += 1
                S = ps[sbk][0:M, 0:N]
                for i_, (l_, r_, ks_) in enumerate(qk):
                    tr.op('pe', lambda e, l_=l_, r_=r_, i_=i_: e.matmul(S, lhsT=l_, rhs=r_, start=(i_ == 0), stop=(i_ == len(qk) - 1)),
                          ks_, [psk(sbk)], inc=(i_ == len(qk) - 1))
                if mask is not None:
                    map_, c0, n_, mk = mask
                    tr.op('dve', lambda e: e.tensor_tensor(out=ps[sbk][0:M, c0:c0 + n_], in0=ps[sbk][0:M, c0:c0 + n_], in1=map_, op=ALU.add), [psk(sbk), mk], [psk(sbk)])
                tr.op('dve', lambda e: e.tensor_reduce(out=mx[0:M, :], in_=S, axis=AX.X, op=ALU.max), [psk(sbk)], ["mx"])
                if first:
                    tr.op('dve', lambda e: e.tensor_copy(out=mst[0:M, :], in_=mx[0:M, :]), ["mx"], ["mst"])
                else:
                    tr.op('dve', lambda e: e.tensor_tensor(out=mnew[0:M, :], in0=mst[0:M, :], in1=mx[0:M, :], op=ALU.max), ["mx", "mst"], ["mnew"])
                    tr.op('dve', lambda e: e.tensor_tensor(out=alp[0:M, :], in0=mst[0:M, :], in1=mnew[0:M, :], op=ALU.subtract), ["mnew", "mst"], ["alp"])
                    tr.op('act', lambda e: e.activation(out=alp[0:M, :], in_=alp[0:M, :], func=AF.Exp, scale=SM_SCALE), ["alp"], ["alp"])
                    tr.op('dve', lambda e: e.tensor_copy(out=mst[0:M, :], in_=mnew[0:M, :]), ["mnew"], ["mst"])
                tr.op('dve', lambda e: e.tensor_scalar(out=nbias[0:M, :], in0=mst[0:M, :], scalar1=-SM_SCALE, scalar2=None, op0=ALU.mult), ["mst"], ["nbias"])
                tr.op('act', lambda e: e.activation(out=pbf[0:M, 0:N], in_=S, func=AF.Exp, bias=nbias[0:M, 0:1], scale=SM_SCALE, accum_out=rsum[0:M, 0:1]),
                      [psk(sbk), "nbias"], ["pbf", "rsum"])
                if first:
                    tr.op('dve', lambda e: e.tensor_copy(out=lst[0:M, :], in_=rsum[0:M, :]), ["rsum"], ["lst"])
                else:
                    tr.op('dve', lambda e: e.scalar_tensor_tensor(out=lst[0:M, :], in0=lst[0:M, :], scalar=alp[0:M, 0:1], in1=rsum[0:M, :], op0=ALU.mult, op1=ALU.add),
                          ["lst", "alp", "rsum"], ["lst"])
                pTp = ps[2][:, :].bitcast(BF16)
                c0 = 0
                for kt, (v_, nk, vk) in enumerate(vts):
                    tr.op('pe', lambda e, kt=kt, nk=nk, c0=c0: e.transpose(out=pTp[0:nk, kt * 128: kt * 128 + M], in_=pbf[0:M, c0:c0 + nk], identity=idb[0:M, 0:M]),
                          ["pbf", "idb"], [psk(2)], inc=(kt == len(vts) - 1))
                    c0 += nk
                nv = len(vts)
                tr.op('act', lambda e: e.activation(out=pT[:, 0:nv, 0:M], in_=pTp[:, 0:nv * 128].rearrange("p (k m) -> p k m", k=nv)[:, :, 0:M], func=AF.Copy), [psk(2)], ["pT"])
                for kt, (v_, nk, vk) in enumerate(vts):
                    tr.op('pe', lambda e, kt=kt, nk=nk, v_=v_: e.matmul(ps[3][0:M, 0:KVL], lhsT=pT[0:nk, kt, 0:M], rhs=v_, start=(kt == 0), stop=(kt == nv - 1)),
                          ["pT", vk], [psk(3)], inc=(kt == nv - 1))
                if first:
                    tr.op('dve', lambda e: e.tensor_copy(out=oacc[0:M, :], in_=ps[3][0:M, 0:KVL]), [psk(3)], ["oacc"])
                else:
                    tr.op('dve', lambda e: e.scalar_tensor_tensor(out=oacc[0:M, :], in0=oacc[0:M, :], scalar=alp[0:M, 0:1], in1=ps[3][0:M, 0:KVL], op0=ALU.mult, op1=ALU.add),
                          ["oacc", "alp", psk(3)], ["oacc"])
            attend_block.n = 0

            def finish_rows(M):
                tr.op('dve', lambda e: e.reciprocal(out=lst[0:M, :], in_=lst[0:M, :]), ["lst"], ["lst"])
                tr.op('dve', lambda e: e.tensor_scalar(out=olat[0:M, :], in0=oacc[0:M, :], scalar1=lst[0:M, 0:1], scalar2=None, op0=ALU.mult), ["oacc", "lst"], ["olat"])

            for ti in range(NTT):
                samp = (ti == NT)
                tr.dma('sp', ht[:], hB[ti], [("hB", ti)], ["ht"], "ht")
                tr.dma('sp', cosb[:], c_cos[ti], [], ["cosb"], "cosb"); tr.dma('sp', sinb[:], c_sin[ti], [], ["sinb"], "sinb")
                rms_scale((jk3, ss3, rs3), ht[:], "ht", D, jkey="jk3")
                tr.op('act', lambda e: e.activation(out=xs3[:], in_=ht[:], func=AF.Copy, scale=rs3[:, 0:1]), ["ht", "rstd"], ["xs3"])
                ptb6 = ps[6][:, :].bitcast(BF16)
                for c in range(8):
                    tr.op('pe', lambda e, c=c: e.transpose(out=ptb6[:, c * 128:(c + 1) * 128], in_=xs3[:, c * 128:(c + 1) * 128], identity=idb[:]), ["xs3", "idb"], [psk(6)], inc=(c == 7))
                pv6 = ptb6[:, :].rearrange("p (c t) -> p c t", c=8)
                tr.op('dve', lambda e: e.tensor_tensor(out=nkvT[:], in0=pv6, in1=gvs[:, 2, :].unsqueeze(2).broadcast_to([128, 8, 128]), op=ALU.mult), [psk(6), "gvs"], ["nkvT"])
                tr.op('dve', lambda e: e.tensor_tensor(out=xnbT[:], in0=pv6, in1=gvs[:, 3, :].unsqueeze(2).broadcast_to([128, 8, 128]), op=ALU.mult), [psk(6), "gvs"], ["xnbT"])
                for dc in range(8):
                    tr.op('pe', lambda e, dc=dc: e.matmul(ps[4][:, 0:320], lhsT=nkvT[:, dc, :], rhs=wdkv_sb[:, dc, :], start=(dc == 0), stop=(dc == 7)),
                          ["nkvT", "wdkv_sb"], [psk(4)], inc=(dc == 7))
                rms_scale((jk3, ss3, rs3), ps[4][:, 0:KVL], psk(4), KVL, jkey="jk3")
                tr.op('dve', lambda e: e.scalar_tensor_tensor(out=cf[:], in0=ps[4][:, 0:KVL], scalar=rs3[:, 0:1], in1=gkv_b[:], op0=ALU.mult, op1=ALU.mult),
                      [psk(4), "rstd", "gkv_b"], ["cf"])
                tr.dma('sp', (ckv_s[:, :] if samp else ckv_p[ti * 128:(ti + 1) * 128, :]), cf[:], ["cf"], ["ckvout"], "cf")
                ctk = ctoks[:] if samp else ctok[:, ti, :]
                ctkey = "ctoks" if samp else ("ctok", ti)
                tr.op('act', lambda e: e.activation(out=ctk, in_=cf[:], func=AF.Copy), ["cf"], [ctkey])
                x1 = ps[4][:, 256:288]; x2 = ps[4][:, 288:320]
                tr.op('dve', lambda e: e.tensor_tensor(out=krf[:, 0:32], in0=x1, in1=cosb[:], op=ALU.mult), [psk(4), "cosb"], ["krf"])
                tr.op('dve', lambda e: e.tensor_tensor(out=r1[:, 0, :], in0=x2, in1=sinb[:], op=ALU.mult), [psk(4), "sinb"], ["r1"])
                tr.op('dve', lambda e: e.tensor_tensor(out=krf[:, 0:32], in0=krf[:, 0:32], in1=r1[:, 0, :], op=ALU.subtract), ["krf", "r1"], ["krf"])
                tr.op('dve', lambda e: e.tensor_tensor(out=krf[:, 32:64], in0=x2, in1=cosb[:], op=ALU.mult), [psk(4), "cosb"], ["krf"])
                tr.op('dve', lambda e: e.tensor_tensor(out=r1[:, 0, :], in0=x1, in1=sinb[:], op=ALU.mult), [psk(4), "sinb"], ["r1"])
                tr.op('dve', lambda e: e.tensor_tensor(out=krf[:, 32:64], in0=krf[:, 32:64], in1=r1[:, 0, :], op=ALU.add), ["krf", "r1"], ["krf"])
                tr.dma('sp', (kr_s[:, :] if samp else kr_p[ti * 128:(ti + 1) * 128, :]), krf[:], ["krf"], ["krout"], "krf")
                tr.op('act', lambda e: e.activation(out=krb[:], in_=krf[:], func=AF.Copy), ["krf"], ["krb"])
                for cc in range(2):
                    tr.op('pe', lambda e, cc=cc: e.transpose(out=ptb6[:, cc * 128:(cc + 1) * 128], in_=ctk[:, cc * 128:(cc + 1) * 128], identity=idb[:]), [ctkey, "idb"], [psk(6)], inc=False)
                tr.op('pe', lambda e: e.transpose(out=ptb6[0:64, 256:384], in_=krb[:, :], identity=idb[:]), ["krb", "idb"], [psk(6)])
                cTd = cTs[:] if samp else cT[:, :, ti * 128:(ti + 1) * 128]
                cTk = "cTs" if samp else ("cT", ti)
                krTd = krTs[:] if samp else krT[:, ti * 128:(ti + 1) * 128]
                tr.op('dve', lambda e: e.tensor_copy(out=cTd, in_=ptb6[:, 0:256].rearrange("p (c t) -> p c t", c=2)), [psk(6)], [cTk])
                tr.op('dve', lambda e: e.tensor_copy(out=krTd, in_=ptb6[0:64, 256:384]), [psk(6)], [cTk])
                for dc in range(8):
                    tr.op('pe', lambda e, dc=dc: e.matmul(ps[5][:, 0:QL], lhsT=xnbT[:, dc, :], rhs=wdq_sb[:, dc, :], start=(dc == 0), stop=(dc == 7)),
                          ["xnbT", "wdq_sb"], [psk(5)], inc=(dc == 7))
                rms_scale((jk3, ss3, rs3), ps[5][:, 0:QL], psk(5), QL, jkey="jk3")
                tr.op('act', lambda e: e.activation(out=cqb[:], in_=ps[5][:, 0:QL], func=AF.Copy, scale=rs3[:, 0:1]), [psk(5), "rstd"], ["cqb"])
                ptb7 = ps[7][:, :].bitcast(BF16)
                for c in range(3):
                    tr.op('pe', lambda e, c=c: e.transpose(out=ptb7[:, c * 128:(c + 1) * 128], in_=cqb[:, c * 128:(c + 1) * 128], identity=idb[:]), ["cqb", "idb"], [psk(7)], inc=(c == 2))
                tr.op('dve', lambda e: e.tensor_tensor(out=cqT[:], in0=ptb7[:, 0:384].rearrange("p (c t) -> p c t", c=3), in1=gqs[:].unsqueeze(2).broadcast_to([128, 3, 128]), op=ALU.mult),
                      [psk(7), "gqs"], ["cqT"])
                for h in range(NH):
                    b = 4 + h // 4
                    for qc in range(3):
                        tr.op('pe', lambda e, h=h, qc=qc, b=b: e.matmul(ps[b][:, (h % 4) * 128:(h % 4 + 1) * 128], lhsT=wuq_sb[:, qc, h * 192:h * 192 + 128], rhs=cqT[:, qc, :],
                                                                        start=(qc == 0), stop=(qc == 2)), ["wuq_sb", "cqT"], [psk(b)], inc=(qc == 2 and h % 4 == 3))
                for hb in range(2):
                    tr.op('act', lambda e, hb=hb: e.activation(out=qnT[:, hb * 4:(hb + 1) * 4, :], in_=ps[4 + hb][:, :].rearrange("p (h t) -> p h t", h=4), func=AF.Copy), [psk(4 + hb)], ["qnT"])
                wr_ = wuq_sb[:, :, :].rearrange("p c (h x) -> p c h x", h=NH)
                for qc in range(3):
                    tr.op('pe', lambda e, qc=qc: e.matmul(ps[6][:, :].rearrange("p (h x) -> p h x", h=NH), lhsT=cqT[:, qc, :], rhs=wr_[:, qc, :, 128:192], start=(qc == 0), stop=(qc == 2)),
                          ["wuq_sb", "cqT"], [psk(6)], inc=(qc == 2))
                q3 = ps[6][:, :].rearrange("p (h x) -> p h x", h=NH)
                cb3 = cosb[:].unsqueeze(1).broadcast_to([128, NH, 32]); sb3 = sinb[:].unsqueeze(1).broadcast_to([128, NH, 32])
                tr.op('dve', lambda e: e.tensor_tensor(out=qrf[:, :, 0:32], in0=q3[:, :, 0:32], in1=cb3, op=ALU.mult), [psk(6), "cosb"], ["qrf"])
                tr.op('dve', lambda e: e.tensor_tensor(out=r1[:], in0=q3[:, :, 32:64], in1=sb3, op=ALU.mult), [psk(6), "sinb"], ["r1"])
                tr.op('dve', lambda e: e.tensor_tensor(out=qrf[:, :, 0:32], in0=qrf[:, :, 0:32], in1=r1[:], op=ALU.subtract), ["qrf", "r1"], ["qrf"])
                tr.op('dve', lambda e: e.tensor_tensor(out=qrf[:, :, 32:64], in0=q3[:, :, 32:64], in1=cb3, op=ALU.mult), [psk(6), "cosb"], ["qrf"])
                tr.op('dve', lambda e: e.tensor_tensor(out=r2[:], in0=q3[:, :, 0:32], in1=sb3, op=ALU.mult), [psk(6), "sinb"], ["r2"])
                tr.op('dve', lambda e: e.tensor_tensor(out=qrf[:, :, 32:64], in0=qrf[:, :, 32:64], in1=r2[:], op=ALU.add), ["qrf", "r2"], ["qrf"])
                tr.op('act', lambda e: e.activation(out=qrb[:], in_=qrf[:], func=AF.Copy), ["qrf"], ["qrb"])
                for h in range(NH):
                    tr.op('pe', lambda e, h=h: e.transpose(out=ptb7[0:64, h * 128:(h + 1) * 128], in_=qrb[:, h, :], identity=idb[:]), ["qrb", "idb"], [psk(7)], inc=(h == NH - 1))
                tr.op('dve', lambda e: e.tensor_copy(out=qrT[:], in_=ptb7[0:64, :].rearrange("p (h t) -> p h t", h=NH)), [psk(7)], ["qrT"])
                for h in range(NH):
                    for cc in range(2):
                        i_ = h * 2 + cc; b = 4 + i_ // 4
                        tr.op('pe', lambda e, h=h, cc=cc, i_=i_, b=b: e.matmul(ps[b][:, (i_ % 4) * 128:(i_ % 4 + 1) * 128], lhsT=wukT[:, h, cc * 128:(cc + 1) * 128], rhs=qnT[:, h, :],
                                                                              start=True, stop=True), ["wukT", "qnT"], [psk(b)], inc=(i_ % 4 == 3))
                for b in range(4):
                    tr.op('act', lambda e, b=b: e.activation(out=qaT[:, :, 2 * b:2 * b + 2, :].rearrange("p c h t -> p h c t"),
                                                             in_=ps[4 + b][:, :].rearrange("p (h c t) -> p h c t", h=2, c=2), func=AF.Copy), [psk(4 + b)], ["qaT"])
                if not samp:
                    for h in range(NH):
                        nkb = ti // 4 + 1
                        for kb in range(nkb):
                            t0 = kb * 4; t1_ = min(t0 + 4, ti + 1); N = (t1_ - t0) * 128
                            kkeys = [("cT", t_) for t_ in range(t0, t1_)]
                            qk = [(qaT[:, 0, h, :], cT[:, 0, t0 * 128:t1_ * 128], ["qaT"] + kkeys), (qaT[:, 1, h, :], cT[:, 1, t0 * 128:t1_ * 128], ["qaT"] + kkeys),
                                  (qrT[:, h, :], krT[:, t0 * 128:t1_ * 128], ["qrT"] + kkeys)]
                            mask = (cb[:], (ti - t0) * 128, 128, "cb") if kb == nkb - 1 else None
                            vts = [(ctok[:, t_, :], 128, ("ctok", t_)) for t_ in range(t0, t1_)]
                            attend_block(128, qk, N, mask, vts, kb == 0)
                        finish_rows(128)
                        for cc in range(2):
                            tr.op('pe', lambda e, cc=cc: e.transpose(out=ptb7[:, cc * 128:(cc + 1) * 128], in_=olat[:, cc * 128:(cc + 1) * 128], identity=idb[:]), ["olat", "idb"], [psk(7)], inc=(cc == 1))
                        tr.op('act', lambda e, h=h: e.activation(out=olatT[:, :, h, :], in_=ptb7[:, 0:256].rearrange("p (c t) -> p c t", c=2), func=AF.Copy), [psk(7)], ["olatT"])
                else:
                    npg = NPG
                    gn = 0
                    for b_ in range(SB):
                        tr.op('pool', lambda e, b_=b_: e.tensor_copy(out=qas[:], in_=qaT[:, :, :, b_ * ST:(b_ + 1) * ST]), ["qaT"], ["qas"])
                        tr.op('pool', lambda e, b_=b_: e.tensor_copy(out=qrs[:], in_=qrT[:, :, b_ * ST:(b_ + 1) * ST]), ["qrT"], ["qrs"])
                        qa0 = qas[:, 0, :, :].rearrange("p h t -> p (h t)"); qa1 = qas[:, 1, :, :].rearrange("p h t -> p (h t)")
                        qr_ = qrs[:, :, :].rearrange("p h t -> p (h t)")
                        first = True
                        for k0 in range(0, PAGE, KC):
                            gi = gn % 2; gn += 1
                            tr.dma('pool', gc[gi][0:npg].rearrange("p k c -> p (k c)"), ckv.rearrange("n k c -> n (k c)"), ["pti"], ["gc%d" % gi], "gc%d" % gi,
                                   indirect=(pti[0:npg, b_:b_ + 1], k0 * KVL))
                            tr.dma('pool', gr[gi][0:npg].rearrange("p k c -> p (k c)"), ckr.rearrange("n k c -> n (k c)"), ["pti"], ["gr%d" % gi], "gr%d" % gi,
                                   indirect=(pti[0:npg, b_:b_ + 1], k0 * QKR))
                            for r0 in range(0, KC, 4):
                                pg = ps[4 + (r0 // 4) % 2][:, :].bitcast(BF16); pgk = psk(4 + (r0 // 4) % 2)
                                for rr in range(4):
                                    for cc in range(2):
                                        tr.op('pe', lambda e, rr=rr, cc=cc, r0=r0, pg=pg: e.transpose(out=pg[:, cc * 512 + rr * npg: cc * 512 + (rr + 1) * npg], in_=gc[gi][0:npg, r0 + rr, cc * 128:(cc + 1) * 128],
                                                                                                   identity=idb[0:npg, 0:npg]), ["gc%d" % gi, "idb"], [pgk], inc=(rr == 3 and cc == 1))
                                tr.op('dve', lambda e, pg=pg: e.tensor_copy(out=gcT[:, :, 0:4 * npg], in_=pg[:, :].rearrange("p (c n) -> p c n", c=2)[:, :, 0:4 * npg]), [pgk], ["gcT"])
                                pr = ps[6][:, :].bitcast(BF16)
                                for rr in range(4):
                                    tr.op('pe', lambda e, rr=rr, r0=r0: e.transpose(out=pr[0:64, rr * npg:(rr + 1) * npg], in_=gr[gi][0:npg, r0 + rr, :], identity=idb[0:npg, 0:npg]),
                                          ["gr%d" % gi, "idb"], [psk(6)], inc=(rr == 3))
                                tr.op('act', lambda e: e.activation(out=grT[:, 0:4 * npg], in_=pr[0:64, 0:4 * npg], func=AF.Copy), [psk(6)], ["grT"])
                                qk = [(qa0, gcT[:, 0, 0:4 * npg], ["qas", "gcT"]), (qa1, gcT[:, 1, 0:4 * npg], ["qas", "gcT"]), (qr_, grT[:, 0:4 * npg], ["qrs", "grT"])]
                                vts = [(gc[gi][0:npg, r0 + rr, :], npg, "gc%d" % gi) for rr in range(4)]
                                attend_block(64, qk, 4 * npg, None, vts, first)
                                first = False
                        qk = [(qa0, cTs[:, 0, :], ["qas", "cTs"]), (qa1, cTs[:, 1, :], ["qas", "cTs"]), (qr_, krTs[:, :], ["qrs", "cTs"])]
                        attend_block(64, qk, 128, (msk[:, b_, :], 0, 128, "msk"), [(ctoks[:, :], 128, "ctoks")], False)
                        finish_rows(64)
                        for cc in range(2):
                            tr.op('pe', lambda e, cc=cc: e.transpose(out=ptb7[:, cc * 64:(cc + 1) * 64], in_=olat[0:64, cc * 128:(cc + 1) * 128], identity=idb[0:64, 0:64]), ["olat", "idb"], [psk(7)], inc=(cc == 1))
                        tr.op('act', lambda e, b_=b_: e.activation(out=olatT[:, :, :, b_ * ST:(b_ + 1) * ST], in_=ptb7[:, 0:128].rearrange("p (c h t) -> p c h t", c=2, h=NH), func=AF.Copy),
                              [psk(7)], ["olatT"])
                for h in range(NH):
                    b = 4 + h // 4
                    for cc in range(2):
                        tr.op('pe', lambda e, h=h, cc=cc, b=b: e.matmul(ps[b][:, (h % 4) * 128:(h % 4 + 1) * 128], lhsT=wukv_sb[:, cc, h * 256 + 128:h * 256 + 256], rhs=olatT[:, cc, h, :],
                                                                        start=(cc == 0), stop=(cc == 1)), ["wukv_sb", "olatT"], [psk(b)], inc=(cc == 1 and h % 4 == 3))
                for hb in range(2):
                    tr.op('act', lambda e, hb=hb: e.activation(out=oT[:, hb * 4:(hb + 1) * 4, :], in_=ps[4 + hb][:, :].rearrange("p (h t) -> p h t", h=4), func=AF.Copy), [psk(4 + hb)], ["oT"])
                for hb in range(2):
                    for h in range(NH):
                        tr.op('pe', lambda e, hb=hb, h=h: e.matmul(ps[4 + hb][:, :], lhsT=oT[:, h, :], rhs=woutb_sb[:, h, hb * 512:(hb + 1) * 512], start=(h == 0), stop=(h == NH - 1)),
                              ["oT", "woutb_sb"], [psk(4 + hb)], inc=(h == NH - 1))
                for hb in range(2):
                    tr.op('dve', lambda e, hb=hb: e.tensor_tensor(out=ht[:, hb * 512:(hb + 1) * 512], in0=ht[:, hb * 512:(hb + 1) * 512], in1=ps[4 + hb][:, :], op=ALU.add), [psk(4 + hb), "ht"], ["ht"])
                tr.dma('sp', hA[ti], ht[:], ["ht"], [("hA", ti)], "ht")

        outk = ["hg_p", "s0f", "cf", "krf"]
        if phases == 3:
            emit_debug(hA, outk)
            return nc, tr
        ffn_phase(hA, None, 4, NE, True)
        tr.final_wait('sp', outk + ["yst"])
    return nc, tr


def _consts(T, NPG):
    NT = T // 128
    bf = ml_dtypes.bfloat16
    c = {}
    c["c_idf"] = np.eye(128, dtype=np.float32)
    c["c_idb"] = np.eye(128, dtype=np.float32).astype(bf)
    s = np.arange(128)[:, None]; t = np.arange(128)[None, :]
    tri = np.zeros((2, 128, 128), np.float32); up = np.zeros((2, 128, 128), np.float32)
    for a, L in enumerate((64, 8)):
        same = (s // L) == (t // L)
        tri[a] = (same & (s <= t)).astype(np.float32)
        up[a] = (same & (s > t)).astype(np.float32)
    c["c_tri"] = tri; c["c_up"] = up
    c["c_cb"] = np.where(t <= s, 0.0, NEG).astype(np.float32)
    r = np.arange(64)[:, None]; kk = np.arange(8)[None, :]
    c["c_cbs"] = np.where(kk <= (r % 8), 0.0, NEG).astype(np.float32)
    rr_ = np.arange(64)[None, :, None]; kk_ = np.arange(128)[None, None, :]; bb_ = np.arange(SB)[:, None, None]
    c["c_msk"] = np.where(((kk_ // ST) == bb_) & ((kk_ % ST) <= (rr_ % ST)), 0.0, NEG).astype(np.float32)
    c["c_bm"] = ((np.arange(128)[:, None] // ST) == np.arange(SB)[None, :]).astype(np.float32)
    half = 32
    inv = (10000.0 ** (-np.arange(half, dtype=np.float32) / half)).astype(np.float32)
    pos = np.zeros((NT + 1, 128), np.float32)
    pos[:NT] = np.arange(T, dtype=np.float32).reshape(NT, 128)
    pos[NT] = (NPG * PAGE + (np.arange(128) % ST)).astype(np.float32)
    ang = pos[:, :, None] * inv[None, None, :]
    c["c_cos"] = np.cos(ang).astype(np.float32); c["c_sin"] = np.sin(ang).astype(np.float32)
    return c


def make_in_maps(inp, T, NPG):
    f = lambda a: np.ascontiguousarray(np.asarray(a))
    cst = _consts(T, NPG)
    gl = f(inp["gamma_lb"])
    shared = {
        "ckv": f(inp["cache_ckv"]), "ckr": f(inp["cache_krope"]),
        "w_in": f(inp["w_in_a"][0]), "w_outa": f(inp["w_out_a"][0]),
        "glb": gl, "glbT": f(gl.reshape(2, NH, 128).transpose(2, 0, 1)),
        "gv": f(np.stack([inp["g_mix_a"][0], inp["g_ffn"][0], inp["g_kv_in"], inp["g_mix_b"][0], inp["g_ffn"][1]], 0).reshape(5, 8, 128).transpose(2, 0, 1)),
        "gq": f(np.asarray(inp["g_q"][0]).reshape(3, 128).T),
        "g_o": f(inp["g_onorm_a"][0]), "g_kv": f(inp["g_kv"]), "g_fin": f(inp["g_final"]),
        "w_dkv": f(inp["w_dkv"]), "w_ukv": f(inp["w_ukv"]), "w_dq": f(inp["w_dq"][0]), "w_uq": f(inp["w_uq"][0]),
        "w_outb": f(inp["w_out_b"][0]),
        "w_fg": f(inp["w_ff_gate"][0]), "w_fu": f(inp["w_ff_up"][0]), "w_fd": f(inp["w_ff_down"][0]),
        "w_r": f(inp["w_router"][0]), "w_eg": f(inp["w_e_gate"][0]), "w_eu": f(inp["w_e_up"][0]), "w_ed": f(inp["w_e_down"][0]),
    }
    shared.update(cst)
    maps = []
    xp = np.asarray(inp["x_prompt"]); xs_ = np.asarray(inp["x_sample"]); st = np.asarray(inp["state_hgrn"]); pt = np.asarray(inp["page_table"])
    for c in range(NCORES):
        m = dict(shared)
        m["x_p"] = f(xp[c]); m["x_s"] = f(xs_[c * SB:(c + 1) * SB].reshape(SB * ST, D))
        m["st_in"] = f(st[0, c * SB:(c + 1) * SB]); m["ptT"] = f(pt[c * SB:(c + 1) * SB].T.astype(np.int32))
        maps.append(m)
    return maps


def kernel(**inputs):
    T = int(np.asarray(inputs["x_prompt"]).shape[1])
    NPG = int(np.asarray(inputs["page_table"]).shape[1])
    NPOOL = int(np.asarray(inputs["cache_ckv"]).shape[0])
    nc, _ = build(T, NPG, NPOOL)
    maps = make_in_maps(inputs, T, NPG)
    res = run_bass_kernel_spmd(nc, maps, core_ids=list(range(NCORES)))
    r = res.results
    f32 = np.float32
    y_p = np.stack([r[c]["y_p"] for c in range(NCORES)]).astype(f32)
    y_s = np.concatenate([r[c]["y_s"].reshape(SB, ST, D) for c in range(NCORES)], 0).astype(f32)
    ckv_p = np.stack([r[c]["ckv_p"] for c in range(NCORES)]).astype(f32)
    kr_p = np.stack([r[c]["kr_p"] for c in range(NCORES)]).astype(f32)
    ckv_s = np.concatenate([r[c]["ckv_s"].reshape(SB, ST, KVL) for c in range(NCORES)], 0).astype(f32)
    kr_s = np.concatenate([r[c]["kr_s"].reshape(SB, ST, QKR) for c in range(NCORES)], 0).astype(f32)
    hg_p = np.stack([r[c]["hg_p"] for c in range(NCORES)])[None].astype(f32)
    hg_s = np.concatenate([r[c]["hg_s"] for c in range(NCORES)], 0)[None].astype(f32)
    return (y_p, y_s, ckv_p, kr_p, ckv_s, kr_s, hg_p, hg_s)
```

```python
import numpy as np
import ml_dtypes
from contextlib import ExitStack
import concourse.bass as bass
import concourse.mybir as mybir
from concourse.bass_utils import run_bass_kernel_spmd

F32 = mybir.dt.float32
BF16 = mybir.dt.bfloat16
I32 = mybir.dt.int32
AF = mybir.ActivationFunctionType
ALU = mybir.AluOpType
AX = mybir.AxisListType

D = 1024
NH = 8
DFF = 2816
NFC = DFF // 128
NE = 8
KVL = 256
QKR = 64
QL = 384
EPS = 1e-6
SM_SCALE = (128 + 64) ** -0.5
NEG = -1e30
NCORES = 8
SB = 16
ST = 8
PAGE = 128


class Tr:
    def __init__(self, nc):
        self.nc = nc
        self.eng = {'pe': nc.tensor, 'act': nc.scalar, 'dve': nc.vector, 'pool': nc.gpsimd, 'sp': nc.sync}
        self.sem = {e: nc.alloc_semaphore("sem_" + e) for e in self.eng}
        self.cnt = {e: 0 for e in self.eng}
        self.pending = {e: False for e in self.eng}
        self.W = {}
        self.R = {}
        self.waited = {e: {} for e in self.eng}
        self.dsem = {}
        self.nwait = 0
        self.nins = 0

    def _deps(self, reads, writes):
        deps = {}

        def add(sem, val):
            if deps.get(sem, 0) < val:
                deps[sem] = val
        for r in reads:
            w = self.W.get(r)
            if w:
                add(*w)
        for w_ in writes:
            w = self.W.get(w_)
            if w:
                add(*w)
            for s, v in self.R.get(w_, {}).items():
                add(s, v)
        return deps

    def _wait(self, e, deps):
        wd = self.waited[e]
        for sem, val in deps.items():
            if e == 'pe' and sem is self.sem['pe']:
                continue
            if wd.get(sem, 0) >= val:
                continue
            self.eng[e].wait_ge(sem, val)
            wd[sem] = val
            self.nwait += 1

    def _record(self, ev, reads, writes):
        for r in reads:
            d = self.R.setdefault(r, {})
            if d.get(ev[0], 0) < ev[1]:
                d[ev[0]] = ev[1]
        for w in writes:
            self.W[w] = ev
            self.R[w] = {}

    def op(self, e, fn, reads=(), writes=(), inc=True):
        self._wait(e, self._deps(reads, writes))
        ins = fn(self.eng[e])
        self.nins += 1
        if inc:
            self.cnt[e] += 1
            ins.then_inc(self.sem[e], 1)
            ev = (self.sem[e], self.cnt[e])
        else:
            ev = (self.sem[e], self.cnt[e] + 1)
        self._record(ev, reads, writes)
        return ins

    def dma(self, e, out, in_, reads, writes, key, indirect=None):
        self._wait(e, self._deps(reads, writes))
        if key not in self.dsem:
            self.dsem[key] = [self.nc.alloc_semaphore("dsem_%d" % len(self.dsem)), 0]
        ds = self.dsem[key]
        if indirect is None:
            ins = self.eng[e].dma_start(out=out, in_=in_)
        else:
            idx_ap, eoff = indirect
            ins = self.eng[e].indirect_dma_start(
                out=out, out_offset=None, in_=in_,
                in_offset=bass.IndirectOffsetOnAxis(ap=idx_ap, axis=0), element_offset=eoff)
        ds[1] += 16
        ins.then_inc(ds[0], 16)
        self.nins += 1
        self._record((ds[0], ds[1]), reads, writes)

    def barrier(self):
        deps = {self.sem[e]: self.cnt[e] for e in self.eng if self.cnt[e] > 0}
        for k, ds in self.dsem.items():
            if ds[1] > 0:
                deps[ds[0]] = ds[1]
        for e in self.eng:
            self._wait(e, deps)

    def final_wait(self, e, keys):
        deps = {}
        for k in keys:
            ds = self.dsem[k]
            deps[ds[0]] = ds[1]
        self._wait(e, deps)


def build(T, NPG, NPOOL, phases=4):
    NT = T // 128
    NTT = NT + 1
    nc = bass.Bass("TRN2", target_bir_lowering=False)
    tr = Tr(nc)

    def din(name, shape, dt=F32):
        return nc.dram_tensor(name, list(shape), dt, kind="ExternalInput").ap()

    def dout(name, shape, dt=F32):
        return nc.dram_tensor(name, list(shape), dt, kind="ExternalOutput").ap()

    x_p = din("x_p", [T, D]); x_s = din("x_s", [128, D])
    ckv = din("ckv", [NPOOL, PAGE, KVL]); ckr = din("ckr", [NPOOL, PAGE, QKR])
    st_in = din("st_in", [SB, NH, 128, 128]); ptT = din("ptT", [NPG, SB], I32)
    w_in = din("w_in", [D, 4 * D]); w_outa = din("w_outa", [D, D])
    glb = din("glb", [2, D])
    glbT = din("glbT", [128, 2, NH])
    gv = din("gv", [128, 5, 8])
    gq = din("gq", [128, 3])
    g_o = din("g_o", [128]); g_kv = din("g_kv", [KVL]); g_fin = din("g_fin", [D])
    w_dkv = din("w_dkv", [D, KVL + QKR]); w_ukv = din("w_ukv", [KVL, NH * 256])
    w_dq = din("w_dq", [D, QL]); w_uq = din("w_uq", [QL, NH * 192]); w_outb = din("w_outb", [D, D])
    w_fg = din("w_fg", [D, DFF]); w_fu = din("w_fu", [D, DFF]); w_fd = din("w_fd", [DFF, D])
    w_r = din("w_r", [D, NE])
    w_eg = din("w_eg", [NE, D, DFF]); w_eu = din("w_eu", [NE, D, DFF]); w_ed = din("w_ed", [NE, DFF, D])
    c_idf = din("c_idf", [128, 128]); c_idb = din("c_idb", [128, 128], BF16)
    c_tri = din("c_tri", [2, 128, 128]); c_up = din("c_up", [2, 128, 128])
    c_cb = din("c_cb", [128, 128]); c_cbs = din("c_cbs", [64, 8]); c_bm = din("c_bm", [128, SB])
    c_msk = din("c_msk", [SB, 64, 128])
    c_cos = din("c_cos", [NTT, 128, 32]); c_sin = din("c_sin", [NTT, 128, 32])

    y_p = dout("y_p", [T, D]); y_s = dout("y_s", [128, D])
    ckv_p = dout("ckv_p", [T, KVL]); kr_p = dout("kr_p", [T, QKR])
    ckv_s = dout("ckv_s", [128, KVL]); kr_s = dout("kr_s", [128, QKR])
    hg_p = dout("hg_p", [NH, 128, 128]); hg_s = dout("hg_s", [SB, NH, 128, 128])
    hA = nc.dram_tensor("hA", [NTT, 128, D], F32, kind="Internal").ap()
    hB = nc.dram_tensor("hB", [NTT, 128, D], F32, kind="Internal").ap()

    def xrows(i):
        return x_p[i * 128:(i + 1) * 128, :] if i < NT else x_s[:, :]

    ps = [nc.alloc_psum_tensor("ps%d" % i, [128, 512], F32) for i in range(8)]

    def psk(i):
        return ("ps", i)

    with ExitStack() as g:
        used_names = {}

        def sb(stack, name, shape, dt=F32):
            n = used_names.get(name, 0)
            used_names[name] = n + 1
            if n:
                name = "%s_v%d" % (name, n)
            return stack.enter_context(nc.sbuf_tensor(name, list(shape), dt))

        idf = sb(g, "idf", [128, 128]); idb = sb(g, "idb", [128, 128], BF16)
        tri = sb(g, "tri", [128, 2, 128]); up = sb(g, "up", [128, 2, 128])
        trib = sb(g, "trib", [128, 2, 128], BF16)
        cb = sb(g, "cb", [128, 128]); cbs = sb(g, "cbs", [64, 8]); bm = sb(g, "bm", [128, SB])
        gvs = sb(g, "gvs", [128, 5, 8]); gqs = sb(g, "gqs", [128, 3])
        glbTs = sb(g, "glbTs", [128, 2, NH]); omlT = sb(g, "omlT", [128, NH])
        lb_b = sb(g, "lb_b", [128, D]); oml_b = sb(g, "oml_b", [128, D])
        go_b = sb(g, "go_b", [128, 128]); gkv_b = sb(g, "gkv_b", [128, KVL]); gfin_b = sb(g, "gfin_b", [128, D])
        ctmp = sb(g, "ctmp", [128, D])

        def ld(dst, src, key, eng='sp'):
            tr.dma(eng, dst, src, [], [key], key)
        ld(idf[:], c_idf[:, :], "idf"); ld(idb[:], c_idb[:, :], "idb")
        ld(tri[:], c_tri.rearrange("a s t -> s a t"), "tri"); ld(up[:], c_up.rearrange("a s t -> s a t"), "up")
        ld(cb[:], c_cb[:, :], "cb"); ld(cbs[:], c_cbs[:, :], "cbs"); ld(bm[:], c_bm[:, :], "bm")
        ld(gvs[:], gv[:, :, :], "gvs"); ld(gqs[:], gq[:, :], "gqs"); ld(glbTs[:], glbT[:, :, :], "glbTs")
        ld(lb_b[:], glb[0, :].partition_broadcast(128), "lb_b")
        ld(ctmp[:], glb[1, :].partition_broadcast(128), "ctmp")
        ld(go_b[:], g_o.partition_broadcast(128), "go_b")
        ld(gkv_b[:], g_kv.partition_broadcast(128), "gkv_b")
        ld(gfin_b[:], g_fin.partition_broadcast(128), "gfin_b")
        tr.op('dve', lambda e: e.tensor_copy(out=trib[:], in_=tri[:]), ["tri"], ["trib"])
        tr.op('dve', lambda e: e.tensor_tensor(out=ctmp[:], in0=ctmp[:], in1=lb_b[:], op=ALU.subtract), ["ctmp", "lb_b"], ["ctmp"])
        tr.op('act', lambda e: e.activation(out=ctmp[:], in_=ctmp[:], func=AF.Exp), ["ctmp"], ["ctmp"])
        tr.op('dve', lambda e: e.tensor_scalar(out=ctmp[:], in0=ctmp[:], scalar1=1.0, scalar2=None, op0=ALU.add), ["ctmp"], ["ctmp"])
        tr.op('dve', lambda e: e.reciprocal(out=lb_b[:], in_=ctmp[:]), ["ctmp"], ["lb_b"])
        tr.op('dve', lambda e: e.tensor_scalar(out=oml_b[:], in0=lb_b[:], scalar1=-1.0, scalar2=1.0, op0=ALU.mult, op1=ALU.add), ["lb_b"], ["oml_b"])
        tr.op('dve', lambda e: e.tensor_tensor(out=omlT[:], in0=glbTs[:, 1, :], in1=glbTs[:, 0, :], op=ALU.subtract), ["glbTs"], ["omlT"])
        tr.op('act', lambda e: e.activation(out=omlT[:], in_=omlT[:], func=AF.Exp), ["omlT"], ["omlT"])
        tr.op('dve', lambda e: e.tensor_scalar(out=omlT[:], in0=omlT[:], scalar1=1.0, scalar2=None, op0=ALU.add), ["omlT"], ["omlT"])
        tr.op('dve', lambda e: e.reciprocal(out=omlT[:], in_=omlT[:]), ["omlT"], ["omlT"])
        tr.op('dve', lambda e: e.tensor_scalar(out=omlT[:], in0=omlT[:], scalar1=-1.0, scalar2=1.0, op0=ALU.mult, op1=ALU.add), ["omlT"], ["omlT"])

        wsrc = [("w_in", w_in), ("w_outa", w_outa), ("w_dkv", w_dkv), ("w_ukv", w_ukv), ("w_dq", w_dq), ("w_uq", w_uq),
                ("w_outb", w_outb), ("w_fg", w_fg), ("w_fu", w_fu), ("w_fd", w_fd)]
        wb = {}
        for nm, ap_ in wsrc:
            wb[nm] = nc.dram_tensor("b_" + nm, list(ap_.shape), BF16, kind="Internal").ap()
        for nm, ap_ in (("w_eg", w_eg), ("w_eu", w_eu), ("w_ed", w_ed)):
            wb[nm] = nc.dram_tensor("b_" + nm, list(ap_.shape), BF16, kind="Internal").ap()
        with ExitStack() as p0:
            NSL = 4
            stg = [sb(p0, "stg%d" % i, [128, 4096]) for i in range(NSL)]
            stb = [sb(p0, "stb%d" % i, [128, 4096], BF16) for i in range(NSL)]
            kk_ = [0]

            def precast(src2d, dst2d):
                tot = src2d.shape[0] * src2d.shape[1]
                per = tot // 128
                sv = src2d.rearrange("a b -> (a b)").rearrange("(p f) -> p f", p=128)
                dv = dst2d.rearrange("a b -> (a b)").rearrange("(p f) -> p f", p=128)
                for f0 in range(0, per, 4096):
                    n = min(4096, per - f0)
                    k = kk_[0]; kk_[0] += 1
                    sl = k % NSL
                    tr.dma('sp', stg[sl][:, 0:n], sv[:, f0:f0 + n], [], ["stg%d" % sl], "stg%d" % sl)
                    eng = ('act', 'dve', 'pool')[k % 3]
                    if eng == 'act':
                        tr.op('act', lambda e, sl=sl, n=n: e.activation(out=stb[sl][:, 0:n], in_=stg[sl][:, 0:n], func=AF.Copy), ["stg%d" % sl], ["stb%d" % sl])
                    else:
                        tr.op(eng, lambda e, sl=sl, n=n: e.tensor_copy(out=stb[sl][:, 0:n], in_=stg[sl][:, 0:n]), ["stg%d" % sl], ["stb%d" % sl])
                    tr.dma('sp', dv[:, f0:f0 + n], stb[sl][:, 0:n], ["stb%d" % sl], ["wbout"], "stb%d" % sl)
            for nm, ap_ in wsrc:
                precast(ap_, wb[nm])
            for nm, ap_ in (("w_eg", w_eg), ("w_eu", w_eu), ("w_ed", w_ed)):
                for ex in range(NE):
                    precast(ap_[ex], wb[nm][ex])
        tr.barrier()
        w_in, w_outa, w_dkv, w_ukv, w_dq, w_uq, w_outb, w_fg, w_fu, w_fd = [wb[nm] for nm, _ in wsrc]
        w_eg, w_eu, w_ed = wb["w_eg"], wb["w_eu"], wb["w_ed"]

        def rms_scale(stack_bufs, xt, xkey, width, jkey="junk"):
            junk, ss, rstd = stack_bufs
            tr.op('act', lambda e: e.activation(out=junk[:, 0:width], in_=xt, func=AF.Square, accum_out=ss[:, 0:1]),
                  [xkey], [jkey, "ss"])
            tr.op('dve', lambda e: e.tensor_scalar(out=rstd[:, 0:1], in0=ss[:, 0:1], scalar1=1.0 / width, scalar2=EPS,
                                                   op0=ALU.mult, op1=ALU.add), ["ss"], ["rstd"])
            tr.op('act', lambda e: e.activation(out=rstd[:, 0:1], in_=rstd[:, 0:1], func=AF.Ln), ["rstd"], ["rstd"])
            tr.op('act', lambda e: e.activation(out=rstd[:, 0:1], in_=rstd[:, 0:1], func=AF.Exp, scale=-0.5), ["rstd"], ["rstd"])

        def transpose_bf(dstT, dkey, src_bf, skey, nchunk, bank, gain=None, gkey=None, eng='dve'):
            pt = ps[bank][:, :].bitcast(BF16)
            for c in range(nchunk):
                tr.op('pe', lambda e, c=c: e.transpose(out=pt[:, c * 128:(c + 1) * 128], in_=src_bf[:, c * 128:(c + 1) * 128], identity=idb[:]),
                      [skey, "idb"], [psk(bank)], inc=(c == nchunk - 1))
            pv = pt[:, 0:nchunk * 128].rearrange("p (c t) -> p c t", c=nchunk)
            if gain is None:
                if eng == 'act':
                    tr.op('act', lambda e: e.activation(out=dstT, in_=pv, func=AF.Copy), [psk(bank)], [dkey])
                else:
                    tr.op('dve', lambda e: e.tensor_copy(out=dstT, in_=pv), [psk(bank)], [dkey])
            else:
                tr.op('dve', lambda e: e.tensor_tensor(out=dstT, in0=pv, in1=gain.unsqueeze(2).broadcast_to([128, nchunk, 128]), op=ALU.mult),
                      [psk(bank), gkey], [dkey])

        def sigmoid_from_exp(buf, key, eng='dve'):
            tr.op(eng, lambda e: e.tensor_scalar(out=buf, in0=buf, scalar1=1.0, scalar2=None, op0=ALU.add), [key], [key])
            tr.op('dve', lambda e: e.reciprocal(out=buf, in_=buf), [key], [key])

        with ExitStack() as p1:
            w_in_sb = sb(p1, "w_in_sb", [128, 8, 4 * D], BF16)
            w_out_sb = sb(p1, "w_out_sb", [128, 8, D], BF16)
            for dc in range(8):
                for hh in range(2):
                    tr.dma('sp', w_in_sb[:, dc, hh * 2048:(hh + 1) * 2048], w_in[dc * 128:(dc + 1) * 128, hh * 2048:(hh + 1) * 2048],
                           [], ["w_in_sb"], "w_in_sb")
                tr.dma('sp', w_out_sb[:, dc, :], w_outa[dc * 128:(dc + 1) * 128, :], [], ["w_out_sb"], "w_out_sb")
            xt = [sb(p1, "xt%d" % i, [128, D]) for i in range(2)]
            ss = sb(p1, "ss", [128, 1]); rstd = sb(p1, "rstd", [128, 1])
            xs = sb(p1, "xs", [128, D], BF16); xnT = sb(p1, "xnT", [128, 8, 128], BF16)
            tA = sb(p1, "tA", [128, D]); tB = sb(p1, "tB", [128, D]); tC = sb(p1, "tC", [128, D])
            logf = sb(p1, "logf", [128, D]); ktok = sb(p1, "ktok", [128, D])
            kd = sb(p1, "kd", [128, D], BF16); vtok = sb(p1, "vtok", [128, D], BF16)
            sgate = sb(p1, "sgate", [128, D])
            sq = sb(p1, "sq", [128, NH, 128]); kT = sb(p1, "kT", [128, NH, 128])
            qeT = sb(p1, "qeT", [128, NH, 128], BF16); keT = sb(p1, "keT", [128, NH, 128], BF16)
            qem = sb(p1, "qem", [128, NH, 2, 128], BF16)
            qems = sb(p1, "qems", [128, SB, 128], BF16)
            ebl = sb(p1, "ebl", [128, NH, SB])
            scm = sb(p1, "scm", [128, NH, 128], BF16)
            S32 = sb(p1, "S32", [128, NH, 128]); S1_32 = sb(p1, "S1_32", [128, NH, 128])
            Sb = sb(p1, "Sb", [128, NH, 128], BF16); S1b = sb(p1, "S1b", [128, NH, 128], BF16)
            rs8 = sb(p1, "rs8", [128, NH]); onb = sb(p1, "onb", [128, D], BF16); onT = sb(p1, "onT", [128, 8, 128], BF16)
            s0f = sb(p1, "s0f", [128, SB, 128]); s0b = sb(p1, "s0b", [128, SB, 128], BF16)
            kdm = sb(p1, "kdm", [128, SB, 128], BF16); snw = s0f

            tr.op('pool', lambda e: e.memset(qem[:], 0.0), [], ["qem"])
            tr.op('pool', lambda e: e.memset(qems[:], 0.0), [], ["qems"])
            tr.op('pool', lambda e: e.memset(S32[:], 0.0), [], ["S32"])
            tr.op('pool', lambda e: e.memset(Sb[:], 0.0), [], ["Sb"])

            def proj_tok(col0, banks):
                for hb in range(2):
                    for dc in range(8):
                        tr.op('pe', lambda e, hb=hb, dc=dc: e.matmul(ps[banks[hb]][:, :], lhsT=xnT[:, dc, :],
                                                                      rhs=w_in_sb[:, dc, col0 + hb * 512: col0 + (hb + 1) * 512],
                                                                      start=(dc == 0), stop=(dc == 7)),
                              ["xnT", "w_in_sb"], [psk(banks[hb])], inc=(dc == 7))

            def proj_feat(col0, banks):
                for fc in range(8):
                    b = banks[fc // 4]
                    for dc in range(8):
                        tr.op('pe', lambda e, fc=fc, dc=dc, b=b: e.matmul(ps[b][:, (fc % 4) * 128:(fc % 4 + 1) * 128],
                                                                          lhsT=w_in_sb[:, dc, col0 + fc * 128: col0 + (fc + 1) * 128],
                                                                          rhs=xnT[:, dc, :], start=(dc == 0), stop=(dc == 7)),
                              ["xnT", "w_in_sb"], [psk(b)], inc=(dc == 7))

            def ps2(banks):
                return [(ps[banks[0]][:, :], slice(0, 512), psk(banks[0])), (ps[banks[1]][:, :], slice(512, 1024), psk(banks[1]))]

            for ti in range(NTT):
                samp = (ti == NT)
                cm = 1 if samp else 0
                x_t = xt[ti % 2]; xk = "xt%d" % (ti % 2)
                tr.dma('sp', x_t[:], xrows(ti), [], [xk], xk)
                rms_scale((tB, ss, rstd), x_t[:], xk, D, jkey="tB")
                tr.op('act', lambda e: e.activation(out=xs[:], in_=x_t[:], func=AF.Copy, scale=rstd[:, 0:1]), [xk, "rstd"], ["xs"])
                transpose_bf(xnT[:], "xnT", xs, "xs", 8, 4, gain=gvs[:, 0, :], gkey="gvs")
                proj_feat(0, (0, 1))
                sqf = sq[:].rearrange("p h t -> p (h t)")
                for pa, sl, pk in ps2((0, 1)):
                    tr.op('act', lambda e, pa=pa, sl=sl: e.activation(out=sqf[:, sl], in_=pa, func=AF.Exp, scale=-1.0), [pk], ["sq"])
                sigmoid_from_exp(sqf, "sq")
                for pa, sl, pk in ps2((0, 1)):
                    tr.op('dve', lambda e, pa=pa, sl=sl: e.tensor_tensor(out=sqf[:, sl], in0=sqf[:, sl], in1=pa, op=ALU.mult), [pk, "sq"], ["sq"])
                proj_feat(D, (2, 3))
                kTf = kT[:].rearrange("p h t -> p (h t)")
                for pa, sl, pk in ps2((2, 3)):
                    tr.op('act', lambda e, pa=pa, sl=sl: e.activation(out=kTf[:, sl], in_=pa, func=AF.Exp), [pk], ["kT"])
                sigmoid_from_exp(kTf, "kT")
                tr.op('dve', lambda e: e.tensor_tensor(out=kT[:], in0=kT[:], in1=omlT[:].unsqueeze(2).broadcast_to([128, NH, 128]), op=ALU.mult),
                      ["kT", "omlT"], ["kT"])
                proj_tok(D, (0, 1))
                for pa, sl, pk in ps2((0, 1)):
                    tr.op('act', lambda e, pa=pa, sl=sl: e.activation(out=tA[:, sl], in_=pa, func=AF.Exp, scale=-1.0), [pk], ["tA"])
                sigmoid_from_exp(tA[:], "tA")
                tr.op('dve', lambda e: e.tensor_tensor(out=tA[:], in0=tA[:], in1=oml_b[:], op=ALU.mult), ["tA", "oml_b"], ["tA"])
                tr.op('dve', lambda e: e.tensor_tensor(out=tA[:], in0=tA[:], in1=lb_b[:], op=ALU.add), ["tA", "lb_b"], ["tA"])
                tr.op('act', lambda e: e.activation(out=logf[:], in_=tA[:], func=AF.Ln), ["tA"], ["logf"])
                tr.op('pool', lambda e: e.tensor_scalar(out=ktok[:], in0=tA[:], scalar1=-1.0, scalar2=1.0, op0=ALU.mult, op1=ALU.add), ["tA"], ["ktok"])
                for h in range(NH):
                    b = 2 + h // 4
                    tr.op('pe', lambda e, h=h, b=b: e.matmul(ps[b][:, (h % 4) * 128:(h % 4 + 1) * 128], lhsT=logf[:, h * 128:(h + 1) * 128],
                                                             rhs=tri[:, cm, :], start=True, stop=True),
                          ["logf", "tri"], [psk(b)], inc=(h % 4 == 3))
                for hb in range(2):
                    tr.op('pe', lambda e, hb=hb: e.matmul(ps[hb][:, :], lhsT=up[:, cm, :], rhs=logf[:, hb * 512:(hb + 1) * 512], start=True, stop=True),
                          ["logf", "up"], [psk(hb)])
                tBf = tB[:]; tCf = tC[:]
                for pa, sl, pk in ps2((2, 3)):
                    tr.op('act', lambda e, pa=pa, sl=sl: e.activation(out=tBf[:, sl], in_=pa, func=AF.Exp), [pk], ["tB"])
                    tr.op('act', lambda e, pa=pa, sl=sl: e.activation(out=tCf[:, sl], in_=pa, func=AF.Exp, scale=-1.0), [pk], ["tC"])
                tr.op('dve', lambda e: e.tensor_tensor(out=qeT[:].rearrange("p h t -> p (h t)"), in0=sqf, in1=tBf, op=ALU.mult), ["sq", "tB"], ["qeT"])
                tr.op('dve', lambda e: e.tensor_tensor(out=keT[:].rearrange("p h t -> p (h t)"), in0=kTf, in1=tCf, op=ALU.mult), ["kT", "tC"], ["keT"])
                nch = SB if samp else 2
                cl = 128 // nch
                tB3 = tB[:].rearrange("p (h c l) -> p h c l", h=NH, c=nch)
                tr.op('pool', lambda e: e.tensor_copy(out=ebl[:, :, 0:nch], in_=tB3[:, :, :, cl - 1]), ["tB"], ["ebl"])
                for pa, sl, pk in ps2((0, 1)):
                    tr.op('act', lambda e, pa=pa, sl=sl: e.activation(out=tA[:, sl], in_=pa, func=AF.Exp), [pk], ["tA"])
                tr.op('dve', lambda e: e.tensor_tensor(out=kd[:], in0=ktok[:], in1=tA[:], op=ALU.mult), ["ktok", "tA"], ["kd"])
                proj_tok(2 * D, (2, 3))
                for pa, sl, pk in ps2((2, 3)):
                    tr.op('act', lambda e, pa=pa, sl=sl: e.activation(out=vtok[:, sl], in_=pa, func=AF.Copy), [pk], ["vtok"])
                proj_tok(3 * D, (0, 1))
                for pa, sl, pk in ps2((0, 1)):
                    tr.op('act', lambda e, pa=pa, sl=sl: e.activation(out=sgate[:, sl], in_=pa, func=AF.Exp, scale=-1.0), [pk], ["sgate"])
                sigmoid_from_exp(sgate[:], "sgate")
                for pa, sl, pk in ps2((0, 1)):
                    tr.op('dve', lambda e, pa=pa, sl=sl: e.tensor_tensor(out=sgate[:, sl], in0=sgate[:, sl], in1=pa, op=ALU.mult), [pk, "sgate"], ["sgate"])
                for h in range(NH):
                    b = 4 + h // 4
                    tr.op('pe', lambda e, h=h, b=b: e.matmul(ps[b][:, (h % 4) * 128:(h % 4 + 1) * 128], lhsT=keT[:, h, :], rhs=qeT[:, h, :],
                                                             start=True, stop=True), ["keT", "qeT"], [psk(b)], inc=(h % 4 == 3))
                for hb in range(2):
                    tr.op('dve', lambda e, hb=hb: e.tensor_tensor(out=scm[:, hb * 4:(hb + 1) * 4, :],
                                                                  in0=ps[4 + hb][:, :].rearrange("p (h t) -> p h t", h=4),
                                                                  in1=tri[:, cm, :].unsqueeze(1).broadcast_to([128, 4, 128]), op=ALU.mult),
                          [psk(4 + hb), "tri"], ["scm"])
                if not samp:
                    tr.op('pool', lambda e: e.tensor_copy(out=qem[:, :, 0, 0:64], in_=qeT[:, :, 0:64]), ["qeT"], ["qem"])
                    tr.op('pool', lambda e: e.tensor_copy(out=qem[:, :, 1, 64:128], in_=qeT[:, :, 64:128]), ["qeT"], ["qem"])
                    for h in range(NH):
                        b = 6 + h // 4
                        tr.op('pe', lambda e, h=h, b=b: e.matmul(ps[b][:, (h % 4) * 128:(h % 4 + 1) * 128], lhsT=kd[0:64, h * 128:(h + 1) * 128],
                                                                 rhs=vtok[0:64, h * 128:(h + 1) * 128], start=True, stop=True),
                              ["kd", "vtok"], [psk(b)], inc=(h % 4 == 3))
                    tr.op('dve', lambda e: e.tensor_tensor(out=S1_32[:], in0=S32[:], in1=ebl[:, :, 0:1].broadcast_to([128, NH, 128]), op=ALU.mult),
                          ["S32", "ebl"], ["S1_32"])
                    for hb in range(2):
                        tr.op('dve', lambda e, hb=hb: e.tensor_tensor(out=S1_32[:, hb * 4:(hb + 1) * 4, :], in0=S1_32[:, hb * 4:(hb + 1) * 4, :],
                                                                      in1=ps[6 + hb][:, :].rearrange("p (h v) -> p h v", h=4), op=ALU.add),
                              [psk(6 + hb), "S1_32"], ["S1_32"])
                    tr.op('pool', lambda e: e.tensor_copy(out=S1b[:], in_=S1_32[:]), ["S1_32"], ["S1b"])
                    for h in range(NH):
                        b = 2 + h // 4
                        osl = ps[b][:, (h % 4) * 128:(h % 4 + 1) * 128]
                        tr.op('pe', lambda e, h=h, osl=osl: e.matmul(osl, lhsT=qem[:, h, 0, :], rhs=Sb[:, h, :], start=True, stop=False),
                              ["qem", "Sb"], [psk(b)], inc=False)
                        tr.op('pe', lambda e, h=h, osl=osl: e.matmul(osl, lhsT=qem[:, h, 1, :], rhs=S1b[:, h, :], start=False, stop=False),
                              ["qem", "S1b"], [psk(b)], inc=False)
                        tr.op('pe', lambda e, h=h, osl=osl: e.matmul(osl, lhsT=scm[:, h, :], rhs=vtok[:, h * 128:(h + 1) * 128], start=False, stop=True),
                              ["scm", "vtok"], [psk(b)], inc=(h % 4 == 3))
                    for h in range(NH):
                        b = 6 + h // 4
                        tr.op('pe', lambda e, h=h, b=b: e.matmul(ps[b][:, (h % 4) * 128:(h % 4 + 1) * 128], lhsT=kd[64:128, h * 128:(h + 1) * 128],
                                                                 rhs=vtok[64:128, h * 128:(h + 1) * 128], start=True, stop=True),
                              ["kd", "vtok"], [psk(b)], inc=(h % 4 == 3))
                    tr.op('dve', lambda e: e.tensor_tensor(out=S32[:], in0=S1_32[:], in1=ebl[:, :, 1:2].broadcast_to([128, NH, 128]), op=ALU.mult),
                          ["S1_32", "ebl"], ["S32"])
                    for hb in range(2):
                        tr.op('dve', lambda e, hb=hb: e.tensor_tensor(out=S32[:, hb * 4:(hb + 1) * 4, :], in0=S32[:, hb * 4:(hb + 1) * 4, :],
                                                                      in1=ps[6 + hb][:, :].rearrange("p (h v) -> p h v", h=4), op=ALU.add),
                              [psk(6 + hb), "S32"], ["S32"])
                    tr.op('pool', lambda e: e.tensor_copy(out=Sb[:], in_=S32[:]), ["S32"], ["Sb"])
                    if ti == NT - 1:
                        tr.dma('sp', hg_p.rearrange("h k v -> k h v"), S32[:], ["S32"], ["hg_p"], "hg_p")
                else:
                    for h in range(NH):
                        tr.dma('sp', s0f[:], st_in[:, h, :, :].rearrange("b k v -> k b v"), [], ["s0f"], "s0f")
                        tr.op('act', lambda e: e.activation(out=s0b[:], in_=s0f[:], func=AF.Copy), ["s0f"], ["s0b"])
                        qv = qems[:].rearrange("p b (c l) -> p b c l", c=SB)
                        for b_ in range(SB):
                            tr.op('pool', lambda e, b_=b_, h=h: e.tensor_copy(out=qems[:, b_, b_ * ST:(b_ + 1) * ST], in_=qeT[:, h, b_ * ST:(b_ + 1) * ST]),
                                  ["qeT"], ["qems"])
                        ob = 4 + h // 4
                        osl = ps[ob][:, (h % 4) * 128:(h % 4 + 1) * 128]
                        for b_ in range(SB):
                            tr.op('pe', lambda e, b_=b_, osl=osl: e.matmul(osl, lhsT=qems[:, b_, :], rhs=s0b[:, b_, :], start=(b_ == 0), stop=False),
                                  ["qems", "s0b"], [psk(ob)], inc=False)
                        tr.op('pe', lambda e, h=h, osl=osl: e.matmul(osl, lhsT=scm[:, h, :], rhs=vtok[:, h * 128:(h + 1) * 128], start=False, stop=True),
                              ["scm", "vtok"], [psk(ob)])
                        tr.op('dve', lambda e, h=h: e.tensor_tensor(out=kdm[:], in0=kd[:, h * 128:(h + 1) * 128].unsqueeze(1).broadcast_to([128, SB, 128]),
                                                                    in1=bm[:].unsqueeze(2).broadcast_to([128, SB, 128]), op=ALU.mult),
                              ["kd", "bm"], ["kdm"])
                        for b_ in range(SB):
                            bk = b_ // 4
                            tr.op('pe', lambda e, b_=b_, bk=bk, h=h: e.matmul(ps[bk][:, (b_ % 4) * 128:(b_ % 4 + 1) * 128], lhsT=kdm[:, b_, :],
                                                                             rhs=vtok[:, h * 128:(h + 1) * 128], start=True, stop=True),
                                  ["kdm", "vtok"], [psk(bk)], inc=(b_ % 4 == 3))
                        tr.op('dve', lambda e, h=h: e.tensor_tensor(out=snw[:], in0=s0f[:], in1=ebl[:, h, :].unsqueeze(2).broadcast_to([128, SB, 128]), op=ALU.mult),
                              ["s0f", "ebl"], ["s0f"])
                        for bk in range(4):
                            tr.op('dve', lambda e, bk=bk: e.tensor_tensor(out=snw[:, bk * 4:(bk + 1) * 4, :], in0=snw[:, bk * 4:(bk + 1) * 4, :],
                                                                          in1=ps[bk][:, :].rearrange("p (b v) -> p b v", b=4), op=ALU.add),
                                  [psk(bk), "s0f"], ["s0f"])
                        tr.dma('sp', hg_s[:, h, :, :].rearrange("b k v -> k b v"), snw[:], ["s0f"], ["hg_s"], "s0f")
                obanks = (4, 5) if samp else (2, 3)
                for pa, sl, pk in ps2(obanks):
                    tr.op('act', lambda e, pa=pa, sl=sl: e.activation(out=tB[:, sl], in_=pa, func=AF.Square), [pk], ["tB"])
                tr.op('dve', lambda e: e.tensor_reduce(out=rs8[:], in_=tB[:].rearrange("p (h v) -> p h v", h=NH), axis=AX.X, op=ALU.add), ["tB"], ["rs8"])
                tr.op('dve', lambda e: e.tensor_scalar(out=rs8[:], in0=rs8[:], scalar1=1.0 / 128, scalar2=EPS, op0=ALU.mult, op1=ALU.add), ["rs8"], ["rs8"])
                tr.op('act', lambda e: e.activation(out=rs8[:], in_=rs8[:], func=AF.Ln), ["rs8"], ["rs8"])
                tr.op('act', lambda e: e.activation(out=rs8[:], in_=rs8[:], func=AF.Exp, scale=-0.5), ["rs8"], ["rs8"])
                for hb, (pa, sl, pk) in enumerate(ps2(obanks)):
                    tr.op('dve', lambda e, pa=pa, sl=sl, hb=hb: e.tensor_tensor(out=tC[:, sl].rearrange("p (h v) -> p h v", h=4),
                                                                               in0=pa.rearrange("p (h v) -> p h v", h=4),
                                                                               in1=rs8[:, hb * 4:(hb + 1) * 4].unsqueeze(2).broadcast_to([128, 4, 128]), op=ALU.mult),
                          [pk, "rs8"], ["tC"])
                tr.op('pool', lambda e: e.tensor_tensor(out=tC[:].rearrange("p (h v) -> p h v", h=NH), in0=tC[:].rearrange("p (h v) -> p h v", h=NH),
                                                        in1=go_b[:].unsqueeze(1).broadcast_to([128, NH, 128]), op=ALU.mult), ["tC", "go_b"], ["tC"])
                tr.op('dve', lambda e: e.tensor_tensor(out=onb[:], in0=tC[:], in1=sgate[:], op=ALU.mult), ["tC", "sgate"], ["onb"])
                transpose_bf(onT[:], "onT", onb, "onb", 8, 6)
                for hb in range(2):
                    for c in range(8):
                        tr.op('pe', lambda e, hb=hb, c=c: e.matmul(ps[hb][:, :], lhsT=onT[:, c, :], rhs=w_out_sb[:, c, hb * 512:(hb + 1) * 512],
                                                                   start=(c == 0), stop=(c == 7)), ["onT", "w_out_sb"], [psk(hb)], inc=(c == 7))
                for pa, sl, pk in ps2((0, 1)):
                    tr.op('dve', lambda e, pa=pa, sl=sl: e.tensor_tensor(out=x_t[:, sl], in0=x_t[:, sl], in1=pa, op=ALU.add), [pk, xk], [xk])
                tr.dma('sp', hA[ti], x_t[:], [xk], [("hA", ti)], xk)

        def emit_debug(hsrc, keys):
            with ExitStack() as pd:
                t_ = sb(pd, "dbg", [128, D])
                for ti in range(NTT):
                    tr.dma('sp', t_[:], hsrc[ti], [(hsrc.tensor.name, ti)], ["dbg"], "dbg")
                    dst = y_p[ti * 128:(ti + 1) * 128, :] if ti < NT else y_s[:, :]
                    tr.dma('sp', dst, t_[:], ["dbg"], ["yout"], "dbgo")
            tr.final_wait('sp', ["dbgo"] + keys)

        if phases == 1:
            emit_debug(hA, ["hg_p", "s0f"])
            return nc, tr

        groups = [list(range(g0, min(g0 + 4, NT))) for g0 in range(0, NT, 4)] + [[NT]]

        def ffn_phase(hin, hout, gidx, experts, moe):
            tr.barrier()
            with ExitStack() as pf:
                hres = sb(pf, "hres", [128, 4, D]); xnTg = sb(pf, "xnTg", [128, 8, 512], BF16)
                xsb = sb(pf, "xsb", [128, D], BF16); ssf = sb(pf, "ssf", [128, 1]); rstf = sb(pf, "rstf", [128, 1])
                jk = sb(pf, "jk", [128, D])
                wgs = [sb(pf, "wgs%d" % i, [128, 8, 512], BF16) for i in range(2)]
                wus = [sb(pf, "wus%d" % i, [128, 8, 512], BF16) for i in range(2)]
                wds = [sb(pf, "wds%d" % i, [128, NFC, 512], BF16) for i in range(2)]
                hT = sb(pf, "hT", [128, NFC, 512], BF16)
                tm = [sb(pf, "tm%d" % i, [128, 512]) for i in range(2)]
                if moe:
                    xnT32 = sb(pf, "xnT32", [128, 8, 128]); wrs = sb(pf, "wrs", [128, 8, NE])
                    lg = sb(pf, "lg", [128, NE]); l2 = sb(pf, "l2", [128, NE]); eq1 = sb(pf, "eq1", [128, NE]); eq2 = sb(pf, "eq2", [128, NE])
                    m1 = sb(pf, "m1", [128, 1]); m2 = sb(pf, "m2", [128, 1]); g1 = sb(pf, "g1", [128, 1]); g2 = sb(pf, "g2", [128, 1])
                    comb = sb(pf, "comb", [128, 4, NE]); yst = sb(pf, "yst", [128, D])
                    tr.dma('sp', wrs[:], w_r.rearrange("(c p) e -> p c e", p=128), [], ["wrs"], "wrs")
                slab_n = 0
                wd_n = 0
                for grp in groups:
                    nt_ = len(grp); GT = nt_ * 128
                    for li, ti in enumerate(grp):
                        hk_ = ("hres", li)
                        tr.dma('sp', hres[:, li, :], hin[ti], [(hin.tensor.name, ti)], [hk_], "hres%d" % li)
                        rms_scale((jk, ssf, rstf), hres[:, li, :], hk_, D, jkey="jk")
                        tr.op('act', lambda e, li=li: e.activation(out=xsb[:], in_=hres[:, li, :], func=AF.Copy, scale=rstf[:, 0:1]), [hk_, "rstd"], ["xsb"])
                        transpose_bf(xnTg[:, :, li * 128:(li + 1) * 128], "xnTg", xsb, "xsb", 8, 6, gain=gvs[:, gidx, :], gkey="gvs")
                        if moe:
                            tr.op('act', lambda e, li=li: e.activation(out=jk[:], in_=hres[:, li, :], func=AF.Copy, scale=rstf[:, 0:1]), [hk_, "rstd"], ["jk"])
                            for c in range(8):
                                b = 6 + c // 4
                                tr.op('pe', lambda e, c=c, b=b: e.transpose(out=ps[b][:, (c % 4) * 128:(c % 4 + 1) * 128], in_=jk[:, c * 128:(c + 1) * 128], identity=idf[:]),
                                      ["jk", "idf"], [psk(b)], inc=(c % 4 == 3))
                            for hb in range(2):
                                tr.op('dve', lambda e, hb=hb: e.tensor_tensor(out=xnT32[:, hb * 4:(hb + 1) * 4, :], in0=ps[6 + hb][:, :].rearrange("p (c t) -> p c t", c=4),
                                                                              in1=gvs[:, gidx, hb * 4:(hb + 1) * 4].unsqueeze(2).broadcast_to([128, 4, 128]), op=ALU.mult),
                                      [psk(6 + hb), "gvs"], ["xnT32"])
                            for c in range(8):
                                tr.op('pe', lambda e, c=c: e.matmul(ps[6][:, 0:NE], lhsT=xnT32[:, c, :], rhs=wrs[:, c, :], start=(c == 0), stop=(c == 7)),
                                      ["xnT32", "wrs"], [psk(6)], inc=(c == 7))
                            tr.op('dve', lambda e: e.tensor_copy(out=lg[:], in_=ps[6][:, 0:NE]), [psk(6)], ["lg"])
                            tr.op('dve', lambda e: e.tensor_reduce(out=m1[:], in_=lg[:], axis=AX.X, op=ALU.max), ["lg"], ["m1"])
                            tr.op('dve', lambda e: e.tensor_scalar(out=eq1[:], in0=lg[:], scalar1=m1[:, 0:1], scalar2=None, op0=ALU.is_equal), ["lg", "m1"], ["eq1"])
                            tr.op('dve', lambda e: e.scalar_tensor_tensor(out=l2[:], in0=eq1[:], scalar=NEG, in1=lg[:], op0=ALU.mult, op1=ALU.add), ["eq1", "lg"], ["l2"])
                            tr.op('dve', lambda e: e.tensor_reduce(out=m2[:], in_=l2[:], axis=AX.X, op=ALU.max), ["l2"], ["m2"])
                            tr.op('dve', lambda e: e.tensor_scalar(out=eq2[:], in0=l2[:], scalar1=m2[:, 0:1], scalar2=None, op0=ALU.is_equal), ["l2", "m2"], ["eq2"])
                            tr.op('dve', lambda e: e.tensor_tensor(out=g2[:], in0=m2[:], in1=m1[:], op=ALU.subtract), ["m1", "m2"], ["g2"])
                            tr.op('act', lambda e: e.activation(out=g2[:], in_=g2[:], func=AF.Exp), ["g2"], ["g2"])
                            tr.op('dve', lambda e: e.tensor_scalar(out=g1[:], in0=g2[:], scalar1=1.0, scalar2=None, op0=ALU.add), ["g2"], ["g1"])
                            tr.op('dve', lambda e: e.reciprocal(out=g1[:], in_=g1[:]), ["g1"], ["g1"])
                            tr.op('dve', lambda e: e.tensor_tensor(out=g2[:], in0=g2[:], in1=g1[:], op=ALU.mult), ["g1", "g2"], ["g2"])
                            tr.op('dve', lambda e, li=li: e.tensor_scalar(out=comb[:, li, :], in0=eq1[:], scalar1=g1[:, 0:1], scalar2=None, op0=ALU.mult), ["eq1", "g1"], ["comb"])
                            tr.op('dve', lambda e, li=li: e.scalar_tensor_tensor(out=comb[:, li, :], in0=eq2[:], scalar=g2[:, 0:1], in1=comb[:, li, :], op0=ALU.mult, op1=ALU.add),
                                  ["eq2", "g2", "comb"], ["comb"])
                    for ex in range(experts):
                        wg_, wu_, wd_ = (w_eg[ex], w_eu[ex], w_ed[ex]) if moe else (w_fg, w_fu, w_fd)
                        for jb in range(6):
                            ncol = 512 if jb < 5 else 256
                            sl_ = slab_n % 2; slab_n += 1
                            tr.dma('sp', wgs[sl_][:, :, 0:ncol], wg_.rearrange("(c p) f -> p c f", p=128)[:, :, jb * 512: jb * 512 + ncol], [], ["wgs%d" % sl_], "wgs%d" % sl_)
                            tr.dma('sp', wus[sl_][:, :, 0:ncol], wu_.rearrange("(c p) f -> p c f", p=128)[:, :, jb * 512: jb * 512 + ncol], [], ["wus%d" % sl_], "wus%d" % sl_)
                            for jj in range(ncol // 128):
                                j = jb * 4 + jj
                                ba, bb = (0, 1) if j % 2 == 0 else (2, 3)
                                for dc in range(8):
                                    tr.op('pe', lambda e, dc=dc, jj=jj, ba=ba: e.matmul(ps[ba][:, 0:GT], lhsT=wgs[sl_][:, dc, jj * 128:(jj + 1) * 128], rhs=xnTg[:, dc, 0:GT],
                                                                                      start=(dc == 0), stop=(dc == 7)), ["wgs%d" % sl_, "xnTg"], [psk(ba)], inc=(dc == 7))
                                for dc in range(8):
                                    tr.op('pe', lambda e, dc=dc, jj=jj, bb=bb: e.matmul(ps[bb][:, 0:GT], lhsT=wus[sl_][:, dc, jj * 128:(jj + 1) * 128], rhs=xnTg[:, dc, 0:GT],
                                                                                      start=(dc == 0), stop=(dc == 7)), ["wus%d" % sl_, "xnTg"], [psk(bb)], inc=(dc == 7))
                                t_ = tm[j % 2]; tk = "tm%d" % (j % 2)
                                tr.op('act', lambda e, t_=t_, ba=ba: e.activation(out=t_[:, 0:GT], in_=ps[ba][:, 0:GT], func=AF.Exp, scale=-1.0), [psk(ba)], [tk])
                                tr.op('pool', lambda e, t_=t_: e.tensor_scalar(out=t_[:, 0:GT], in0=t_[:, 0:GT], scalar1=1.0, scalar2=None, op0=ALU.add), [tk], [tk])
                                tr.op('dve', lambda e, t_=t_: e.reciprocal(out=t_[:, 0:GT], in_=t_[:, 0:GT]), [tk], [tk])
                                tr.op('dve', lambda e, t_=t_, ba=ba: e.tensor_tensor(out=t_[:, 0:GT], in0=t_[:, 0:GT], in1=ps[ba][:, 0:GT], op=ALU.mult), [tk, psk(ba)], [tk])
                                tr.op('dve', lambda e, t_=t_, bb=bb, j=j: e.tensor_tensor(out=hT[:, j, 0:GT], in0=t_[:, 0:GT], in1=ps[bb][:, 0:GT], op=ALU.mult), [tk, psk(bb)], [("hT", j)])
                        for half in range(2):
                            ws_ = wd_n % 2; wd_n += 1
                            tr.dma('sp', wds[ws_][:, :, :], wd_.rearrange("(j p) d -> p j d", p=128)[:, :, half * 512:(half + 1) * 512],
                                   [], ["wds%d" % ws_], "wds%d" % ws_)
                            for li in range(nt_):
                                bk = 4 + (li % 2)
                                for j in range(NFC):
                                    tr.op('pe', lambda e, j=j, li=li, bk=bk: e.matmul(ps[bk][:, :], lhsT=hT[:, j, li * 128:(li + 1) * 128], rhs=wds[ws_][:, j, :],
                                                                                    start=(j == 0), stop=(j == NFC - 1)), [("hT", j), "wds%d" % ws_], [psk(bk)], inc=(j == NFC - 1))
                                hsl = hres[:, li, half * 512:(half + 1) * 512]
                                if moe:
                                    tr.op('dve', lambda e, hsl=hsl, bk=bk, li=li, ex=ex: e.scalar_tensor_tensor(out=hsl, in0=ps[bk][:, :], scalar=comb[:, li, ex:ex + 1], in1=hsl,
                                                                                                           op0=ALU.mult, op1=ALU.add), [psk(bk), "comb", ("hres", li)], [("hres", li)])
                                else:
                                    tr.op('dve', lambda e, hsl=hsl, bk=bk: e.tensor_tensor(out=hsl, in0=hsl, in1=ps[bk][:, :], op=ALU.add), [psk(bk), ("hres", li)], [("hres", li)])
                    for li, ti in enumerate(grp):
                        hk_ = ("hres", li)
                        if not moe:
                            tr.dma('sp', hout[ti], hres[:, li, :], [hk_], [(hout.tensor.name, ti)], "hres%d" % li)
                        else:
                            rms_scale((jk, ssf, rstf), hres[:, li, :], hk_, D, jkey="jk")
                            tr.op('dve', lambda e, li=li: e.scalar_tensor_tensor(out=yst[:], in0=hres[:, li, :], scalar=rstf[:, 0:1], in1=gfin_b[:], op0=ALU.mult, op1=ALU.mult),
                                  [hk_, "rstd", "gfin_b"], ["yst"])
                            dst = y_p[ti * 128:(ti + 1) * 128, :] if ti < NT else y_s[:, :]
                            tr.dma('sp', dst, yst[:], ["yst"], ["yout"], "yst")

        ffn_phase(hA, hB, 1, 1, False)
        if phases == 2:
            emit_debug(hB, ["hg_p", "s0f"])
            return nc, tr
        if phases == 24:
            ffn_phase(hB, None, 4, NE, True)
            tr.final_wait('sp', ["hg_p", "s0f", "yst"])
            return nc, tr

        tr.barrier()
        with ExitStack() as p3:
            wdkv_sb = sb(p3, "wdkv_sb", [128, 8, 320], BF16); wdq_sb = sb(p3, "wdq_sb", [128, 8, QL], BF16)
            wuq_sb = sb(p3, "wuq_sb", [128, 3, NH * 192], BF16); wukv_sb = sb(p3, "wukv_sb", [128, 2, NH * 256], BF16)
            woutb_sb = sb(p3, "woutb_sb", [128, 8, D], BF16); wukT = sb(p3, "wukT", [128, NH, 256], BF16)
            tr.dma('sp', wdkv_sb[:], w_dkv.rearrange("(c p) f -> p c f", p=128), [], ["wdkv_sb"], "wdkv_sb")
            tr.dma('sp', wdq_sb[:], w_dq.rearrange("(c p) f -> p c f", p=128), [], ["wdq_sb"], "wdq_sb")
            tr.dma('sp', wuq_sb[:], w_uq.rearrange("(c p) f -> p c f", p=128), [], ["wuq_sb"], "wuq_sb")
            for cc in range(2):
                tr.dma('sp', wukv_sb[:, cc, :], w_ukv[cc * 128:(cc + 1) * 128, :], [], ["wukv_sb"], "wukv_sb")
            for c in range(8):
                tr.dma('sp', woutb_sb[:, c, :], w_outb[c * 128:(c + 1) * 128, :], [], ["woutb_sb"], "woutb_sb")
            ptb = ps[6][:, :].bitcast(BF16)
            for h in range(NH):
                for cc in range(2):
                    tr.op('pe', lambda e, h=h, cc=cc: e.transpose(out=ptb[:, (h % 4) * 256 + cc * 128:(h % 4) * 256 + (cc + 1) * 128],
                                                                  in_=wukv_sb[:, cc, h * 256:h * 256 + 128], identity=idb[:]),
                          ["wukv_sb", "idb"], [psk(6)], inc=(cc == 1 and h % 4 == 3))
                if h % 4 == 3:
                    tr.op('dve', lambda e, h=h: e.tensor_copy(out=wukT[:, h - 3:h + 1, :], in_=ptb[:, :].rearrange("p (h c) -> p h c", h=4)), [psk(6)], ["wukT"])
            cT = sb(p3, "cT", [128, 2, T], BF16); ctok = sb(p3, "ctok", [128, NT, KVL], BF16); krT = sb(p3, "krT", [64, T], BF16)
            cTs = sb(p3, "cTs", [128, 2, 128], BF16); ctoks = sb(p3, "ctoks", [128, KVL], BF16); krTs = sb(p3, "krTs", [64, 128], BF16)
            ht = sb(p3, "ht", [128, D]); jk3 = sb(p3, "jk3", [128, D]); ss3 = sb(p3, "ss3", [128, 1]); rs3 = sb(p3, "rs3", [128, 1])
            xs3 = sb(p3, "xs3", [128, D], BF16); nkvT = sb(p3, "nkvT", [128, 8, 128], BF16); xnbT = sb(p3, "xnbT", [128, 8, 128], BF16)
            cf = sb(p3, "cf", [128, KVL]); cbf = sb(p3, "cbf", [128, KVL], BF16); krf = sb(p3, "krf", [128, QKR]); krb = sb(p3, "krb", [128, QKR], BF16)
            cosb = sb(p3, "cosb", [128, 32]); sinb = sb(p3, "sinb", [128, 32]); r1 = sb(p3, "r1", [128, NH, 32]); r2 = sb(p3, "r2", [128, NH, 32])
            cqb = sb(p3, "cqb", [128, QL], BF16); cqT = sb(p3, "cqT", [128, 3, 128], BF16)
            qnT = sb(p3, "qnT", [128, NH, 128], BF16); qaT = sb(p3, "qaT", [128, 2, NH, 128], BF16)
            qrf = sb(p3, "qrf", [128, NH, QKR]); qrb = sb(p3, "qrb", [128, NH, QKR], BF16); qrT = sb(p3, "qrT", [64, NH, 128], BF16)
            mst = sb(p3, "mst", [128, 1]); mnew = sb(p3, "mnew", [128, 1]); lst = sb(p3, "lst", [128, 1]); alp = sb(p3, "alp", [128, 1])
            nbias = sb(p3, "nbias", [128, 1]); rsum = sb(p3, "rsum", [128, 1]); mx = sb(p3, "mx", [128, 1])
            oacc = sb(p3, "oacc", [128, KVL]); pbf = sb(p3, "pbf", [128, 512], BF16); pT = sb(p3, "pT", [128, 4, 128], BF16)
            olat = sb(p3, "olat", [128, KVL], BF16); olatT = sb(p3, "olatT", [128, 2, NH, 128], BF16); oT = sb(p3, "oT", [128, NH, 128], BF16)
            qas = sb(p3, "qas", [128, 2, NH, ST], BF16); qrs = sb(p3, "qrs", [64, NH, ST], BF16)
            KC = 16
            gc = [sb(p3, "gc%d" % i, [128, KC, KVL], BF16) for i in range(2)]
            gr = [sb(p3, "gr%d" % i, [128, KC, QKR], BF16) for i in range(2)]
            gcT = sb(p3, "gcT", [128, 2, 512], BF16); grT = sb(p3, "grT", [64, 512], BF16)
            pti = sb(p3, "pti", [128, SB], I32); msk = sb(p3, "msk", [64, SB, 128])
            tr.dma('sp', pti[0:NPG, :], ptT[:, :], [], ["pti"], "pti")
            tr.dma('sp', msk[:], c_msk.rearrange("b r k -> r b k"), [], ["msk"], "msk")

            def attend_block(M, qk, N, mask, vts, first):
                sbk = attend_block.n % 2; attend_block.n += 1
                S = ps[sbk][0:M, 0:N]
                for i_, (l_, r_, ks_) in enumerate(qk):
                    tr.op('pe', lambda e, l_=l_, r_=r_, i_=i_: e.matmul(S, lhsT=l_, rhs=r_, start=(i_ == 0), stop=(i_ == len(qk) - 1)),
                          ks_, [psk(sbk)], inc=(i_ == len(qk) - 1))
                if mask is not None:
                    map_, c0, n_, mk = mask
                    tr.op('dve', lambda e: e.tensor_tensor(out=ps[sbk][0:M, c0:c0 + n_], in0=ps[sbk][0:M, c0:c0 + n_], in1=map_, op=ALU.add), [psk(sbk), mk], [psk(sbk)])
                tr.op('dve', lambda e: e.tensor_reduce(out=mx[0:M, :], in_=S, axis=AX.X, op=ALU.max), [psk(sbk)], ["mx"])
                if first:
                    tr.op('dve', lambda e: e.tensor_copy(out=mst[0:M, :], in_=mx[0:M, :]), ["mx"], ["mst"])
                else:
                    tr.op('dve', lambda e: e.tensor_tensor(out=mnew[0:M, :], in0=mst[0:M, :], in1=mx[0:M, :], op=ALU.max), ["mx", "mst"], ["mnew"])
                    tr.op('dve', lambda e: e.tensor_tensor(out=alp[0:M, :], in0=mst[0:M, :], in1=mnew[0:M, :], op=ALU.subtract), ["mnew", "mst"], ["alp"])
                    tr.op('act', lambda e: e.activation(out=alp[0:M, :], in_=alp[0:M, :], func=AF.Exp, scale=SM_SCALE), ["alp"], ["alp"])
                    tr.op('dve', lambda e: e.tensor_copy(out=mst[0:M, :], in_=mnew[0:M, :]), ["mnew"], ["mst"])
                tr.op('dve', lambda e: e.tensor_scalar(out=nbias[0:M, :], in0=mst[0:M, :], scalar1=-SM_SCALE, scalar2=None, op0=ALU.mult), ["mst"], ["nbias"])
                tr.op('act', lambda e: e.activation(out=pbf[0:M, 0:N], in_=S, func=AF.Exp, bias=nbias[0:M, 0:1], scale=SM_SCALE, accum_out=rsum[0:M, 0:1]),
                      [psk(sbk), "nbias"], ["pbf", "rsum"])
                if first:
                    tr.op('dve', lambda e: e.tensor_copy(out=lst[0:M, :], in_=rsum[0:M, :]), ["rsum"], ["lst"])
                else:
                    tr.op('dve', lambda e: e.scalar_tensor_tensor(out=lst[0:M, :], in0=lst[0:M, :], scalar=alp[0:M, 0:1], in1=rsum[0:M, :], op0=ALU.mult, op1=ALU.add),
                          ["lst", "alp", "rsum"], ["lst"])
                pTp = ps[2][:, :].bitcast(BF16)
                c0 = 0
                for kt, (v_, nk, vk) in enumerate(vts):
                    tr.op('pe', lambda e, kt=kt, nk=nk, c0=c0: e.transpose(out=pTp[0:nk, kt * 128: kt * 128 + M], in_=pbf[0:M, c0:c0 + nk], identity=idb[0:M, 0:M]),
                          ["pbf", "idb"], [psk(2)], inc=(kt == len(vts) - 1))
                    c0 += nk
                nv = len(vts)
                tr.op('act', lambda e: e.activation(out=pT[:, 0:nv, 0:M], in_=pTp[:, 0:nv * 128].rearrange("p (k m) -> p k m", k=nv)[:, :, 0:M], func=AF.Copy), [psk(2)], ["pT"])
                for kt, (v_, nk, vk) in enumerate(vts):
                    tr.op('pe', lambda e, kt=kt, nk=nk, v_=v_: e.matmul(ps[3][0:M, 0:KVL], lhsT=pT[0:nk, kt, 0:M], rhs=v_, start=(kt == 0), stop=(kt == nv - 1)),
                          ["pT", vk], [psk(3)], inc=(kt == nv - 1))
                if first:
                    tr.op('dve', lambda e: e.tensor_copy(out=oacc[0:M, :], in_=ps[3][0:M, 0:KVL]), [psk(3)], ["oacc"])
                else:
                    tr.op('dve', lambda e: e.scalar_tensor_tensor(out=oacc[0:M, :], in0=oacc[0:M, :], scalar=alp[0:M, 0:1], in1=ps[3][0:M, 0:KVL], op0=ALU.mult, op1=ALU.add),
                          ["oacc", "alp", psk(3)], ["oacc"])
            attend_block.n = 0

            def finish_rows(M):
                tr.op('dve', lambda e: e.reciprocal(out=lst[0:M, :], in_=lst[0:M, :]), ["lst"], ["lst"])
                tr.op('dve', lambda e: e.tensor_scalar(out=olat[0:M, :], in0=oacc[0:M, :], scalar1=lst[0:M, 0:1], scalar2=None, op0=ALU.mult), ["oacc", "lst"], ["olat"])

            for ti in range(NTT):
                samp = (ti == NT)
                tr.dma('sp', ht[:], hB[ti], [("hB", ti)], ["ht"], "ht")
                tr.dma('sp', cosb[:], c_cos[ti], [], ["cosb"], "cosb"); tr.dma('sp', sinb[:], c_sin[ti], [], ["sinb"], "sinb")
                rms_scale((jk3, ss3, rs3), ht[:], "ht", D, jkey="jk3")
                tr.op('act', lambda e: e.activation(out=xs3[:], in_=ht[:], func=AF.Copy, scale=rs3[:, 0:1]), ["ht", "rstd"], ["xs3"])
                ptb6 = ps[6][:, :].bitcast(BF16)
                for c in range(8):
                    tr.op('pe', lambda e, c=c: e.transpose(out=ptb6[:, c * 128:(c + 1) * 128], in_=xs3[:, c * 128:(c + 1) * 128], identity=idb[:]), ["xs3", "idb"], [psk(6)], inc=(c == 7))
                pv6 = ptb6[:, :].rearrange("p (c t) -> p c t", c=8)
                tr.op('dve', lambda e: e.tensor_tensor(out=nkvT[:], in0=pv6, in1=gvs[:, 2, :].unsqueeze(2).broadcast_to([128, 8, 128]), op=ALU.mult), [psk(6), "gvs"], ["nkvT"])
                tr.op('dve', lambda e: e.tensor_tensor(out=xnbT[:], in0=pv6, in1=gvs[:, 3, :].unsqueeze(2).broadcast_to([128, 8, 128]), op=ALU.mult), [psk(6), "gvs"], ["xnbT"])
                for dc in range(8):
                    tr.op('pe', lambda e, dc=dc: e.matmul(ps[4][:, 0:320], lhsT=nkvT[:, dc, :], rhs=wdkv_sb[:, dc, :], start=(dc == 0), stop=(dc == 7)),
                          ["nkvT", "wdkv_sb"], [psk(4)], inc=(dc == 7))
                rms_scale((jk3, ss3, rs3), ps[4][:, 0:KVL], psk(4), KVL, jkey="jk3")
                tr.op('dve', lambda e: e.scalar_tensor_tensor(out=cf[:], in0=ps[4][:, 0:KVL], scalar=rs3[:, 0:1], in1=gkv_b[:], op0=ALU.mult, op1=ALU.mult),
                      [psk(4), "rstd", "gkv_b"], ["cf"])
                tr.dma('sp', (ckv_s[:, :] if samp else ckv_p[ti * 128:(ti + 1) * 128, :]), cf[:], ["cf"], ["ckvout"], "cf")
                ctk = ctoks[:] if samp else ctok[:, ti, :]
                ctkey = "ctoks" if samp else ("ctok", ti)
                tr.op('act', lambda e: e.activation(out=ctk, in_=cf[:], func=AF.Copy), ["cf"], [ctkey])
                x1 = ps[4][:, 256:288]; x2 = ps[4][:, 288:320]
                tr.op('dve', lambda e: e.tensor_tensor(out=krf[:, 0:32], in0=x1, in1=cosb[:], op=ALU.mult), [psk(4), "cosb"], ["krf"])
                tr.op('dve', lambda e: e.tensor_tensor(out=r1[:, 0, :], in0=x2, in1=sinb[:], op=ALU.mult), [psk(4), "sinb"], ["r1"])
                tr.op('dve', lambda e: e.tensor_tensor(out=krf[:, 0:32], in0=krf[:, 0:32], in1=r1[:, 0, :], op=ALU.subtract), ["krf", "r1"], ["krf"])
                tr.op('dve', lambda e: e.tensor_tensor(out=krf[:, 32:64], in0=x2, in1=cosb[:], op=ALU.mult), [psk(4), "cosb"], ["krf"])
                tr.op('dve', lambda e: e.tensor_tensor(out=r1[:, 0, :], in0=x1, in1=sinb[:], op=ALU.mult), [psk(4), "sinb"], ["r1"])
                tr.op('dve', lambda e: e.tensor_tensor(out=krf[:, 32:64], in0=krf[:, 32:64], in1=r1[:, 0, :], op=ALU.add), ["krf", "r1"], ["krf"])
                tr.dma('sp', (kr_s[:, :] if samp else kr_p[ti * 128:(ti + 1) * 128, :]), krf[:], ["krf"], ["krout"], "krf")
                tr.op('act', lambda e: e.activation(out=krb[:], in_=krf[:], func=AF.Copy), ["krf"], ["krb"])
                for cc in range(2):
                    tr.op('pe', lambda e, cc=cc: e.transpose(out=ptb6[:, cc * 128:(cc + 1) * 128], in_=ctk[:, cc * 128:(cc + 1) * 128], identity=idb[:]), [ctkey, "idb"], [psk(6)], inc=False)
                tr.op('pe', lambda e: e.transpose(out=ptb6[0:64, 256:384], in_=krb[:, :], identity=idb[:]), ["krb", "idb"], [psk(6)])
                cTd = cTs[:] if samp else cT[:, :, ti * 128:(ti + 1) * 128]
                cTk = "cTs" if samp else ("cT", ti)
                krTd = krTs[:] if samp else krT[:, ti * 128:(ti + 1) * 128]
                tr.op('dve', lambda e: e.tensor_copy(out=cTd, in_=ptb6[:, 0:256].rearrange("p (c t) -> p c t", c=2)), [psk(6)], [cTk])
                tr.op('dve', lambda e: e.tensor_copy(out=krTd, in_=ptb6[0:64, 256:384]), [psk(6)], [cTk])
                for dc in range(8):
                    tr.op('pe', lambda e, dc=dc: e.matmul(ps[5][:, 0:QL], lhsT=xnbT[:, dc, :], rhs=wdq_sb[:, dc, :], start=(dc == 0), stop=(dc == 7)),
                          ["xnbT", "wdq_sb"], [psk(5)], inc=(dc == 7))
                rms_scale((jk3, ss3, rs3), ps[5][:, 0:QL], psk(5), QL, jkey="jk3")
                tr.op('act', lambda e: e.activation(out=cqb[:], in_=ps[5][:, 0:QL], func=AF.Copy, scale=rs3[:, 0:1]), [psk(5), "rstd"], ["cqb"])
                ptb7 = ps[7][:, :].bitcast(BF16)
                for c in range(3):
                    tr.op('pe', lambda e, c=c: e.transpose(out=ptb7[:, c * 128:(c + 1) * 128], in_=cqb[:, c * 128:(c + 1) * 128], identity=idb[:]), ["cqb", "idb"], [psk(7)], inc=(c == 2))
                tr.op('dve', lambda e: e.tensor_tensor(out=cqT[:], in0=ptb7[:, 0:384].rearrange("p (c t) -> p c t", c=3), in1=gqs[:].unsqueeze(2).broadcast_to([128, 3, 128]), op=ALU.mult),
                      [psk(7), "gqs"], ["cqT"])
                for h in range(NH):
                    b = 4 + h // 4
                    for qc in range(3):
                        tr.op('pe', lambda e, h=h, qc=qc, b=b: e.matmul(ps[b][:, (h % 4) * 128:(h % 4 + 1) * 128], lhsT=wuq_sb[:, qc, h * 192:h * 192 + 128], rhs=cqT[:, qc, :],
                                                                        start=(qc == 0), stop=(qc == 2)), ["wuq_sb", "cqT"], [psk(b)], inc=(qc == 2 and h % 4 == 3))
                for hb in range(2):
                    tr.op('act', lambda e, hb=hb: e.activation(out=qnT[:, hb * 4:(hb + 1) * 4, :], in_=ps[4 + hb][:, :].rearrange("p (h t) -> p h t", h=4), func=AF.Copy), [psk(4 + hb)], ["qnT"])
                wr_ = wuq_sb[:, :, :].rearrange("p c (h x) -> p c h x", h=NH)
                for qc in range(3):
                    tr.op('pe', lambda e, qc=qc: e.matmul(ps[6][:, :].rearrange("p (h x) -> p h x", h=NH), lhsT=cqT[:, qc, :], rhs=wr_[:, qc, :, 128:192], start=(qc == 0), stop=(qc == 2)),
                          ["wuq_sb", "cqT"], [psk(6)], inc=(qc == 2))
                q3 = ps[6][:, :].rearrange("p (h x) -> p h x", h=NH)
                cb3 = cosb[:].unsqueeze(1).broadcast_to([128, NH, 32]); sb3 = sinb[:].unsqueeze(1).broadcast_to([128, NH, 32])
                tr.op('dve', lambda e: e.tensor_tensor(out=qrf[:, :, 0:32], in0=q3[:, :, 0:32], in1=cb3, op=ALU.mult), [psk(6), "cosb"], ["qrf"])
                tr.op('dve', lambda e: e.tensor_tensor(out=r1[:], in0=q3[:, :, 32:64], in1=sb3, op=ALU.mult), [psk(6), "sinb"], ["r1"])
                tr.op('dve', lambda e: e.tensor_tensor(out=qrf[:, :, 0:32], in0=qrf[:, :, 0:32], in1=r1[:], op=ALU.subtract), ["qrf", "r1"], ["qrf"])
                tr.op('dve', lambda e: e.tensor_tensor(out=qrf[:, :, 32:64], in0=q3[:, :, 32:64], in1=cb3, op=ALU.mult), [psk(6), "cosb"], ["qrf"])
                tr.op('dve', lambda e: e.tensor_tensor(out=r2[:], in0=q3[:, :, 0:32], in1=sb3, op=ALU.mult), [psk(6), "sinb"], ["r2"])
                tr.op('dve', lambda e: e.tensor_tensor(out=qrf[:, :, 32:64], in0=qrf[:, :, 32:64], in1=r2[:], op=ALU.add), ["qrf", "r2"], ["qrf"])
                tr.op('act', lambda e: e.activation(out=qrb[:], in_=qrf[:], func=AF.Copy), ["qrf"], ["qrb"])
                for h in range(NH):
                    tr.op('pe', lambda e, h=h: e.transpose(out=ptb7[0:64, h * 128:(h + 1) * 128], in_=qrb[:, h, :], identity=idb[:]), ["qrb", "idb"], [psk(7)], inc=(h == NH - 1))
                tr.op('dve', lambda e: e.tensor_copy(out=qrT[:], in_=ptb7[0:64, :].rearrange("p (h t) -> p h t", h=NH)), [psk(7)], ["qrT"])
                for h in range(NH):
                    for cc in range(2):
                        i_ = h * 2 + cc; b = 4 + i_ // 4
                        tr.op('pe', lambda e, h=h, cc=cc, i_=i_, b=b: e.matmul(ps[b][:, (i_ % 4) * 128:(i_ % 4 + 1) * 128], lhsT=wukT[:, h, cc * 128:(cc + 1) * 128], rhs=qnT[:, h, :],
                                                                              start=True, stop=True), ["wukT", "qnT"], [psk(b)], inc=(i_ % 4 == 3))
                for b in range(4):
                    tr.op('act', lambda e, b=b: e.activation(out=qaT[:, :, 2 * b:2 * b + 2, :].rearrange("p c h t -> p h c t"),
                                                             in_=ps[4 + b][:, :].rearrange("p (h c t) -> p h c t", h=2, c=2), func=AF.Copy), [psk(4 + b)], ["qaT"])
                if not samp:
                    for h in range(NH):
                        nkb = ti // 4 + 1
                        for kb in range(nkb):
                            t0 = kb * 4; t1_ = min(t0 + 4, ti + 1); N = (t1_ - t0) * 128
                            kkeys = [("cT", t_) for t_ in range(t0, t1_)]
                            qk = [(qaT[:, 0, h, :], cT[:, 0, t0 * 128:t1_ * 128], ["qaT"] + kkeys), (qaT[:, 1, h, :], cT[:, 1, t0 * 128:t1_ * 128], ["qaT"] + kkeys),
                                  (qrT[:, h, :], krT[:, t0 * 128:t1_ * 128], ["qrT"] + kkeys)]
                            mask = (cb[:], (ti - t0) * 128, 128, "cb") if kb == nkb - 1 else None
                            vts = [(ctok[:, t_, :], 128, ("ctok", t_)) for t_ in range(t0, t1_)]
                            attend_block(128, qk, N, mask, vts, kb == 0)
                        finish_rows(128)
                        for cc in range(2):
                            tr.op('pe', lambda e, cc=cc: e.transpose(out=ptb7[:, cc * 128:(cc + 1) * 128], in_=olat[:, cc * 128:(cc + 1) * 128], identity=idb[:]), ["olat", "idb"], [psk(7)], inc=(cc == 1))
                        tr.op('act', lambda e, h=h: e.activation(out=olatT[:, :, h, :], in_=ptb7[:, 0:256].rearrange("p (c t) -> p c t", c=2), func=AF.Copy), [psk(7)], ["olatT"])
                else:
                    npg = NPG
                    gn = 0
                    for b_ in range(SB):
                        tr.op('pool', lambda e, b_=b_: e.tensor_copy(out=qas[:], in_=qaT[:, :, :, b_ * ST:(b_ + 1) * ST]), ["qaT"], ["qas"])
                        tr.op('pool', lambda e, b_=b_: e.tensor_copy(out=qrs[:], in_=qrT[:, :, b_ * ST:(b_ + 1) * ST]), ["qrT"], ["qrs"])
                        qa0 = qas[:, 0, :, :].rearrange("p h t -> p (h t)"); qa1 = qas[:, 1, :, :].rearrange("p h t -> p (h t)")
                        qr_ = qrs[:, :, :].rearrange("p h t -> p (h t)")
                        first = True
                        for k0 in range(0, PAGE, KC):
                            gi = gn % 2; gn += 1
                            tr.dma('pool', gc[gi][0:npg].rearrange("p k c -> p (k c)"), ckv.rearrange("n k c -> n (k c)"), ["pti"], ["gc%d" % gi], "gc%d" % gi,
                                   indirect=(pti[0:npg, b_:b_ + 1], k0 * KVL))
                            tr.dma('pool', gr[gi][0:npg].rearrange("p k c -> p (k c)"), ckr.rearrange("n k c -> n (k c)"), ["pti"], ["gr%d" % gi], "gr%d" % gi,
                                   indirect=(pti[0:npg, b_:b_ + 1], k0 * QKR))
                            for r0 in range(0, KC, 4):
                                pg = ps[4 + (r0 // 4) % 2][:, :].bitcast(BF16); pgk = psk(4 + (r0 // 4) % 2)
                                for rr in range(4):
                                    for cc in range(2):
                                        tr.op('pe', lambda e, rr=rr, cc=cc, r0=r0, pg=pg: e.transpose(out=pg[:, cc * 512 + rr * npg: cc * 512 + (rr + 1) * npg], in_=gc[gi][0:npg, r0 + rr, cc * 128:(cc + 1) * 128],
                                                                                                   identity=idb[0:npg, 0:npg]), ["gc%d" % gi, "idb"], [pgk], inc=(rr == 3 and cc == 1))
                                tr.op('dve', lambda e, pg=pg: e.tensor_copy(out=gcT[:, :, 0:4 * npg], in_=pg[:, :].rearrange("p (c n) -> p c n", c=2)[:, :, 0:4 * npg]), [pgk], ["gcT"])
                                pr = ps[6][:, :].bitcast(BF16)
                                for rr in range(4):
                                    tr.op('pe', lambda e, rr=rr, r0=r0: e.transpose(out=pr[0:64, rr * npg:(rr + 1) * npg], in_=gr[gi][0:npg, r0 + rr, :], identity=idb[0:npg, 0:npg]),
                                          ["gr%d" % gi, "idb"], [psk(6)], inc=(rr == 3))
                                tr.op('act', lambda e: e.activation(out=grT[:, 0:4 * npg], in_=pr[0:64, 0:4 * npg], func=AF.Copy), [psk(6)], ["grT"])
                                qk = [(qa0, gcT[:, 0, 0:4 * npg], ["qas", "gcT"]), (qa1, gcT[:, 1, 0:4 * npg], ["qas", "gcT"]), (qr_, grT[:, 0:4 * npg], ["qrs", "grT"])]
                                vts = [(gc[gi][0:npg, r0 + rr, :], npg, "gc%d" % gi) for rr in range(4)]
                                attend_block(64, qk, 4 * npg, None, vts, first)
                                first = False
                        qk = [(qa0, cTs[:, 0, :], ["qas", "cTs"]), (qa1, cTs[:, 1, :], ["qas", "cTs"]), (qr_, krTs[:, :], ["qrs", "cTs"])]
                        attend_block(64, qk, 128, (msk[:, b_, :], 0, 128, "msk"), [(ctoks[:, :], 128, "ctoks")], False)
                        finish_rows(64)
                        for cc in range(2):
                            tr.op('pe', lambda e, cc=cc: e.transpose(out=ptb7[:, cc * 64:(cc + 1) * 64], in_=olat[0:64, cc * 128:(cc + 1) * 128], identity=idb[0:64, 0:64]), ["olat", "idb"], [psk(7)], inc=(cc == 1))
                        tr.op('act', lambda e, b_=b_: e.activation(out=olatT[:, :, :, b_ * ST:(b_ + 1) * ST], in_=ptb7[:, 0:128].rearrange("p (c h t) -> p c h t", c=2, h=NH), func=AF.Copy),
                              [psk(7)], ["olatT"])
                for h in range(NH):
                    b = 4 + h // 4
                    for cc in range(2):
                        tr.op('pe', lambda e, h=h, cc=cc, b=b: e.matmul(ps[b][:, (h % 4) * 128:(h % 4 + 1) * 128], lhsT=wukv_sb[:, cc, h * 256 + 128:h * 256 + 256], rhs=olatT[:, cc, h, :],
                                                                        start=(cc == 0), stop=(cc == 1)), ["wukv_sb", "olatT"], [psk(b)], inc=(cc == 1 and h % 4 == 3))
                for hb in range(2):
                    tr.op('act', lambda e, hb=hb: e.activation(out=oT[:, hb * 4:(hb + 1) * 4, :], in_=ps[4 + hb][:, :].rearrange("p (h t) -> p h t", h=4), func=AF.Copy), [psk(4 + hb)], ["oT"])
                for hb in range(2):
                    for h in range(NH):
                        tr.op('pe', lambda e, hb=hb, h=h: e.matmul(ps[4 + hb][:, :], lhsT=oT[:, h, :], rhs=woutb_sb[:, h, hb * 512:(hb + 1) * 512], start=(h == 0), stop=(h == NH - 1)),
                              ["oT", "woutb_sb"], [psk(4 + hb)], inc=(h == NH - 1))
                for hb in range(2):
                    tr.op('dve', lambda e, hb=hb: e.tensor_tensor(out=ht[:, hb * 512:(hb + 1) * 512], in0=ht[:, hb * 512:(hb + 1) * 512], in1=ps[4 + hb][:, :], op=ALU.add), [psk(4 + hb), "ht"], ["ht"])
                tr.dma('sp', hA[ti], ht[:], ["ht"], [("hA", ti)], "ht")

        outk = ["hg_p", "s0f", "cf", "krf"]
        if phases == 3:
            emit_debug(hA, outk)
            return nc, tr
        ffn_phase(hA, None, 4, NE, True)
        tr.final_wait('sp', outk + ["yst"])
    return nc, tr


def _consts(T, NPG):
    NT = T // 128
    bf = ml_dtypes.bfloat16
    c = {}
    c["c_idf"] = np.eye(128, dtype=np.float32)
    c["c_idb"] = np.eye(128, dtype=np.float32).astype(bf)
    s = np.arange(128)[:, None]; t = np.arange(128)[None, :]
    tri = np.zeros((2, 128, 128), np.float32); up = np.zeros((2, 128, 128), np.float32)
    for a, L in enumerate((64, 8)):
        same = (s // L) == (t // L)
        tri[a] = (same & (s <= t)).astype(np.float32)
        up[a] = (same & (s > t)).astype(np.float32)
    c["c_tri"] = tri; c["c_up"] = up
    c["c_cb"] = np.where(t <= s, 0.0, NEG).astype(np.float32)
    r = np.arange(64)[:, None]; kk = np.arange(8)[None, :]
    c["c_cbs"] = np.where(kk <= (r % 8), 0.0, NEG).astype(np.float32)
    rr_ = np.arange(64)[None, :, None]; kk_ = np.arange(128)[None, None, :]; bb_ = np.arange(SB)[:, None, None]
    c["c_msk"] = np.where(((kk_ // ST) == bb_) & ((kk_ % ST) <= (rr_ % ST)), 0.0, NEG).astype(np.float32)
    c["c_bm"] = ((np.arange(128)[:, None] // ST) == np.arange(SB)[None, :]).astype(np.float32)
    half = 32
    inv = (10000.0 ** (-np.arange(half, dtype=np.float32) / half)).astype(np.float32)
    pos = np.zeros((NT + 1, 128), np.float32)
    pos[:NT] = np.arange(T, dtype=np.float32).reshape(NT, 128)
    pos[NT] = (NPG * PAGE + (np.arange(128) % ST)).astype(np.float32)
    ang = pos[:, :, None] * inv[None, None, :]
    c["c_cos"] = np.cos(ang).astype(np.float32); c["c_sin"] = np.sin(ang).astype(np.float32)
    return c


def make_in_maps(inp, T, NPG):
    f = lambda a: np.ascontiguousarray(np.asarray(a))
    cst = _consts(T, NPG)
    gl = f(inp["gamma_lb"])
    shared = {
        "ckv": f(inp["cache_ckv"]), "ckr": f(inp["cache_krope"]),
        "w_in": f(inp["w_in_a"][0]), "w_outa": f(inp["w_out_a"][0]),
        "glb": gl, "glbT": f(gl.reshape(2, NH, 128).transpose(2, 0, 1)),
        "gv": f(np.stack([inp["g_mix_a"][0], inp["g_ffn"][0], inp["g_kv_in"], inp["g_mix_b"][0], inp["g_ffn"][1]], 0).reshape(5, 8, 128).transpose(2, 0, 1)),
        "gq": f(np.asarray(inp["g_q"][0]).reshape(3, 128).T),
        "g_o": f(inp["g_onorm_a"][0]), "g_kv": f(inp["g_kv"]), "g_fin": f(inp["g_final"]),
        "w_dkv": f(inp["w_dkv"]), "w_ukv": f(inp["w_ukv"]), "w_dq": f(inp["w_dq"][0]), "w_uq": f(inp["w_uq"][0]),
        "w_outb": f(inp["w_out_b"][0]),
        "w_fg": f(inp["w_ff_gate"][0]), "w_fu": f(inp["w_ff_up"][0]), "w_fd": f(inp["w_ff_down"][0]),
        "w_r": f(inp["w_router"][0]), "w_eg": f(inp["w_e_gate"][0]), "w_eu": f(inp["w_e_up"][0]), "w_ed": f(inp["w_e_down"][0]),
    }
    shared.update(cst)
    maps = []
    xp = np.asarray(inp["x_prompt"]); xs_ = np.asarray(inp["x_sample"]); st = np.asarray(inp["state_hgrn"]); pt = np.asarray(inp["page_table"])
    for c in range(NCORES):
        m = dict(shared)
        m["x_p"] = f(xp[c]); m["x_s"] = f(xs_[c * SB:(c + 1) * SB].reshape(SB * ST, D))
        m["st_in"] = f(st[0, c * SB:(c + 1) * SB]); m["ptT"] = f(pt[c * SB:(c + 1) * SB].T.astype(np.int32))
        maps.append(m)
    return maps


def kernel(**inputs):
    T = int(np.asarray(inputs["x_prompt"]).shape[1])
    NPG = int(np.asarray(inputs["page_table"]).shape[1])
    NPOOL = int(np.asarray(inputs["cache_ckv"]).shape[0])
    nc, _ = build(T, NPG, NPOOL)
    maps = make_in_maps(inputs, T, NPG)
    res = run_bass_kernel_spmd(nc, maps, core_ids=list(range(NCORES)))
    r = res.results
    f32 = np.float32
    y_p = np.stack([r[c]["y_p"] for c in range(NCORES)]).astype(f32)
    y_s = np.concatenate([r[c]["y_s"].reshape(SB, ST, D) for c in range(NCORES)], 0).astype(f32)
    ckv_p = np.stack([r[c]["ckv_p"] for c in range(NCORES)]).astype(f32)
    kr_p = np.stack([r[c]["kr_p"] for c in range(NCORES)]).astype(f32)
    ckv_s = np.concatenate([r[c]["ckv_s"].reshape(SB, ST, KVL) for c in range(NCORES)], 0).astype(f32)
    kr_s = np.concatenate([r[c]["kr_s"].reshape(SB, ST, QKR) for c in range(NCORES)], 0).astype(f32)
    hg_p = np.stack([r[c]["hg_p"] for c in range(NCORES)])[None].astype(f32)
    hg_s = np.concatenate([r[c]["hg_s"] for c in range(NCORES)], 0)[None].astype(f32)
    return (y_p, y_s, ckv_p, kr_p, ckv_s, kr_s, hg_p, hg_s)
```

```python
import numpy as np
import ml_dtypes
from contextlib import ExitStack
import concourse.bass as bass
import concourse.mybir as mybir
from concourse.bass_utils import run_bass_kernel_spmd

F32 = mybir.dt.float32
BF16 = mybir.dt.bfloat16
I32 = mybir.dt.int32
AF = mybir.ActivationFunctionType
ALU = mybir.AluOpType
AX = mybir.AxisListType

D = 1024
NH = 8
DFF = 2816
NFC = DFF // 128
NE = 8
KVL = 256
QKR = 64
QL = 384
EPS = 1e-6
SM_SCALE = (128 + 64) ** -0.5
NEG = -1e30
NCORES = 8
SB = 16
ST = 8
PAGE = 128


class Tr:
    def __init__(self, nc):
        self.nc = nc
        self.eng = {'pe': nc.tensor, 'act': nc.scalar, 'dve': nc.vector, 'pool': nc.gpsimd, 'sp': nc.sync}
        self.sem = {e: nc.alloc_semaphore("sem_" + e) for e in self.eng}
        self.cnt = {e: 0 for e in self.eng}
        self.pending = {e: False for e in self.eng}
        self.W = {}
        self.R = {}
        self.waited = {e: {} for e in self.eng}
        self.dsem = {}
        self.nwait = 0
        self.nins = 0

    def _deps(self, reads, writes):
        deps = {}

        def add(sem, val):
            if deps.get(sem, 0) < val:
                deps[sem] = val
        for r in reads:
            w = self.W.get(r)
            if w:
                add(*w)
        for w_ in writes:
            w = self.W.get(w_)
            if w:
                add(*w)
            for s, v in self.R.get(w_, {}).items():
                add(s, v)
        return deps

    def _wait(self, e, deps):
        wd = self.waited[e]
        for sem, val in deps.items():
            if e == 'pe' and sem is self.sem['pe']:
                continue
            if wd.get(sem, 0) >= val:
                continue
            self.eng[e].wait_ge(sem, val)
            wd[sem] = val
            self.nwait += 1

    def _record(self, ev, reads, writes):
        for r in reads:
            d = self.R.setdefault(r, {})
            if d.get(ev[0], 0) < ev[1]:
                d[ev[0]] = ev[1]
        for w in writes:
            self.W[w] = ev
            self.R[w] = {}

    def op(self, e, fn, reads=(), writes=(), inc=True):
        self._wait(e, self._deps(reads, writes))
        ins = fn(self.eng[e])
        self.nins += 1
        if inc:
            self.cnt[e] += 1
            ins.then_inc(self.sem[e], 1)
            ev = (self.sem[e], self.cnt[e])
        else:
            ev = (self.sem[e], self.cnt[e] + 1)
        self._record(ev, reads, writes)
        return ins

    def dma(self, e, out, in_, reads, writes, key, indirect=None):
        self._wait(e, self._deps(reads, writes))
        if key not in self.dsem:
            self.dsem[key] = [self.nc.alloc_semaphore("dsem_%d" % len(self.dsem)), 0]
        ds = self.dsem[key]
        if indirect is None:
            ins = self.eng[e].dma_start(out=out, in_=in_)
        else:
            idx_ap, eoff = indirect
            ins = self.eng[e].indirect_dma_start(
                out=out, out_offset=None, in_=in_,
                in_offset=bass.IndirectOffsetOnAxis(ap=idx_ap, axis=0), element_offset=eoff)
        ds[1] += 16
        ins.then_inc(ds[0], 16)
        self.nins += 1
        self._record((ds[0], ds[1]), reads, writes)

    def barrier(self):
        deps = {self.sem[e]: self.cnt[e] for e in self.eng if self.cnt[e] > 0}
        for k, ds in self.dsem.items():
            if ds[1] > 0:
                deps[ds[0]] = ds[1]
        for e in self.eng:
            self._wait(e, deps)

    def final_wait(self, e, keys):
        deps = {}
        for k in keys:
            ds = self.dsem[k]
            deps[ds[0]] = ds[1]
        self._wait(e, deps)


def build(T, NPG, NPOOL, phases=4):
    NT = T // 128
    NTT = NT + 1
    nc = bass.Bass("TRN2", target_bir_lowering=False)
    tr = Tr(nc)

    def din(name, shape, dt=F32):
        return nc.dram_tensor(name, list(shape), dt, kind="ExternalInput").ap()

    def dout(name, shape, dt=F32):
        return nc.dram_tensor(name, list(shape), dt, kind="ExternalOutput").ap()

    x_p = din("x_p", [T, D]); x_s = din("x_s", [128, D])
    ckv = din("ckv", [NPOOL, PAGE, KVL]); ckr = din("ckr", [NPOOL, PAGE, QKR])
    st_in = din("st_in", [SB, NH, 128, 128]); ptT = din("ptT", [NPG, SB], I32)
    w_in = din("w_in", [D, 4 * D]); w_outa = din("w_outa", [D, D])
    glb = din("glb", [2, D])
    glbT = din("glbT", [128, 2, NH])
    gv = din("gv", [128, 5, 8])
    gq = din("gq", [128, 3])
    g_o = din("g_o", [128]); g_kv = din("g_kv", [KVL]); g_fin = din("g_fin", [D])
    w_dkv = din("w_dkv", [D, KVL + QKR]); w_ukv = din("w_ukv", [KVL, NH * 256])
    w_dq = din("w_dq", [D, QL]); w_uq = din("w_uq", [QL, NH * 192]); w_outb = din("w_outb", [D, D])
    w_fg = din("w_fg", [D, DFF]); w_fu = din("w_fu", [D, DFF]); w_fd = din("w_fd", [DFF, D])
    w_r = din("w_r", [D, NE])
    w_eg = din("w_eg", [NE, D, DFF]); w_eu = din("w_eu", [NE, D, DFF]); w_ed = din("w_ed", [NE, DFF, D])
    c_idf = din("c_idf", [128, 128]); c_idb = din("c_idb", [128, 128], BF16)
    c_tri = din("c_tri", [2, 128, 128]); c_up = din("c_up", [2, 128, 128])
    c_cb = din("c_cb", [128, 128]); c_cbs = din("c_cbs", [64, 8]); c_bm = din("c_bm", [128, SB])
    c_msk = din("c_msk", [SB, 64, 128])
    c_cos = din("c_cos", [NTT, 128, 32]); c_sin = din("c_sin", [NTT, 128, 32])

    y_p = dout("y_p", [T, D]); y_s = dout("y_s", [128, D])
    ckv_p = dout("ckv_p", [T, KVL]); kr_p = dout("kr_p", [T, QKR])
    ckv_s = dout("ckv_s", [128, KVL]); kr_s = dout("kr_s", [128, QKR])
    hg_p = dout("hg_p", [NH, 128, 128]); hg_s = dout("hg_s", [SB, NH, 128, 128])
    hA = nc.dram_tensor("hA", [NTT, 128, D], F32, kind="Internal").ap()
    hB = nc.dram_tensor("hB", [NTT, 128, D], F32, kind="Internal").ap()

    def xrows(i):
        return x_p[i * 128:(i + 1) * 128, :] if i < NT else x_s[:, :]

    ps = [nc.alloc_psum_tensor("ps%d" % i, [128, 512], F32) for i in range(8)]

    def psk(i):
        return ("ps", i)

    with ExitStack() as g:
        used_names = {}

        def sb(stack, name, shape, dt=F32):
            n = used_names.get(name, 0)
            used_names[name] = n + 1
            if n:
                name = "%s_v%d" % (name, n)
            return stack.enter_context(nc.sbuf_tensor(name, list(shape), dt))

        idf = sb(g, "idf", [128, 128]); idb = sb(g, "idb", [128, 128], BF16)
        tri = sb(g, "tri", [128, 2, 128]); up = sb(g, "up", [128, 2, 128])
        trib = sb(g, "trib", [128, 2, 128], BF16)
        cb = sb(g, "cb", [128, 128]); cbs = sb(g, "cbs", [64, 8]); bm = sb(g, "bm", [128, SB])
        gvs = sb(g, "gvs", [128, 5, 8]); gqs = sb(g, "gqs", [128, 3])
        glbTs = sb(g, "glbTs", [128, 2, NH]); omlT = sb(g, "omlT", [128, NH])
        lb_b = sb(g, "lb_b", [128, D]); oml_b = sb(g, "oml_b", [128, D])
        go_b = sb(g, "go_b", [128, 128]); gkv_b = sb(g, "gkv_b", [128, KVL]); gfin_b = sb(g, "gfin_b", [128, D])
        ctmp = sb(g, "ctmp", [128, D])
        ones1 = sb(g, "ones1", [128, 1])
        tr.op('pool', lambda e: e.memset(ones1[:], 1.0), [], ["ones1"])

        def ld(dst, src, key, eng='sp'):
            tr.dma(eng, dst, src, [], [key], key)
        ld(idf[:], c_idf[:, :], "idf"); ld(idb[:], c_idb[:, :], "idb")
        ld(tri[:], c_tri.rearrange("a s t -> s a t"), "tri"); ld(up[:], c_up.rearrange("a s t -> s a t"), "up")
        ld(cb[:], c_cb[:, :], "cb"); ld(cbs[:], c_cbs[:, :], "cbs"); ld(bm[:], c_bm[:, :], "bm")
        ld(gvs[:], gv[:, :, :], "gvs"); ld(gqs[:], gq[:, :], "gqs"); ld(glbTs[:], glbT[:, :, :], "glbTs")
        ld(lb_b[:], glb[0, :].partition_broadcast(128), "lb_b")
        ld(ctmp[:], glb[1, :].partition_broadcast(128), "ctmp")
        ld(go_b[:], g_o.partition_broadcast(128), "go_b")
        ld(gkv_b[:], g_kv.partition_broadcast(128), "gkv_b")
        ld(gfin_b[:], g_fin.partition_broadcast(128), "gfin_b")
        tr.op('dve', lambda e: e.tensor_copy(out=trib[:], in_=tri[:]), ["tri"], ["trib"])
        tr.op('dve', lambda e: e.tensor_tensor(out=ctmp[:], in0=ctmp[:], in1=lb_b[:], op=ALU.subtract), ["ctmp", "lb_b"], ["ctmp"])
        tr.op('act', lambda e: e.activation(out=ctmp[:], in_=ctmp[:], func=AF.Exp), ["ctmp"], ["ctmp"])
        tr.op('dve', lambda e: e.tensor_scalar(out=ctmp[:], in0=ctmp[:], scalar1=1.0, scalar2=None, op0=ALU.add), ["ctmp"], ["ctmp"])
        tr.op('dve', lambda e: e.reciprocal(out=lb_b[:], in_=ctmp[:]), ["ctmp"], ["lb_b"])
        tr.op('dve', lambda e: e.tensor_scalar(out=oml_b[:], in0=lb_b[:], scalar1=-1.0, scalar2=1.0, op0=ALU.mult, op1=ALU.add), ["lb_b"], ["oml_b"])
        tr.op('dve', lambda e: e.tensor_tensor(out=omlT[:], in0=glbTs[:, 1, :], in1=glbTs[:, 0, :], op=ALU.subtract), ["glbTs"], ["omlT"])
        tr.op('act', lambda e: e.activation(out=omlT[:], in_=omlT[:], func=AF.Exp), ["omlT"], ["omlT"])
        tr.op('dve', lambda e: e.tensor_scalar(out=omlT[:], in0=omlT[:], scalar1=1.0, scalar2=None, op0=ALU.add), ["omlT"], ["omlT"])
        tr.op('dve', lambda e: e.reciprocal(out=omlT[:], in_=omlT[:]), ["omlT"], ["omlT"])
        tr.op('dve', lambda e: e.tensor_scalar(out=omlT[:], in0=omlT[:], scalar1=-1.0, scalar2=1.0, op0=ALU.mult, op1=ALU.add), ["omlT"], ["omlT"])

        wsrc = [("w_in", w_in), ("w_outa", w_outa), ("w_dkv", w_dkv), ("w_ukv", w_ukv), ("w_dq", w_dq), ("w_uq", w_uq),
                ("w_outb", w_outb), ("w_fg", w_fg), ("w_fu", w_fu), ("w_fd", w_fd)]
        wb = {}
        for nm, ap_ in wsrc:
            wb[nm] = nc.dram_tensor("b_" + nm, list(ap_.shape), BF16, kind="Internal").ap()
        for nm, ap_ in (("w_eg", w_eg), ("w_eu", w_eu), ("w_ed", w_ed)):
            wb[nm] = nc.dram_tensor("b_" + nm, list(ap_.shape), BF16, kind="Internal").ap()
        with ExitStack() as p0:
            NSL = 4
            stg = [sb(p0, "stg%d" % i, [128, 4096]) for i in range(NSL)]
            stb = [sb(p0, "stb%d" % i, [128, 4096], BF16) for i in range(NSL)]
            kk_ = [0]

            def precast(src2d, dst2d):
                tot = src2d.shape[0] * src2d.shape[1]
                per = tot // 128
                sv = src2d.rearrange("a b -> (a b)").rearrange("(p f) -> p f", p=128)
                dv = dst2d.rearrange("a b -> (a b)").rearrange("(p f) -> p f", p=128)
                for f0 in range(0, per, 4096):
                    n = min(4096, per - f0)
                    k = kk_[0]; kk_[0] += 1
                    sl = k % NSL
                    tr.dma('sp', stg[sl][:, 0:n], sv[:, f0:f0 + n], [], ["stg%d" % sl], "stg%d" % sl)
                    eng = ('act', 'dve')[k % 2]
                    if eng == 'act':
                        tr.op('act', lambda e, sl=sl, n=n: e.activation(out=stb[sl][:, 0:n], in_=stg[sl][:, 0:n], func=AF.Copy), ["stg%d" % sl], ["stb%d" % sl])
                    else:
                        tr.op(eng, lambda e, sl=sl, n=n: e.tensor_copy(out=stb[sl][:, 0:n], in_=stg[sl][:, 0:n]), ["stg%d" % sl], ["stb%d" % sl])
                    tr.dma('sp', dv[:, f0:f0 + n], stb[sl][:, 0:n], ["stb%d" % sl], ["wbout"], "stb%d" % sl)
            for nm, ap_ in wsrc:
                precast(ap_, wb[nm])
            for nm, ap_ in (("w_eg", w_eg), ("w_eu", w_eu), ("w_ed", w_ed)):
                for ex in range(NE):
                    precast(ap_[ex], wb[nm][ex])
        tr.barrier()
        w_in, w_outa, w_dkv, w_ukv, w_dq, w_uq, w_outb, w_fg, w_fu, w_fd = [wb[nm] for nm, _ in wsrc]
        w_eg, w_eu, w_ed = wb["w_eg"], wb["w_eu"], wb["w_ed"]

        def rms_scale(stack_bufs, xt, xkey, width, jkey="junk"):
            junk, ss, rstd = stack_bufs
            tr.op('act', lambda e: e.activation(out=junk[:, 0:width], in_=xt, func=AF.Square, accum_out=ss[:, 0:1]),
                  [xkey], [jkey, "ss"])
            tr.op('dve', lambda e: e.tensor_scalar(out=rstd[:, 0:1], in0=ss[:, 0:1], scalar1=1.0 / width, scalar2=EPS,
                                                   op0=ALU.mult, op1=ALU.add), ["ss"], ["rstd"])
            tr.op('act', lambda e: e.activation(out=rstd[:, 0:1], in_=rstd[:, 0:1], func=AF.Ln), ["rstd"], ["rstd"])
            tr.op('act', lambda e: e.activation(out=rstd[:, 0:1], in_=rstd[:, 0:1], func=AF.Exp, scale=-0.5), ["rstd"], ["rstd"])

        def transpose_bf(dstT, dkey, src_bf, skey, nchunk, bank, gain=None, gkey=None, eng='dve'):
            pt = ps[bank][:, :].bitcast(BF16)
            for c in range(nchunk):
                tr.op('pe', lambda e, c=c: e.transpose(out=pt[:, c * 128:(c + 1) * 128], in_=src_bf[:, c * 128:(c + 1) * 128], identity=idb[:]),
                      [skey, "idb"], [psk(bank)], inc=(c == nchunk - 1))
            pv = pt[:, 0:nchunk * 128].rearrange("p (c t) -> p c t", c=nchunk)
            if gain is None:
                if eng == 'act':
                    tr.op('act', lambda e: e.activation(out=dstT, in_=pv, func=AF.Copy), [psk(bank)], [dkey])
                else:
                    tr.op('dve', lambda e: e.tensor_copy(out=dstT, in_=pv), [psk(bank)], [dkey])
            else:
                tr.op('dve', lambda e: e.tensor_tensor(out=dstT, in0=pv, in1=gain.unsqueeze(2).broadcast_to([128, nchunk, 128]), op=ALU.mult),
                      [psk(bank), gkey], [dkey])

        def sigmoid_from_exp(buf, key, eng='dve'):
            tr.op(eng, lambda e: e.tensor_scalar(out=buf, in0=buf, scalar1=1.0, scalar2=None, op0=ALU.add), [key], [key])
            tr.op('dve', lambda e: e.reciprocal(out=buf, in_=buf), [key], [key])

        with ExitStack() as p1:
            w_in_sb = sb(p1, "w_in_sb", [128, 8, 4 * D], BF16)
            w_out_sb = sb(p1, "w_out_sb", [128, 8, D], BF16)
            for dc in range(8):
                for hh in range(2):
                    tr.dma('sp', w_in_sb[:, dc, hh * 2048:(hh + 1) * 2048], w_in[dc * 128:(dc + 1) * 128, hh * 2048:(hh + 1) * 2048],
                           [], ["w_in_sb"], "w_in_sb")
                tr.dma('sp', w_out_sb[:, dc, :], w_outa[dc * 128:(dc + 1) * 128, :], [], ["w_out_sb"], "w_out_sb")
            xt = [sb(p1, "xt%d" % i, [128, D]) for i in range(2)]
            ss = sb(p1, "ss", [128, 1]); rstd = sb(p1, "rstd", [128, 1])
            xs = sb(p1, "xs", [128, D], BF16); xnT = sb(p1, "xnT", [128, 8, 128], BF16)
            tA = sb(p1, "tA", [128, D]); tB = sb(p1, "tB", [128, D]); tC = sb(p1, "tC", [128, D])
            logf = sb(p1, "logf", [128, D]); ktok = sb(p1, "ktok", [128, D])
            kd = sb(p1, "kd", [128, D], BF16); vtok = sb(p1, "vtok", [128, D], BF16)
            sgate = sb(p1, "sgate", [128, D])
            sq = sb(p1, "sq", [128, NH, 128]); kT = sb(p1, "kT", [128, NH, 128])
            qeT = sb(p1, "qeT", [128, NH, 128], BF16); keT = sb(p1, "keT", [128, NH, 128], BF16)
            qem = sb(p1, "qem", [128, NH, 2, 128], BF16)
            qems = sb(p1, "qems", [128, SB, 128], BF16)
            ebl = sb(p1, "ebl", [128, NH, SB])
            scm = sb(p1, "scm", [128, NH, 128], BF16)
            S32 = sb(p1, "S32", [128, NH, 128]); S1_32 = sb(p1, "S1_32", [128, NH, 128])
            Sb = sb(p1, "Sb", [128, NH, 128], BF16); S1b = sb(p1, "S1b", [128, NH, 128], BF16)
            rs8 = sb(p1, "rs8", [128, NH]); onb = sb(p1, "onb", [128, D], BF16); onT = sb(p1, "onT", [128, 8, 128], BF16)
            s0f = sb(p1, "s0f", [128, SB, 128]); s0b = sb(p1, "s0b", [128, SB, 128], BF16)
            kdm = sb(p1, "kdm", [128, SB, 128], BF16); snw = s0f

            tr.op('pool', lambda e: e.memset(qem[:], 0.0), [], ["qem"])
            tr.op('pool', lambda e: e.memset(qems[:], 0.0), [], ["qems"])
            tr.op('pool', lambda e: e.memset(S32[:], 0.0), [], ["S32"])
            tr.op('pool', lambda e: e.memset(Sb[:], 0.0), [], ["Sb"])

            def proj_tok(col0, banks):
                for hb in range(2):
                    for dc in range(8):
                        tr.op('pe', lambda e, hb=hb, dc=dc: e.matmul(ps[banks[hb]][:, :], lhsT=xnT[:, dc, :],
                                                                      rhs=w_in_sb[:, dc, col0 + hb * 512: col0 + (hb + 1) * 512],
                                                                      start=(dc == 0), stop=(dc == 7)),
                              ["xnT", "w_in_sb"], [psk(banks[hb])], inc=(dc == 7))

            def proj_feat(col0, banks):
                for fc in range(8):
                    b = banks[fc // 4]
                    for dc in range(8):
                        tr.op('pe', lambda e, fc=fc, dc=dc, b=b: e.matmul(ps[b][:, (fc % 4) * 128:(fc % 4 + 1) * 128],
                                                                          lhsT=w_in_sb[:, dc, col0 + fc * 128: col0 + (fc + 1) * 128],
                                                                          rhs=xnT[:, dc, :], start=(dc == 0), stop=(dc == 7)),
                              ["xnT", "w_in_sb"], [psk(b)], inc=(dc == 7))

            def ps2(banks):
                return [(ps[banks[0]][:, :], slice(0, 512), psk(banks[0])), (ps[banks[1]][:, :], slice(512, 1024), psk(banks[1]))]

            for ti in range(NTT):
                samp = (ti == NT)
                cm = 1 if samp else 0
                x_t = xt[ti % 2]; xk = "xt%d" % (ti % 2)
                tr.dma('sp', x_t[:], xrows(ti), [], [xk], xk)
                rms_scale((tB, ss, rstd), x_t[:], xk, D, jkey="tB")
                tr.op('act', lambda e: e.activation(out=xs[:], in_=x_t[:], func=AF.Copy, scale=rstd[:, 0:1]), [xk, "rstd"], ["xs"])
                transpose_bf(xnT[:], "xnT", xs, "xs", 8, 4, gain=gvs[:, 0, :], gkey="gvs")
                proj_feat(0, (0, 1))
                sqf = sq[:].rearrange("p h t -> p (h t)")
                for pa, sl, pk in ps2((0, 1)):
                    tr.op('act', lambda e, pa=pa, sl=sl: e.activation(out=sqf[:, sl], in_=pa, func=AF.Exp, scale=-1.0), [pk], ["sq"])
                sigmoid_from_exp(sqf, "sq")
                for pa, sl, pk in ps2((0, 1)):
                    tr.op('dve', lambda e, pa=pa, sl=sl: e.tensor_tensor(out=sqf[:, sl], in0=sqf[:, sl], in1=pa, op=ALU.mult), [pk, "sq"], ["sq"])
                proj_feat(D, (2, 3))
                kTf = kT[:].rearrange("p h t -> p (h t)")
                for pa, sl, pk in ps2((2, 3)):
                    tr.op('act', lambda e, pa=pa, sl=sl: e.activation(out=kTf[:, sl], in_=pa, func=AF.Exp), [pk], ["kT"])
                sigmoid_from_exp(kTf, "kT")
                tr.op('dve', lambda e: e.tensor_tensor(out=kT[:], in0=kT[:], in1=omlT[:].unsqueeze(2).broadcast_to([128, NH, 128]), op=ALU.mult),
                      ["kT", "omlT"], ["kT"])
                proj_tok(D, (0, 1))
                for pa, sl, pk in ps2((0, 1)):
                    tr.op('act', lambda e, pa=pa, sl=sl: e.activation(out=tA[:, sl], in_=pa, func=AF.Exp, scale=-1.0), [pk], ["tA"])
                sigmoid_from_exp(tA[:], "tA")
                tr.op('dve', lambda e: e.tensor_tensor(out=tA[:], in0=tA[:], in1=oml_b[:], op=ALU.mult), ["tA", "oml_b"], ["tA"])
                tr.op('dve', lambda e: e.tensor_tensor(out=tA[:], in0=tA[:], in1=lb_b[:], op=ALU.add), ["tA", "lb_b"], ["tA"])
                tr.op('act', lambda e: e.activation(out=logf[:], in_=tA[:], func=AF.Ln), ["tA"], ["logf"])
                tr.op('act', lambda e: e.activation(out=ktok[:], in_=tA[:], func=AF.Identity, scale=-1.0, bias=ones1[:, 0:1]), ["tA", "ones1"], ["ktok"])
                for h in range(NH):
                    b = 2 + h // 4
                    tr.op('pe', lambda e, h=h, b=b: e.matmul(ps[b][:, (h % 4) * 128:(h % 4 + 1) * 128], lhsT=logf[:, h * 128:(h + 1) * 128],
                                                             rhs=tri[:, cm, :], start=True, stop=True),
                          ["logf", "tri"], [psk(b)], inc=(h % 4 == 3))
                for hb in range(2):
                    tr.op('pe', lambda e, hb=hb: e.matmul(ps[hb][:, :], lhsT=up[:, cm, :], rhs=logf[:, hb * 512:(hb + 1) * 512], start=True, stop=True),
                          ["logf", "up"], [psk(hb)])
                tBf = tB[:]; tCf = tC[:]
                for pa, sl, pk in ps2((2, 3)):
                    tr.op('act', lambda e, pa=pa, sl=sl: e.activation(out=tBf[:, sl], in_=pa, func=AF.Exp), [pk], ["tB"])
                    tr.op('act', lambda e, pa=pa, sl=sl: e.activation(out=tCf[:, sl], in_=pa, func=AF.Exp, scale=-1.0), [pk], ["tC"])
                tr.op('dve', lambda e: e.tensor_tensor(out=qeT[:].rearrange("p h t -> p (h t)"), in0=sqf, in1=tBf, op=ALU.mult), ["sq", "tB"], ["qeT"])
                tr.op('dve', lambda e: e.tensor_tensor(out=keT[:].rearrange("p h t -> p (h t)"), in0=kTf, in1=tCf, op=ALU.mult), ["kT", "tC"], ["keT"])
                nch = SB if samp else 2
                cl = 128 // nch
                tB3 = tB[:].rearrange("p (h c l) -> p h c l", h=NH, c=nch)
                tr.op('act', lambda e: e.activation(out=ebl[:, :, 0:nch], in_=tB3[:, :, :, cl - 1], func=AF.Copy), ["tB"], ["ebl"])
                for pa, sl, pk in ps2((0, 1)):
                    tr.op('act', lambda e, pa=pa, sl=sl: e.activation(out=tA[:, sl], in_=pa, func=AF.Exp), [pk], ["tA"])
                tr.op('dve', lambda e: e.tensor_tensor(out=kd[:], in0=ktok[:], in1=tA[:], op=ALU.mult), ["ktok", "tA"], ["kd"])
                proj_tok(2 * D, (2, 3))
                for pa, sl, pk in ps2((2, 3)):
                    tr.op('act', lambda e, pa=pa, sl=sl: e.activation(out=vtok[:, sl], in_=pa, func=AF.Copy), [pk], ["vtok"])
                proj_tok(3 * D, (0, 1))
                for pa, sl, pk in ps2((0, 1)):
                    tr.op('act', lambda e, pa=pa, sl=sl: e.activation(out=sgate[:, sl], in_=pa, func=AF.Exp, scale=-1.0), [pk], ["sgate"])
                sigmoid_from_exp(sgate[:], "sgate")
                for pa, sl, pk in ps2((0, 1)):
                    tr.op('dve', lambda e, pa=pa, sl=sl: e.tensor_tensor(out=sgate[:, sl], in0=sgate[:, sl], in1=pa, op=ALU.mult), [pk, "sgate"], ["sgate"])
                for h in range(NH):
                    b = 4 + h // 4
                    tr.op('pe', lambda e, h=h, b=b: e.matmul(ps[b][:, (h % 4) * 128:(h % 4 + 1) * 128], lhsT=keT[:, h, :], rhs=qeT[:, h, :],
                                                             start=True, stop=True), ["keT", "qeT"], [psk(b)], inc=(h % 4 == 3))
                for hb in range(2):
                    tr.op('dve', lambda e, hb=hb: e.tensor_tensor(out=scm[:, hb * 4:(hb + 1) * 4, :],
                                                                  in0=ps[4 + hb][:, :].rearrange("p (h t) -> p h t", h=4),
                                                                  in1=tri[:, cm, :].unsqueeze(1).broadcast_to([128, 4, 128]), op=ALU.mult),
                          [psk(4 + hb), "tri"], ["scm"])
                if not samp:
                    tr.op('act', lambda e: e.activation(out=qem[:, :, 0, 0:64], in_=qeT[:, :, 0:64], func=AF.Copy), ["qeT"], ["qem"])
                    tr.op('act', lambda e: e.activation(out=qem[:, :, 1, 64:128], in_=qeT[:, :, 64:128], func=AF.Copy), ["qeT"], ["qem"])
                    for h in range(NH):
                        b = 6 + h // 4
                        tr.op('pe', lambda e, h=h, b=b: e.matmul(ps[b][:, (h % 4) * 128:(h % 4 + 1) * 128], lhsT=kd[0:64, h * 128:(h + 1) * 128],
                                                                 rhs=vtok[0:64, h * 128:(h + 1) * 128], start=True, stop=True),
                              ["kd", "vtok"], [psk(b)], inc=(h % 4 == 3))
                    tr.op('dve', lambda e: e.tensor_tensor(out=S1_32[:], in0=S32[:], in1=ebl[:, :, 0:1].broadcast_to([128, NH, 128]), op=ALU.mult),
                          ["S32", "ebl"], ["S1_32"])
                    for hb in range(2):
                        tr.op('dve', lambda e, hb=hb: e.tensor_tensor(out=S1_32[:, hb * 4:(hb + 1) * 4, :], in0=S1_32[:, hb * 4:(hb + 1) * 4, :],
                                                                      in1=ps[6 + hb][:, :].rearrange("p (h v) -> p h v", h=4), op=ALU.add),
                              [psk(6 + hb), "S1_32"], ["S1_32"])
                    tr.op('act', lambda e: e.activation(out=S1b[:], in_=S1_32[:], func=AF.Copy), ["S1_32"], ["S1b"])
                    for h in range(NH):
                        b = 2 + h // 4
                        osl = ps[b][:, (h % 4) * 128:(h % 4 + 1) * 128]
                        tr.op('pe', lambda e, h=h, osl=osl: e.matmul(osl, lhsT=qem[:, h, 0, :], rhs=Sb[:, h, :], start=True, stop=False),
                              ["qem", "Sb"], [psk(b)], inc=False)
                        tr.op('pe', lambda e, h=h, osl=osl: e.matmul(osl, lhsT=qem[:, h, 1, :], rhs=S1b[:, h, :], start=False, stop=False),
                              ["qem", "S1b"], [psk(b)], inc=False)
                        tr.op('pe', lambda e, h=h, osl=osl: e.matmul(osl, lhsT=scm[:, h, :], rhs=vtok[:, h * 128:(h + 1) * 128], start=False, stop=True),
                              ["scm", "vtok"], [psk(b)], inc=(h % 4 == 3))
                    for h in range(NH):
                        b = 6 + h // 4
                        tr.op('pe', lambda e, h=h, b=b: e.matmul(ps[b][:, (h % 4) * 128:(h % 4 + 1) * 128], lhsT=kd[64:128, h * 128:(h + 1) * 128],
                                                                 rhs=vtok[64:128, h * 128:(h + 1) * 128], start=True, stop=True),
                              ["kd", "vtok"], [psk(b)], inc=(h % 4 == 3))
                    tr.op('dve', lambda e: e.tensor_tensor(out=S32[:], in0=S1_32[:], in1=ebl[:, :, 1:2].broadcast_to([128, NH, 128]), op=ALU.mult),
                          ["S1_32", "ebl"], ["S32"])
                    for hb in range(2):
                        tr.op('dve', lambda e, hb=hb: e.tensor_tensor(out=S32[:, hb * 4:(hb + 1) * 4, :], in0=S32[:, hb * 4:(hb + 1) * 4, :],
                                                                      in1=ps[6 + hb][:, :].rearrange("p (h v) -> p h v", h=4), op=ALU.add),
                              [psk(6 + hb), "S32"], ["S32"])
                    tr.op('act', lambda e: e.activation(out=Sb[:], in_=S32[:], func=AF.Copy), ["S32"], ["Sb"])
                    if ti == NT - 1:
                        tr.dma('sp', hg_p.rearrange("h k v -> k h v"), S32[:], ["S32"], ["hg_p"], "hg_p")
                else:
                    for h in range(NH):
                        tr.dma('sp', s0f[:], st_in[:, h, :, :].rearrange("b k v -> k b v"), [], ["s0f"], "s0f")
                        tr.op('act', lambda e: e.activation(out=s0b[:], in_=s0f[:], func=AF.Copy), ["s0f"], ["s0b"])
                        qv = qems[:].rearrange("p b (c l) -> p b c l", c=SB)
                        for b_ in range(SB):
                            tr.op('pool', lambda e, b_=b_, h=h: e.tensor_copy(out=qems[:, b_, b_ * ST:(b_ + 1) * ST], in_=qeT[:, h, b_ * ST:(b_ + 1) * ST]),
                                  ["qeT"], ["qems"])
                        ob = 4 + h // 4
                        osl = ps[ob][:, (h % 4) * 128:(h % 4 + 1) * 128]
                        for b_ in range(SB):
                            tr.op('pe', lambda e, b_=b_, osl=osl: e.matmul(osl, lhsT=qems[:, b_, :], rhs=s0b[:, b_, :], start=(b_ == 0), stop=False),
                                  ["qems", "s0b"], [psk(ob)], inc=False)
                        tr.op('pe', lambda e, h=h, osl=osl: e.matmul(osl, lhsT=scm[:, h, :], rhs=vtok[:, h * 128:(h + 1) * 128], start=False, stop=True),
                              ["scm", "vtok"], [psk(ob)])
                        tr.op('dve', lambda e, h=h: e.tensor_tensor(out=kdm[:], in0=kd[:, h * 128:(h + 1) * 128].unsqueeze(1).broadcast_to([128, SB, 128]),
                                                                    in1=bm[:].unsqueeze(2).broadcast_to([128, SB, 128]), op=ALU.mult),
                              ["kd", "bm"], ["kdm"])
                        for b_ in range(SB):
                            bk = b_ // 4
                            tr.op('pe', lambda e, b_=b_, bk=bk, h=h: e.matmul(ps[bk][:, (b_ % 4) * 128:(b_ % 4 + 1) * 128], lhsT=kdm[:, b_, :],
                                                                             rhs=vtok[:, h * 128:(h + 1) * 128], start=True, stop=True),
                                  ["kdm", "vtok"], [psk(bk)], inc=(b_ % 4 == 3))
                        tr.op('dve', lambda e, h=h: e.tensor_tensor(out=snw[:], in0=s0f[:], in1=ebl[:, h, :].unsqueeze(2).broadcast_to([128, SB, 128]), op=ALU.mult),
                              ["s0f", "ebl"], ["s0f"])
                        for bk in range(4):
                            tr.op('dve', lambda e, bk=bk: e.tensor_tensor(out=snw[:, bk * 4:(bk + 1) * 4, :], in0=snw[:, bk * 4:(bk + 1) * 4, :],
                                                                          in1=ps[bk][:, :].rearrange("p (b v) -> p b v", b=4), op=ALU.add),
                                  [psk(bk), "s0f"], ["s0f"])
                        tr.dma('sp', hg_s[:, h, :, :].rearrange("b k v -> k b v"), snw[:], ["s0f"], ["hg_s"], "s0f")
                obanks = (4, 5) if samp else (2, 3)
                for pa, sl, pk in ps2(obanks):
                    tr.op('act', lambda e, pa=pa, sl=sl: e.activation(out=tB[:, sl], in_=pa, func=AF.Square), [pk], ["tB"])
                tr.op('dve', lambda e: e.tensor_reduce(out=rs8[:], in_=tB[:].rearrange("p (h v) -> p h v", h=NH), axis=AX.X, op=ALU.add), ["tB"], ["rs8"])
                tr.op('dve', lambda e: e.tensor_scalar(out=rs8[:], in0=rs8[:], scalar1=1.0 / 128, scalar2=EPS, op0=ALU.mult, op1=ALU.add), ["rs8"], ["rs8"])
                tr.op('act', lambda e: e.activation(out=rs8[:], in_=rs8[:], func=AF.Ln), ["rs8"], ["rs8"])
                tr.op('act', lambda e: e.activation(out=rs8[:], in_=rs8[:], func=AF.Exp, scale=-0.5), ["rs8"], ["rs8"])
                for hb, (pa, sl, pk) in enumerate(ps2(obanks)):
                    tr.op('dve', lambda e, pa=pa, sl=sl, hb=hb: e.tensor_tensor(out=tC[:, sl].rearrange("p (h v) -> p h v", h=4),
                                                                               in0=pa.rearrange("p (h v) -> p h v", h=4),
                                                                               in1=rs8[:, hb * 4:(hb + 1) * 4].unsqueeze(2).broadcast_to([128, 4, 128]), op=ALU.mult),
                          [pk, "rs8"], ["tC"])
                tr.op('dve', lambda e: e.tensor_tensor(out=tC[:].rearrange("p (h v) -> p h v", h=NH), in0=tC[:].rearrange("p (h v) -> p h v", h=NH),
                                                        in1=go_b[:].unsqueeze(1).broadcast_to([128, NH, 128]), op=ALU.mult), ["tC", "go_b"], ["tC"])
                tr.op('dve', lambda e: e.tensor_tensor(out=onb[:], in0=tC[:], in1=sgate[:], op=ALU.mult), ["tC", "sgate"], ["onb"])
                transpose_bf(onT[:], "onT", onb, "onb", 8, 6)
                for hb in range(2):
                    for c in range(8):
                        tr.op('pe', lambda e, hb=hb, c=c: e.matmul(ps[hb][:, :], lhsT=onT[:, c, :], rhs=w_out_sb[:, c, hb * 512:(hb + 1) * 512],
                                                                   start=(c == 0), stop=(c == 7)), ["onT", "w_out_sb"], [psk(hb)], inc=(c == 7))
                for pa, sl, pk in ps2((0, 1)):
                    tr.op('dve', lambda e, pa=pa, sl=sl: e.tensor_tensor(out=x_t[:, sl], in0=x_t[:, sl], in1=pa, op=ALU.add), [pk, xk], [xk])
                tr.dma('sp', hA[ti], x_t[:], [xk], [("hA", ti)], xk)

        def emit_debug(hsrc, keys):
            with ExitStack() as pd:
                t_ = sb(pd, "dbg", [128, D])
                for ti in range(NTT):
                    tr.dma('sp', t_[:], hsrc[ti], [(hsrc.tensor.name, ti)], ["dbg"], "dbg")
                    dst = y_p[ti * 128:(ti + 1) * 128, :] if ti < NT else y_s[:, :]
                    tr.dma('sp', dst, t_[:], ["dbg"], ["yout"], "dbgo")
            tr.final_wait('sp', ["dbgo"] + keys)

        if phases == 1:
            emit_debug(hA, ["hg_p", "s0f"])
            return nc, tr

        groups = [list(range(g0, min(g0 + 4, NT))) for g0 in range(0, NT, 4)] + [[NT]]

        def ffn_phase(hin, hout, gidx, experts, moe):
            tr.barrier()
            with ExitStack() as pf:
                hres = sb(pf, "hres", [128, 4, D]); xnTg = sb(pf, "xnTg", [128, 8, 512], BF16)
                xsb = sb(pf, "xsb", [128, D], BF16); ssf = sb(pf, "ssf", [128, 1]); rstf = sb(pf, "rstf", [128, 1])
                jk = sb(pf, "jk", [128, D])
                wgs = [sb(pf, "wgs%d" % i, [128, 8, 512], BF16) for i in range(2)]
                wus = [sb(pf, "wus%d" % i, [128, 8, 512], BF16) for i in range(2)]
                wds = [sb(pf, "wds%d" % i, [128, NFC, 512], BF16) for i in range(2)]
                hT = sb(pf, "hT", [128, NFC, 512], BF16)
                tm = [sb(pf, "tm%d" % i, [128, 512]) for i in range(2)]
                if moe:
                    xnT32 = sb(pf, "xnT32", [128, 8, 128]); wrs = sb(pf, "wrs", [128, 8, NE])
                    lg = sb(pf, "lg", [128, NE]); l2 = sb(pf, "l2", [128, NE]); eq1 = sb(pf, "eq1", [128, NE]); eq2 = sb(pf, "eq2", [128, NE])
                    m1 = sb(pf, "m1", [128, 1]); m2 = sb(pf, "m2", [128, 1]); g1 = sb(pf, "g1", [128, 1]); g2 = sb(pf, "g2", [128, 1])
                    comb = sb(pf, "comb", [128, 4, NE]); yst = sb(pf, "yst", [128, D])
                    tr.dma('sp', wrs[:], w_r.rearrange("(c p) e -> p c e", p=128), [], ["wrs"], "wrs")
                slab_n = 0
                wd_n = 0
                for grp in groups:
                    nt_ = len(grp); GT = nt_ * 128
                    for li, ti in enumerate(grp):
                        hk_ = ("hres", li)
                        tr.dma('sp', hres[:, li, :], hin[ti], [(hin.tensor.name, ti)], [hk_], "hres%d" % li)
                        rms_scale((jk, ssf, rstf), hres[:, li, :], hk_, D, jkey="jk")
                        tr.op('act', lambda e, li=li: e.activation(out=xsb[:], in_=hres[:, li, :], func=AF.Copy, scale=rstf[:, 0:1]), [hk_, "rstd"], ["xsb"])
                        transpose_bf(xnTg[:, :, li * 128:(li + 1) * 128], "xnTg", xsb, "xsb", 8, 6, gain=gvs[:, gidx, :], gkey="gvs")
                        if moe:
                            tr.op('act', lambda e, li=li: e.activation(out=jk[:], in_=hres[:, li, :], func=AF.Copy, scale=rstf[:, 0:1]), [hk_, "rstd"], ["jk"])
                            for c in range(8):
                                b = 6 + c // 4
                                tr.op('pe', lambda e, c=c, b=b: e.transpose(out=ps[b][:, (c % 4) * 128:(c % 4 + 1) * 128], in_=jk[:, c * 128:(c + 1) * 128], identity=idf[:]),
                                      ["jk", "idf"], [psk(b)], inc=(c % 4 == 3))
                            for hb in range(2):
                                tr.op('dve', lambda e, hb=hb: e.tensor_tensor(out=xnT32[:, hb * 4:(hb + 1) * 4, :], in0=ps[6 + hb][:, :].rearrange("p (c t) -> p c t", c=4),
                                                                              in1=gvs[:, gidx, hb * 4:(hb + 1) * 4].unsqueeze(2).broadcast_to([128, 4, 128]), op=ALU.mult),
                                      [psk(6 + hb), "gvs"], ["xnT32"])
                            for c in range(8):
                                tr.op('pe', lambda e, c=c: e.matmul(ps[6][:, 0:NE], lhsT=xnT32[:, c, :], rhs=wrs[:, c, :], start=(c == 0), stop=(c == 7)),
                                      ["xnT32", "wrs"], [psk(6)], inc=(c == 7))
                            tr.op('dve', lambda e: e.tensor_copy(out=lg[:], in_=ps[6][:, 0:NE]), [psk(6)], ["lg"])
                            tr.op('dve', lambda e: e.tensor_reduce(out=m1[:], in_=lg[:], axis=AX.X, op=ALU.max), ["lg"], ["m1"])
                            tr.op('dve', lambda e: e.tensor_scalar(out=eq1[:], in0=lg[:], scalar1=m1[:, 0:1], scalar2=None, op0=ALU.is_equal), ["lg", "m1"], ["eq1"])
                            tr.op('dve', lambda e: e.scalar_tensor_tensor(out=l2[:], in0=eq1[:], scalar=NEG, in1=lg[:], op0=ALU.mult, op1=ALU.add), ["eq1", "lg"], ["l2"])
                            tr.op('dve', lambda e: e.tensor_reduce(out=m2[:], in_=l2[:], axis=AX.X, op=ALU.max), ["l2"], ["m2"])
                            tr.op('dve', lambda e: e.tensor_scalar(out=eq2[:], in0=l2[:], scalar1=m2[:, 0:1], scalar2=None, op0=ALU.is_equal), ["l2", "m2"], ["eq2"])
                            tr.op('dve', lambda e: e.tensor_tensor(out=g2[:], in0=m2[:], in1=m1[:], op=ALU.subtract), ["m1", "m2"], ["g2"])
                            tr.op('act', lambda e: e.activation(out=g2[:], in_=g2[:], func=AF.Exp), ["g2"], ["g2"])
                            tr.op('dve', lambda e: e.tensor_scalar(out=g1[:], in0=g2[:], scalar1=1.0, scalar2=None, op0=ALU.add), ["g2"], ["g1"])
                            tr.op('dve', lambda e: e.reciprocal(out=g1[:], in_=g1[:]), ["g1"], ["g1"])
                            tr.op('dve', lambda e: e.tensor_tensor(out=g2[:], in0=g2[:], in1=g1[:], op=ALU.mult), ["g1", "g2"], ["g2"])
                            tr.op('dve', lambda e, li=li: e.tensor_scalar(out=comb[:, li, :], in0=eq1[:], scalar1=g1[:, 0:1], scalar2=None, op0=ALU.mult), ["eq1", "g1"], ["comb"])
                            tr.op('dve', lambda e, li=li: e.scalar_tensor_tensor(out=comb[:, li, :], in0=eq2[:], scalar=g2[:, 0:1], in1=comb[:, li, :], op0=ALU.mult, op1=ALU.add),
                                  ["eq2", "g2", "comb"], ["comb"])
                    for ex in range(experts):
                        wg_, wu_, wd_ = (w_eg[ex], w_eu[ex], w_ed[ex]) if moe else (w_fg, w_fu, w_fd)
                        for jb in range(6):
                            ncol = 512 if jb < 5 else 256
                            sl_ = slab_n % 2; slab_n += 1
                            tr.dma('sp', wgs[sl_][:, :, 0:ncol], wg_.rearrange("(c p) f -> p c f", p=128)[:, :, jb * 512: jb * 512 + ncol], [], ["wgs%d" % sl_], "wgs%d" % sl_)
                            tr.dma('sp', wus[sl_][:, :, 0:ncol], wu_.rearrange("(c p) f -> p c f", p=128)[:, :, jb * 512: jb * 512 + ncol], [], ["wus%d" % sl_], "wus%d" % sl_)
                            for jj in range(ncol // 128):
                                j = jb * 4 + jj
                                ba, bb = (0, 1) if j % 2 == 0 else (2, 3)
                                for dc in range(8):
                                    tr.op('pe', lambda e, dc=dc, jj=jj, ba=ba: e.matmul(ps[ba][:, 0:GT], lhsT=wgs[sl_][:, dc, jj * 128:(jj + 1) * 128], rhs=xnTg[:, dc, 0:GT],
                                                                                      start=(dc == 0), stop=(dc == 7)), ["wgs%d" % sl_, "xnTg"], [psk(ba)], inc=(dc == 7))
                                for dc in range(8):
                                    tr.op('pe', lambda e, dc=dc, jj=jj, bb=bb: e.matmul(ps[bb][:, 0:GT], lhsT=wus[sl_][:, dc, jj * 128:(jj + 1) * 128], rhs=xnTg[:, dc, 0:GT],
                                                                                      start=(dc == 0), stop=(dc == 7)), ["wus%d" % sl_, "xnTg"], [psk(bb)], inc=(dc == 7))
                                t_ = tm[j % 2]; tk = "tm%d" % (j % 2)
                                tr.op('act', lambda e, t_=t_, ba=ba: e.activation(out=t_[:, 0:GT], in_=ps[ba][:, 0:GT], func=AF.Exp, scale=-1.0), [psk(ba)], [tk])
                                tr.op('dve', lambda e, t_=t_: e.tensor_scalar(out=t_[:, 0:GT], in0=t_[:, 0:GT], scalar1=1.0, scalar2=None, op0=ALU.add), [tk], [tk])
                                tr.op('dve', lambda e, t_=t_: e.reciprocal(out=t_[:, 0:GT], in_=t_[:, 0:GT]), [tk], [tk])
                                tr.op('dve', lambda e, t_=t_, ba=ba: e.tensor_tensor(out=t_[:, 0:GT], in0=t_[:, 0:GT], in1=ps[ba][:, 0:GT], op=ALU.mult), [tk, psk(ba)], [tk])
                                tr.op('dve', lambda e, t_=t_, bb=bb, j=j: e.tensor_tensor(out=hT[:, j, 0:GT], in0=t_[:, 0:GT], in1=ps[bb][:, 0:GT], op=ALU.mult), [tk, psk(bb)], [("hT", j)])
                        for half in range(2):
                            ws_ = wd_n % 2; wd_n += 1
                            tr.dma('sp', wds[ws_][:, :, :], wd_.rearrange("(j p) d -> p j d", p=128)[:, :, half * 512:(half + 1) * 512],
                                   [], ["wds%d" % ws_], "wds%d" % ws_)
                            for li in range(nt_):
                                bk = 4 + (li % 2)
                                for j in range(NFC):
                                    tr.op('pe', lambda e, j=j, li=li, bk=bk: e.matmul(ps[bk][:, :], lhsT=hT[:, j, li * 128:(li + 1) * 128], rhs=wds[ws_][:, j, :],
                                                                                    start=(j == 0), stop=(j == NFC - 1)), [("hT", j), "wds%d" % ws_], [psk(bk)], inc=(j == NFC - 1))
                                hsl = hres[:, li, half * 512:(half + 1) * 512]
                                if moe:
                                    tr.op('dve', lambda e, hsl=hsl, bk=bk, li=li, ex=ex: e.scalar_tensor_tensor(out=hsl, in0=ps[bk][:, :], scalar=comb[:, li, ex:ex + 1], in1=hsl,
                                                                                                           op0=ALU.mult, op1=ALU.add), [psk(bk), "comb", ("hres", li)], [("hres", li)])
                                else:
                                    tr.op('dve', lambda e, hsl=hsl, bk=bk: e.tensor_tensor(out=hsl, in0=hsl, in1=ps[bk][:, :], op=ALU.add), [psk(bk), ("hres", li)], [("hres", li)])
                    for li, ti in enumerate(grp):
                        hk_ = ("hres", li)
                        if not moe:
                            tr.dma('sp', hout[ti], hres[:, li, :], [hk_], [(hout.tensor.name, ti)], "hres%d" % li)
                        else:
                            rms_scale((jk, ssf, rstf), hres[:, li, :], hk_, D, jkey="jk")
                            tr.op('dve', lambda e, li=li: e.scalar_tensor_tensor(out=yst[:], in0=hres[:, li, :], scalar=rstf[:, 0:1], in1=gfin_b[:], op0=ALU.mult, op1=ALU.mult),
                                  [hk_, "rstd", "gfin_b"], ["yst"])
                            dst = y_p[ti * 128:(ti + 1) * 128, :] if ti < NT else y_s[:, :]
                            tr.dma('sp', dst, yst[:], ["yst"], ["yout"], "yst")

        ffn_phase(hA, hB, 1, 1, False)
        if phases == 2:
            emit_debug(hB, ["hg_p", "s0f"])
            return nc, tr
        if phases == 24:
            ffn_phase(hB, None, 4, NE, True)
            tr.final_wait('sp', ["hg_p", "s0f", "yst"])
            return nc, tr

        tr.barrier()
        with ExitStack() as p3:
            wdkv_sb = sb(p3, "wdkv_sb", [128, 8, 320], BF16); wdq_sb = sb(p3, "wdq_sb", [128, 8, QL], BF16)
            wuq_sb = sb(p3, "wuq_sb", [128, 3, NH * 192], BF16); wukv_sb = sb(p3, "wukv_sb", [128, 2, NH * 256], BF16)
            woutb_sb = sb(p3, "woutb_sb", [128, 8, D], BF16); wukT = sb(p3, "wukT", [128, NH, 256], BF16)
            tr.dma('sp', wdkv_sb[:], w_dkv.rearrange("(c p) f -> p c f", p=128), [], ["wdkv_sb"], "wdkv_sb")
            tr.dma('sp', wdq_sb[:], w_dq.rearrange("(c p) f -> p c f", p=128), [], ["wdq_sb"], "wdq_sb")
            tr.dma('sp', wuq_sb[:], w_uq.rearrange("(c p) f -> p c f", p=128), [], ["wuq_sb"], "wuq_sb")
            for cc in range(2):
                tr.dma('sp', wukv_sb[:, cc, :], w_ukv[cc * 128:(cc + 1) * 128, :], [], ["wukv_sb"], "wukv_sb")
            for c in range(8):
                tr.dma('sp', woutb_sb[:, c, :], w_outb[c * 128:(c + 1) * 128, :], [], ["woutb_sb"], "woutb_sb")
            ptb = ps[6][:, :].bitcast(BF16)
            for h in range(NH):
                for cc in range(2):
                    tr.op('pe', lambda e, h=h, cc=cc: e.transpose(out=ptb[:, (h % 4) * 256 + cc * 128:(h % 4) * 256 + (cc + 1) * 128],
                                                                  in_=wukv_sb[:, cc, h * 256:h * 256 + 128], identity=idb[:]),
                          ["wukv_sb", "idb"], [psk(6)], inc=(cc == 1 and h % 4 == 3))
                if h % 4 == 3:
                    tr.op('dve', lambda e, h=h: e.tensor_copy(out=wukT[:, h - 3:h + 1, :], in_=ptb[:, :].rearrange("p (h c) -> p h c", h=4)), [psk(6)], ["wukT"])
            cT = sb(p3, "cT", [128, 2, T], BF16); ctok = sb(p3, "ctok", [128, NT, KVL], BF16); krT = sb(p3, "krT", [64, T], BF16)
            cTs = sb(p3, "cTs", [128, 2, 128], BF16); ctoks = sb(p3, "ctoks", [128, KVL], BF16); krTs = sb(p3, "krTs", [64, 128], BF16)
            ht = sb(p3, "ht", [128, D]); jk3 = sb(p3, "jk3", [128, D]); ss3 = sb(p3, "ss3", [128, 1]); rs3 = sb(p3, "rs3", [128, 1])
            xs3 = sb(p3, "xs3", [128, D], BF16); nkvT = sb(p3, "nkvT", [128, 8, 128], BF16); xnbT = sb(p3, "xnbT", [128, 8, 128], BF16)
            cf = sb(p3, "cf", [128, KVL]); cbf = sb(p3, "cbf", [128, KVL], BF16); krf = sb(p3, "krf", [128, QKR]); krb = sb(p3, "krb", [128, QKR], BF16)
            cosb = sb(p3, "cosb", [128, 32]); sinb = sb(p3, "sinb", [128, 32]); r1 = sb(p3, "r1", [128, NH, 32]); r2 = sb(p3, "r2", [128, NH, 32])
            cqb = sb(p3, "cqb", [128, QL], BF16); cqT = sb(p3, "cqT", [128, 3, 128], BF16)
            qnT = sb(p3, "qnT", [128, NH, 128], BF16); qaT = sb(p3, "qaT", [128, 2, NH, 128], BF16)
            qrf = sb(p3, "qrf", [128, NH, QKR]); qrb = sb(p3, "qrb", [128, NH, QKR], BF16); qrT = sb(p3, "qrT", [64, NH, 128], BF16)
            mst = sb(p3, "mst", [128, 1]); mnew = sb(p3, "mnew", [128, 1]); lst = sb(p3, "lst", [128, 1]); alp = sb(p3, "alp", [128, 1])
            nbias = sb(p3, "nbias", [128, 1]); rsum = sb(p3, "rsum", [128, 1]); mx = sb(p3, "mx", [128, 1])
            oacc = sb(p3, "oacc", [128, KVL]); pbf = sb(p3, "pbf", [128, 512], BF16); pT = sb(p3, "pT", [128, 4, 128], BF16)
            olat = sb(p3, "olat", [128, KVL], BF16); olatT = sb(p3, "olatT", [128, 2, NH, 128], BF16); oT = sb(p3, "oT", [128, NH, 128], BF16)
            qas = sb(p3, "qas", [128, 2, NH, ST], BF16); qrs = sb(p3, "qrs", [64, NH, ST], BF16)
            KC = 16
            gc = [sb(p3, "gc%d" % i, [128, KC, KVL], BF16) for i in range(2)]
            gr = [sb(p3, "gr%d" % i, [128, KC, QKR], BF16) for i in range(2)]
            gcT = sb(p3, "gcT", [128, 2, 512], BF16); grT = sb(p3, "grT", [64, 512], BF16)
            pti = sb(p3, "pti", [128, SB], I32); msk = sb(p3, "msk", [64, SB, 128])
            tr.dma('sp', pti[0:NPG, :], ptT[:, :], [], ["pti"], "pti")
            tr.dma('sp', msk[:], c_msk.rearrange("b r k -> r b k"), [], ["msk"], "msk")

            def attend_block(M, qk, N, mask, vts, first):
                sbk = attend_block.n % 2; attend_block.n += 1
                S = ps[sbk][0:M, 0:N]
                for i_, (l_, r_, ks_) in enumerate(qk):
                    tr.op('pe', lambda e, l_=l_, r_=r_, i_=i_: e.matmul(S, lhsT=l_, rhs=r_, start=(i_ == 0), stop=(i_ == len(qk) - 1)),
                          ks_, [psk(sbk)], inc=(i_ == len(qk) - 1))
                if mask is not None:
                    map_, c0, n_, mk = mask
                    tr.op('dve', lambda e: e.tensor_tensor(out=ps[sbk][0:M, c0:c0 + n_], in0=ps[sbk][0:M, c0:c0 + n_], in1=map_, op=ALU.add), [psk(sbk), mk], [psk(sbk)])
                tr.op('dve', lambda e: e.tensor_reduce(out=mx[0:M, :], in_=S, axis=AX.X, op=ALU.max), [psk(sbk)], ["mx"])
                if first:
                    tr.op('dve', lambda e: e.tensor_copy(out=mst[0:M, :], in_=mx[0:M, :]), ["mx"], ["mst"])
                else:
                    tr.op('dve', lambda e: e.tensor_tensor(out=mnew[0:M, :], in0=mst[0:M, :], in1=mx[0:M, :], op=ALU.max), ["mx", "mst"], ["mnew"])
                    tr.op('dve', lambda e: e.tensor_tensor(out=alp[0:M, :], in0=mst[0:M, :], in1=mnew[0:M, :], op=ALU.subtract), ["mnew", "mst"], ["alp"])
                    tr.op('act', lambda e: e.activation(out=alp[0:M, :], in_=alp[0:M, :], func=AF.Exp, scale=SM_SCALE), ["alp"], ["alp"])
                    tr.op('dve', lambda e: e.tensor_copy(out=mst[0:M, :], in_=mnew[0:M, :]), ["mnew"], ["mst"])
                tr.op('dve', lambda e: e.tensor_scalar(out=nbias[0:M, :], in0=mst[0:M, :], scalar1=-SM_SCALE, scalar2=None, op0=ALU.mult), ["mst"], ["nbias"])
                tr.op('act', lambda e: e.activation(out=pbf[0:M, 0:N], in_=S, func=AF.Exp, bias=nbias[0:M, 0:1], scale=SM_SCALE, accum_out=rsum[0:M, 0:1]),
                      [psk(sbk), "nbias"], ["pbf", "rsum"])
                if first:
                    tr.op('dve', lambda e: e.tensor_copy(out=lst[0:M, :], in_=rsum[0:M, :]), ["rsum"], ["lst"])
                else:
                    tr.op('dve', lambda e: e.scalar_tensor_tensor(out=lst[0:M, :], in0=lst[0:M, :], scalar=alp[0:M, 0:1], in1=rsum[0:M, :], op0=ALU.mult, op1=ALU.add),
                          ["lst", "alp", "rsum"], ["lst"])
                pTp = ps[2][:, :].bitcast(BF16)
                c0 = 0
                for kt, (v_, nk, vk) in enumerate(vts):
                    tr.op('pe', lambda e, kt=kt, nk=nk, c0=c0: e.transpose(out=pTp[0:nk, kt * 128: kt * 128 + M], in_=pbf[0:M, c0:c0 + nk], identity=idb[0:M, 0:M]),
                          ["pbf", "idb"], [psk(2)], inc=(kt == len(vts) - 1))
                    c0 += nk
                nv = len(vts)
                tr.op('act', lambda e: e.activation(out=pT[:, 0:nv, 0:M], in_=pTp[:, 0:nv * 128].rearrange("p (k m) -> p k m", k=nv)[:, :, 0:M], func=AF.Copy), [psk(2)], ["pT"])
                for kt, (v_, nk, vk) in enumerate(vts):
                    tr.op('pe', lambda e, kt=kt, nk=nk, v_=v_: e.matmul(ps[3][0:M, 0:KVL], lhsT=pT[0:nk, kt, 0:M], rhs=v_, start=(kt == 0), stop=(kt == nv - 1)),
                          ["pT", vk], [psk(3)], inc=(kt == nv - 1))
                if first:
                    tr.op('dve', lambda e: e.tensor_copy(out=oacc[0:M, :], in_=ps[3][0:M, 0:KVL]), [psk(3)], ["oacc"])
                else:
                    tr.op('dve', lambda e: e.scalar_tensor_tensor(out=oacc[0:M, :], in0=oacc[0:M, :], scalar=alp[0:M, 0:1], in1=ps[3][0:M, 0:KVL], op0=ALU.mult, op1=ALU.add),
                          ["oacc", "alp", psk(3)], ["oacc"])
            attend_block.n = 0

            def finish_rows(M):
                tr.op('dve', lambda e: e.reciprocal(out=lst[0:M, :], in_=lst[0:M, :]), ["lst"], ["lst"])
                tr.op('dve', lambda e: e.tensor_scalar(out=olat[0:M, :], in0=oacc[0:M, :], scalar1=lst[0:M, 0:1], scalar2=None, op0=ALU.mult), ["oacc", "lst"], ["olat"])

            for ti in range(NTT):
                samp = (ti == NT)
                tr.dma('sp', ht[:], hB[ti], [("hB", ti)], ["ht"], "ht")
                tr.dma('sp', cosb[:], c_cos[ti], [], ["cosb"], "cosb"); tr.dma('sp', sinb[:], c_sin[ti], [], ["sinb"], "sinb")
                rms_scale((jk3, ss3, rs3), ht[:], "ht", D, jkey="jk3")
                tr.op('act', lambda e: e.activation(out=xs3[:], in_=ht[:], func=AF.Copy, scale=rs3[:, 0:1]), ["ht", "rstd"], ["xs3"])
                ptb6 = ps[6][:, :].bitcast(BF16)
                for c in range(8):
                    tr.op('pe', lambda e, c=c: e.transpose(out=ptb6[:, c * 128:(c + 1) * 128], in_=xs3[:, c * 128:(c + 1) * 128], identity=idb[:]), ["xs3", "idb"], [psk(6)], inc=(c == 7))
                pv6 = ptb6[:, :].rearrange("p (c t) -> p c t", c=8)
                tr.op('dve', lambda e: e.tensor_tensor(out=nkvT[:], in0=pv6, in1=gvs[:, 2, :].unsqueeze(2).broadcast_to([128, 8, 128]), op=ALU.mult), [psk(6), "gvs"], ["nkvT"])
                tr.op('dve', lambda e: e.tensor_tensor(out=xnbT[:], in0=pv6, in1=gvs[:, 3, :].unsqueeze(2).broadcast_to([128, 8, 128]), op=ALU.mult), [psk(6), "gvs"], ["xnbT"])
                for dc in range(8):
                    tr.op('pe', lambda e, dc=dc: e.matmul(ps[4][:, 0:320], lhsT=nkvT[:, dc, :], rhs=wdkv_sb[:, dc, :], start=(dc == 0), stop=(dc == 7)),
                          ["nkvT", "wdkv_sb"], [psk(4)], inc=(dc == 7))
                rms_scale((jk3, ss3, rs3), ps[4][:, 0:KVL], psk(4), KVL, jkey="jk3")
                tr.op('dve', lambda e: e.scalar_tensor_tensor(out=cf[:], in0=ps[4][:, 0:KVL], scalar=rs3[:, 0:1], in1=gkv_b[:], op0=ALU.mult, op1=ALU.mult),
                      [psk(4), "rstd", "gkv_b"], ["cf"])
                tr.dma('sp', (ckv_s[:, :] if samp else ckv_p[ti * 128:(ti + 1) * 128, :]), cf[:], ["cf"], ["ckvout"], "cf")
                ctk = ctoks[:] if samp else ctok[:, ti, :]
                ctkey = "ctoks" if samp else ("ctok", ti)
                tr.op('act', lambda e: e.activation(out=ctk, in_=cf[:], func=AF.Copy), ["cf"], [ctkey])
                x1 = ps[4][:, 256:288]; x2 = ps[4][:, 288:320]
                tr.op('dve', lambda e: e.tensor_tensor(out=krf[:, 0:32], in0=x1, in1=cosb[:], op=ALU.mult), [psk(4), "cosb"], ["krf"])
                tr.op('dve', lambda e: e.tensor_tensor(out=r1[:, 0, :], in0=x2, in1=sinb[:], op=ALU.mult), [psk(4), "sinb"], ["r1"])
                tr.op('dve', lambda e: e.tensor_tensor(out=krf[:, 0:32], in0=krf[:, 0:32], in1=r1[:, 0, :], op=ALU.subtract), ["krf", "r1"], ["krf"])
                tr.op('dve', lambda e: e.tensor_tensor(out=krf[:, 32:64], in0=x2, in1=cosb[:], op=ALU.mult), [psk(4), "cosb"], ["krf"])
                tr.op('dve', lambda e: e.tensor_tensor(out=r1[:, 0, :], in0=x1, in1=sinb[:], op=ALU.mult), [psk(4), "sinb"], ["r1"])
                tr.op('dve', lambda e: e.tensor_tensor(out=krf[:, 32:64], in0=krf[:, 32:64], in1=r1[:, 0, :], op=ALU.add), ["krf", "r1"], ["krf"])
                tr.dma('sp', (kr_s[:, :] if samp else kr_p[ti * 128:(ti + 1) * 128, :]), krf[:], ["krf"], ["krout"], "krf")
                tr.op('act', lambda e: e.activation(out=krb[:], in_=krf[:], func=AF.Copy), ["krf"], ["krb"])
                for cc in range(2):
                    tr.op('pe', lambda e, cc=cc: e.transpose(out=ptb6[:, cc * 128:(cc + 1) * 128], in_=ctk[:, cc * 128:(cc + 1) * 128], identity=idb[:]), [ctkey, "idb"], [psk(6)], inc=False)
                tr.op('pe', lambda e: e.transpose(out=ptb6[0:64, 256:384], in_=krb[:, :], identity=idb[:]), ["krb", "idb"], [psk(6)])
                cTd = cTs[:] if samp else cT[:, :, ti * 128:(ti + 1) * 128]
                cTk = "cTs" if samp else ("cT", ti)
                krTd = krTs[:] if samp else krT[:, ti * 128:(ti + 1) * 128]
                tr.op('dve', lambda e: e.tensor_copy(out=cTd, in_=ptb6[:, 0:256].rearrange("p (c t) -> p c t", c=2)), [psk(6)], [cTk])
                tr.op('dve', lambda e: e.tensor_copy(out=krTd, in_=ptb6[0:64, 256:384]), [psk(6)], [cTk])
                for dc in range(8):
                    tr.op('pe', lambda e, dc=dc: e.matmul(ps[5][:, 0:QL], lhsT=xnbT[:, dc, :], rhs=wdq_sb[:, dc, :], start=(dc == 0), stop=(dc == 7)),
                          ["xnbT", "wdq_sb"], [psk(5)], inc=(dc == 7))
                rms_scale((jk3, ss3, rs3), ps[5][:, 0:QL], psk(5), QL, jkey="jk3")
                tr.op('act', lambda e: e.activation(out=cqb[:], in_=ps[5][:, 0:QL], func=AF.Copy, scale=rs3[:, 0:1]), [psk(5), "rstd"], ["cqb"])
                ptb7 = ps[7][:, :].bitcast(BF16)
                for c in range(3):
                    tr.op('pe', lambda e, c=c: e.transpose(out=ptb7[:, c * 128:(c + 1) * 128], in_=cqb[:, c * 128:(c + 1) * 128], identity=idb[:]), ["cqb", "idb"], [psk(7)], inc=(c == 2))
                tr.op('dve', lambda e: e.tensor_tensor(out=cqT[:], in0=ptb7[:, 0:384].rearrange("p (c t) -> p c t", c=3), in1=gqs[:].unsqueeze(2).broadcast_to([128, 3, 128]), op=ALU.mult),
                      [psk(7), "gqs"], ["cqT"])
                for h in range(NH):
                    b = 4 + h // 4
                    for qc in range(3):
                        tr.op('pe', lambda e, h=h, qc=qc, b=b: e.matmul(ps[b][:, (h % 4) * 128:(h % 4 + 1) * 128], lhsT=wuq_sb[:, qc, h * 192:h * 192 + 128], rhs=cqT[:, qc, :],
                                                                        start=(qc == 0), stop=(qc == 2)), ["wuq_sb", "cqT"], [psk(b)], inc=(qc == 2 and h % 4 == 3))
                for hb in range(2):
                    tr.op('act', lambda e, hb=hb: e.activation(out=qnT[:, hb * 4:(hb + 1) * 4, :], in_=ps[4 + hb][:, :].rearrange("p (h t) -> p h t", h=4), func=AF.Copy), [psk(4 + hb)], ["qnT"])
                wr_ = wuq_sb[:, :, :].rearrange("p c (h x) -> p c h x", h=NH)
                for qc in range(3):
                    tr.op('pe', lambda e, qc=qc: e.matmul(ps[6][:, :].rearrange("p (h x) -> p h x", h=NH), lhsT=cqT[:, qc, :], rhs=wr_[:, qc, :, 128:192], start=(qc == 0), stop=(qc == 2)),
                          ["wuq_sb", "cqT"], [psk(6)], inc=(qc == 2))
                q3 = ps[6][:, :].rearrange("p (h x) -> p h x", h=NH)
                cb3 = cosb[:].unsqueeze(1).broadcast_to([128, NH, 32]); sb3 = sinb[:].unsqueeze(1).broadcast_to([128, NH, 32])
                tr.op('dve', lambda e: e.tensor_tensor(out=qrf[:, :, 0:32], in0=q3[:, :, 0:32], in1=cb3, op=ALU.mult), [psk(6), "cosb"], ["qrf"])
                tr.op('dve', lambda e: e.tensor_tensor(out=r1[:], in0=q3[:, :, 32:64], in1=sb3, op=ALU.mult), [psk(6), "sinb"], ["r1"])
                tr.op('dve', lambda e: e.tensor_tensor(out=qrf[:, :, 0:32], in0=qrf[:, :, 0:32], in1=r1[:], op=ALU.subtract), ["qrf", "r1"], ["qrf"])
                tr.op('dve', lambda e: e.tensor_tensor(out=qrf[:, :, 32:64], in0=q3[:, :, 32:64], in1=cb3, op=ALU.mult), [psk(6), "cosb"], ["qrf"])
                tr.op('dve', lambda e: e.tensor_tensor(out=r2[:], in0=q3[:, :, 0:32], in1=sb3, op=ALU.mult), [psk(6), "sinb"], ["r2"])
                tr.op('dve', lambda e: e.tensor_tensor(out=qrf[:, :, 32:64], in0=qrf[:, :, 32:64], in1=r2[:], op=ALU.add), ["qrf", "r2"], ["qrf"])
                tr.op('act', lambda e: e.activation(out=qrb[:], in_=qrf[:], func=AF.Copy), ["qrf"], ["qrb"])
                for h in range(NH):
                    tr.op('pe', lambda e, h=h: e.transpose(out=ptb7[0:64, h * 128:(h + 1) * 128], in_=qrb[:, h, :], identity=idb[:]), ["qrb", "idb"], [psk(7)], inc=(h == NH - 1))
                tr.op('dve', lambda e: e.tensor_copy(out=qrT[:], in_=ptb7[0:64, :].rearrange("p (h t) -> p h t", h=NH)), [psk(7)], ["qrT"])
                for h in range(NH):
                    for cc in range(2):
                        i_ = h * 2 + cc; b = 4 + i_ // 4
                        tr.op('pe', lambda e, h=h, cc=cc, i_=i_, b=b: e.matmul(ps[b][:, (i_ % 4) * 128:(i_ % 4 + 1) * 128], lhsT=wukT[:, h, cc * 128:(cc + 1) * 128], rhs=qnT[:, h, :],
                                                                              start=True, stop=True), ["wukT", "qnT"], [psk(b)], inc=(i_ % 4 == 3))
                for b in range(4):
                    tr.op('act', lambda e, b=b: e.activation(out=qaT[:, :, 2 * b:2 * b + 2, :].rearrange("p c h t -> p h c t"),
                                                             in_=ps[4 + b][:, :].rearrange("p (h c t) -> p h c t", h=2, c=2), func=AF.Copy), [psk(4 + b)], ["qaT"])
                if not samp:
                    for h in range(NH):
                        nkb = ti // 4 + 1
                        for kb in range(nkb):
                            t0 = kb * 4; t1_ = min(t0 + 4, ti + 1); N = (t1_ - t0) * 128
                            kkeys = [("cT", t_) for t_ in range(t0, t1_)]
                            qk = [(qaT[:, 0, h, :], cT[:, 0, t0 * 128:t1_ * 128], ["qaT"] + kkeys), (qaT[:, 1, h, :], cT[:, 1, t0 * 128:t1_ * 128], ["qaT"] + kkeys),
                                  (qrT[:, h, :], krT[:, t0 * 128:t1_ * 128], ["qrT"] + kkeys)]
                            mask = (cb[:], (ti - t0) * 128, 128, "cb") if kb == nkb - 1 else None
                            vts = [(ctok[:, t_, :], 128, ("ctok", t_)) for t_ in range(t0, t1_)]
                            attend_block(128, qk, N, mask, vts, kb == 0)
                        finish_rows(128)
                        for cc in range(2):
                            tr.op('pe', lambda e, cc=cc: e.transpose(out=ptb7[:, cc * 128:(cc + 1) * 128], in_=olat[:, cc * 128:(cc + 1) * 128], identity=idb[:]), ["olat", "idb"], [psk(7)], inc=(cc == 1))
                        tr.op('act', lambda e, h=h: e.activation(out=olatT[:, :, h, :], in_=ptb7[:, 0:256].rearrange("p (c t) -> p c t", c=2), func=AF.Copy), [psk(7)], ["olatT"])
                else:
                    npg = NPG
                    gn = 0
                    for b_ in range(SB):
                        tr.op('pool', lambda e, b_=b_: e.tensor_copy(out=qas[:], in_=qaT[:, :, :, b_ * ST:(b_ + 1) * ST]), ["qaT"], ["qas"])
                        tr.op('pool', lambda e, b_=b_: e.tensor_copy(out=qrs[:], in_=qrT[:, :, b_ * ST:(b_ + 1) * ST]), ["qrT"], ["qrs"])
                        qa0 = qas[:, 0, :, :].rearrange("p h t -> p (h t)"); qa1 = qas[:, 1, :, :].rearrange("p h t -> p (h t)")
                        qr_ = qrs[:, :, :].rearrange("p h t -> p (h t)")
                        first = True
                        for k0 in range(0, PAGE, KC):
                            gi = gn % 2; gn += 1
                            tr.dma('pool', gc[gi][0:npg].rearrange("p k c -> p (k c)"), ckv.rearrange("n k c -> n (k c)"), ["pti"], ["gc%d" % gi], "gc%d" % gi,
                                   indirect=(pti[0:npg, b_:b_ + 1], k0 * KVL))
                            tr.dma('pool', gr[gi][0:npg].rearrange("p k c -> p (k c)"), ckr.rearrange("n k c -> n (k c)"), ["pti"], ["gr%d" % gi], "gr%d" % gi,
                                   indirect=(pti[0:npg, b_:b_ + 1], k0 * QKR))
                            for r0 in range(0, KC, 4):
                                pg = ps[4 + (r0 // 4) % 2][:, :].bitcast(BF16); pgk = psk(4 + (r0 // 4) % 2)
                                for rr in range(4):
                                    for cc in range(2):
                                        tr.op('pe', lambda e, rr=rr, cc=cc, r0=r0, pg=pg: e.transpose(out=pg[:, cc * 512 + rr * npg: cc * 512 + (rr + 1) * npg], in_=gc[gi][0:npg, r0 + rr, cc * 128:(cc + 1) * 128],
                                                                                                   identity=idb[0:npg, 0:npg]), ["gc%d" % gi, "idb"], [pgk], inc=(rr == 3 and cc == 1))
                                tr.op('dve', lambda e, pg=pg: e.tensor_copy(out=gcT[:, :, 0:4 * npg], in_=pg[:, :].rearrange("p (c n) -> p c n", c=2)[:, :, 0:4 * npg]), [pgk], ["gcT"])
                                pr = ps[6][:, :].bitcast(BF16)
                                for rr in range(4):
                                    tr.op('pe', lambda e, rr=rr, r0=r0: e.transpose(out=pr[0:64, rr * npg:(rr + 1) * npg], in_=gr[gi][0:npg, r0 + rr, :], identity=idb[0:npg, 0:npg]),
                                          ["gr%d" % gi, "idb"], [psk(6)], inc=(rr == 3))
                                tr.op('act', lambda e: e.activation(out=grT[:, 0:4 * npg], in_=pr[0:64, 0:4 * npg], func=AF.Copy), [psk(6)], ["grT"])
                                qk = [(qa0, gcT[:, 0, 0:4 * npg], ["qas", "gcT"]), (qa1, gcT[:, 1, 0:4 * npg], ["qas", "gcT"]), (qr_, grT[:, 0:4 * npg], ["qrs", "grT"])]
                                vts = [(gc[gi][0:npg, r0 + rr, :], npg, "gc%d" % gi) for rr in range(4)]
                                attend_block(64, qk, 4 * npg, None, vts, first)
                                first = False
                        qk = [(qa0, cTs[:, 0, :], ["qas", "cTs"]), (qa1, cTs[:, 1, :], ["qas", "cTs"]), (qr_, krTs[:, :], ["qrs", "cTs"])]
                        attend_block(64, qk, 128, (msk[:, b_, :], 0, 128, "msk"), [(ctoks[:, :], 128, "ctoks")], False)
                        finish_rows(64)
                        for cc in range(2):
                            tr.op('pe', lambda e, cc=cc: e.transpose(out=ptb7[:, cc * 64:(cc + 1) * 64], in_=olat[0:64, cc * 128:(cc + 1) * 128], identity=idb[0:64, 0:64]), ["olat", "idb"], [psk(7)], inc=(cc == 1))
                        tr.op('act', lambda e, b_=b_: e.activation(out=olatT[:, :, :, b_ * ST:(b_ + 1) * ST], in_=ptb7[:, 0:128].rearrange("p (c h t) -> p c h t", c=2, h=NH), func=AF.Copy),
                              [psk(7)], ["olatT"])
                for h in range(NH):
                    b = 4 + h // 4
                    for cc in range(2):
                        tr.op('pe', lambda e, h=h, cc=cc, b=b: e.matmul(ps[b][:, (h % 4) * 128:(h % 4 + 1) * 128], lhsT=wukv_sb[:, cc, h * 256 + 128:h * 256 + 256], rhs=olatT[:, cc, h, :],
                                                                        start=(cc == 0), stop=(cc == 1)), ["wukv_sb", "olatT"], [psk(b)], inc=(cc == 1 and h % 4 == 3))
                for hb in range(2):
                    tr.op('act', lambda e, hb=hb: e.activation(out=oT[:, hb * 4:(hb + 1) * 4, :], in_=ps[4 + hb][:, :].rearrange("p (h t) -> p h t", h=4), func=AF.Copy), [psk(4 + hb)], ["oT"])
                for hb in range(2):
                    for h in range(NH):
                        tr.op('pe', lambda e, hb=hb, h=h: e.matmul(ps[4 + hb][:, :], lhsT=oT[:, h, :], rhs=woutb_sb[:, h, hb * 512:(hb + 1) * 512], start=(h == 0), stop=(h == NH - 1)),
                              ["oT", "woutb_sb"], [psk(4 + hb)], inc=(h == NH - 1))
                for hb in range(2):
                    tr.op('dve', lambda e, hb=hb: e.tensor_tensor(out=ht[:, hb * 512:(hb + 1) * 512], in0=ht[:, hb * 512:(hb + 1) * 512], in1=ps[4 + hb][:, :], op=ALU.add), [psk(4 + hb), "ht"], ["ht"])
                tr.dma('sp', hA[ti], ht[:], ["ht"], [("hA", ti)], "ht")

        outk = ["hg_p", "s0f", "cf", "krf"]
        if phases == 3:
            emit_debug(hA, outk)
            return nc, tr
        ffn_phase(hA, None, 4, NE, True)
        tr.final_wait('sp', outk + ["yst"])
    return nc, tr


def _consts(T, NPG):
    NT = T // 128
    bf = ml_dtypes.bfloat16
    c = {}
    c["c_idf"] = np.eye(128, dtype=np.float32)
    c["c_idb"] = np.eye(128, dtype=np.float32).astype(bf)
    s = np.arange(128)[:, None]; t = np.arange(128)[None, :]
    tri = np.zeros((2, 128, 128), np.float32); up = np.zeros((2, 128, 128), np.float32)
    for a, L in enumerate((64, 8)):
        same = (s // L) == (t // L)
        tri[a] = (same & (s <= t)).astype(np.float32)
        up[a] = (same & (s > t)).astype(np.float32)
    c["c_tri"] = tri; c["c_up"] = up
    c["c_cb"] = np.where(t <= s, 0.0, NEG).astype(np.float32)
    r = np.arange(64)[:, None]; kk = np.arange(8)[None, :]
    c["c_cbs"] = np.where(kk <= (r % 8), 0.0, NEG).astype(np.float32)
    rr_ = np.arange(64)[None, :, None]; kk_ = np.arange(128)[None, None, :]; bb_ = np.arange(SB)[:, None, None]
    c["c_msk"] = np.where(((kk_ // ST) == bb_) & ((kk_ % ST) <= (rr_ % ST)), 0.0, NEG).astype(np.float32)
    c["c_bm"] = ((np.arange(128)[:, None] // ST) == np.arange(SB)[None, :]).astype(np.float32)
    half = 32
    inv = (10000.0 ** (-np.arange(half, dtype=np.float32) / half)).astype(np.float32)
    pos = np.zeros((NT + 1, 128), np.float32)
    pos[:NT] = np.arange(T, dtype=np.float32).reshape(NT, 128)
    pos[NT] = (NPG * PAGE + (np.arange(128) % ST)).astype(np.float32)
    ang = pos[:, :, None] * inv[None, None, :]
    c["c_cos"] = np.cos(ang).astype(np.float32); c["c_sin"] = np.sin(ang).astype(np.float32)
    return c


def make_in_maps(inp, T, NPG):
    f = lambda a: np.ascontiguousarray(np.asarray(a))
    cst = _consts(T, NPG)
    gl = f(inp["gamma_lb"])
    shared = {
        "ckv": f(inp["cache_ckv"]), "ckr": f(inp["cache_krope"]),
        "w_in": f(inp["w_in_a"][0]), "w_outa": f(inp["w_out_a"][0]),
        "glb": gl, "glbT": f(gl.reshape(2, NH, 128).transpose(2, 0, 1)),
        "gv": f(np.stack([inp["g_mix_a"][0], inp["g_ffn"][0], inp["g_kv_in"], inp["g_mix_b"][0], inp["g_ffn"][1]], 0).reshape(5, 8, 128).transpose(2, 0, 1)),
        "gq": f(np.asarray(inp["g_q"][0]).reshape(3, 128).T),
        "g_o": f(inp["g_onorm_a"][0]), "g_kv": f(inp["g_kv"]), "g_fin": f(inp["g_final"]),
        "w_dkv": f(inp["w_dkv"]), "w_ukv": f(inp["w_ukv"]), "w_dq": f(inp["w_dq"][0]), "w_uq": f(inp["w_uq"][0]),
        "w_outb": f(inp["w_out_b"][0]),
        "w_fg": f(inp["w_ff_gate"][0]), "w_fu": f(inp["w_ff_up"][0]), "w_fd": f(inp["w_ff_down"][0]),
        "w_r": f(inp["w_router"][0]), "w_eg": f(inp["w_e_gate"][0]), "w_eu": f(inp["w_e_up"][0]), "w_ed": f(inp["w_e_down"][0]),
    }
    shared.update(cst)
    maps = []
    xp = np.asarray(inp["x_prompt"]); xs_ = np.asarray(inp["x_sample"]); st = np.asarray(inp["state_hgrn"]); pt = np.asarray(inp["page_table"])
    for c in range(NCORES):
        m = dict(shared)
        m["x_p"] = f(xp[c]); m["x_s"] = f(xs_[c * SB:(c + 1) * SB].reshape(SB * ST, D))
        m["st_in"] = f(st[0, c * SB:(c + 1) * SB]); m["ptT"] = f(pt[c * SB:(c + 1) * SB].T.astype(np.int32))
        maps.append(m)
    return maps


def kernel(**inputs):
    T = int(np.asarray(inputs["x_prompt"]).shape[1])
    NPG = int(np.asarray(inputs["page_table"]).shape[1])
    NPOOL = int(np.asarray(inputs["cache_ckv"]).shape[0])
    nc, _ = build(T, NPG, NPOOL)
    maps = make_in_maps(inputs, T, NPG)
    res = run_bass_kernel_spmd(nc, maps, core_ids=list(range(NCORES)))
    r = res.results
    f32 = np.float32
    y_p = np.stack([r[c]["y_p"] for c in range(NCORES)]).astype(f32)
    y_s = np.concatenate([r[c]["y_s"].reshape(SB, ST, D) for c in range(NCORES)], 0).astype(f32)
    ckv_p = np.stack([r[c]["ckv_p"] for c in range(NCORES)]).astype(f32)
    kr_p = np.stack([r[c]["kr_p"] for c in range(NCORES)]).astype(f32)
    ckv_s = np.concatenate([r[c]["ckv_s"].reshape(SB, ST, KVL) for c in range(NCORES)], 0).astype(f32)
    kr_s = np.concatenate([r[c]["kr_s"].reshape(SB, ST, QKR) for c in range(NCORES)], 0).astype(f32)
    hg_p = np.stack([r[c]["hg_p"] for c in range(NCORES)])[None].astype(f32)
    hg_s = np.concatenate([r[c]["hg_s"] for c in range(NCORES)], 0)[None].astype(f32)
    return (y_p, y_s, ckv_p, kr_p, ckv_s, kr_s, hg_p, hg_s)
```
